# Optimizing a Trainium2 kernel written in Bass

```python
import math
import jax, jax.numpy as jnp
from jax import lax
import numpy as np

D_MODEL = 1024
BATCH = 8
SEQ = 2048
DEPTH = 4

CHUNK = 64
Q_BLOCK = 128
N_BRANCH = 3
MIX_WIDTH = D_MODEL // 2

N_HEADS_A = 4
HEAD_DIM_A = MIX_WIDTH // (2 * N_HEADS_A)
LAMBDA_INIT_BASE = 0.8
LAMBDA_INIT_SCALE = 0.6
LAMBDA_INIT_DECAY = 0.3
N_REL_BUCKETS = 32
REL_MAX_DIST = 128

SGU_CHUNK = 128
SGU_GROUPS = 4
SGU_WIDTH = MIX_WIDTH
SGU_GROUP_CH = SGU_WIDTH // SGU_GROUPS

N_HEADS_C = 8
Q_LORA = D_MODEL // 4
KV_LORA = D_MODEL // 8
NOPE_DIM = 64
ROPE_DIM = 32
V_DIM_C = MIX_WIDTH // N_HEADS_C
ROPE_THETA = 10000.0

N_GROUPS = 4
EXPERTS_PER_GROUP = 8
TOP_K_IN_GROUP = 2
D_FF_EXPERT = D_MODEL // 4

EPS = 1e-6
NEG_INF = -1e30
MAX_POS_OFFSET = 8192

IN_SIZES = (N_HEADS_A * 2 * HEAD_DIM_A,
            N_HEADS_A * 2 * HEAD_DIM_A,
            N_HEADS_A * 2 * HEAD_DIM_A,
            2 * SGU_WIDTH,
            Q_LORA,
            KV_LORA,
            ROPE_DIM,
            N_BRANCH * D_MODEL)
IN_WIDTH = sum(IN_SIZES)
IN_SPLITS = tuple(int(v) for v in np.cumsum(IN_SIZES)[:-1])

kernel_name = "hybrid_chunk_causal_diffattn_sgu_mla_hmoe"


def rms_norm(x, g):
    xf = x.astype(jnp.float32)
    y = xf * lax.rsqrt(jnp.mean(xf * xf, axis=-1, keepdims=True) + EPS)
    return (y * g.astype(jnp.float32)).astype(x.dtype)


def chunk_mask(q_start, kv_len):
    q_chunk = (q_start + jnp.arange(Q_BLOCK)) // CHUNK
    k_chunk = jnp.arange(kv_len) // CHUNK
    return k_chunk[None, :] <= q_chunk[:, None]


def t5_bucket(rel):
    nb = N_REL_BUCKETS // 2
    max_exact = nb // 2
    n = jnp.abs(rel)
    large = max_exact + (jnp.log(jnp.maximum(n, 1).astype(jnp.float32) / max_exact)
                         / math.log(REL_MAX_DIST / max_exact) * (nb - max_exact)).astype(jnp.int32)
    large = jnp.minimum(large, nb - 1)
    return jnp.where(rel > 0, nb, 0) + jnp.where(n < max_exact, n, large)


def rel_bias_block(table, q_start, kv_len):
    rel = jnp.arange(kv_len)[None, :] - (q_start + jnp.arange(Q_BLOCK))[:, None]
    return jnp.transpose(table[t5_bucket(rel)], (2, 0, 1)).astype(jnp.float32)


def masked_softmax(scores, mask):
    return jax.nn.softmax(jnp.where(mask, scores, NEG_INF), axis=-1)


def block_sweep(fn, seq_len):
    return jnp.concatenate([fn(start, start + Q_BLOCK) for start in range(0, seq_len, Q_BLOCK)], axis=1)


def apply_rope(x, cos, sin):
    half = x.shape[-1] // 2
    x1, x2 = x[..., :half], x[..., half:]
    return jnp.concatenate([x1 * cos - x2 * sin, x2 * cos + x1 * sin], axis=-1)


def differential_attention(q, k, v, lam, lambda_init, out_g, rel_table):
    b, s = q.shape[:2]
    scale = HEAD_DIM_A ** -0.5

    def block(start, end):
        sc = jnp.einsum('bqhmd,bkhmd->bmhqk', q[:, start:end], k[:, :end]).astype(jnp.float32) * scale
        sc = sc + rel_bias_block(rel_table, start, end)
        p = masked_softmax(sc, chunk_mask(start, end))
        attn = p[:, 0] - lam * p[:, 1]
        return jnp.einsum('bhqk,bkhe->bqhe', attn.astype(v.dtype), v[:, :end])

    o = block_sweep(block, s)
    o = rms_norm(o, out_g) * (1.0 - lambda_init)
    return o.reshape(b, s, N_HEADS_A * 2 * HEAD_DIM_A)


def spatial_gating(z, v_g, w_s, b_s):
    b, s = z.shape[:2]
    z = jax.nn.gelu(z)
    u, v = jnp.split(z, 2, axis=-1)
    v = rms_norm(v, v_g).reshape(b, s // SGU_CHUNK, SGU_CHUNK, SGU_GROUPS, SGU_GROUP_CH)
    pos_chunk = jnp.arange(SGU_CHUNK) // CHUNK
    allowed = pos_chunk[None, :] <= pos_chunk[:, None]
    w = jnp.where(allowed[None], w_s, 0)
    vs = jnp.einsum('gij,bnjgc->bnigc', w, v) + jnp.transpose(b_s)[:, :, None]
    return u * vs.reshape(b, s, SGU_WIDTH)


def latent_attention(xq, xkv, xr, cos, sin, lat_g, w_uq, w_ukv, qk_g):
    b, s = xq.shape[:2]
    cq = rms_norm(xq, lat_g[:Q_LORA])
    ckv = rms_norm(xkv, lat_g[Q_LORA:])
    q = (cq @ w_uq).reshape(b, s, N_HEADS_C, NOPE_DIM + ROPE_DIM)
    kv = (ckv @ w_ukv).reshape(b, s, N_HEADS_C, NOPE_DIM + V_DIM_C)
    q_nope = rms_norm(q[..., :NOPE_DIM], qk_g[0, :NOPE_DIM])
    q_rope = apply_rope(rms_norm(q[..., NOPE_DIM:], qk_g[0, NOPE_DIM:]), cos[:, :, None], sin[:, :, None])
    k_nope = rms_norm(kv[..., :NOPE_DIM], qk_g[1, :NOPE_DIM])
    v = kv[..., NOPE_DIM:]
    k_rope = apply_rope(rms_norm(xr, qk_g[1, NOPE_DIM:]), cos, sin)
    scale = (NOPE_DIM + ROPE_DIM) ** -0.5

    def block(start, end):
        sc = (jnp.einsum('bqhd,bkhd->bhqk', q_nope[:, start:end], k_nope[:, :end])
              + jnp.einsum('bqhr,bkr->bhqk', q_rope[:, start:end], k_rope[:, :end])).astype(jnp.float32) * scale
        p = masked_softmax(sc, chunk_mask(start, end))
        return jnp.einsum('bhqk,bkhe->bqhe', p.astype(v.dtype), v[:, :end])

    return block_sweep(block, s).reshape(b, s, N_HEADS_C * V_DIM_C)


def hierarchical_moe(h, wg, bg, we, be, w_gate, w_up, w_down):
    b, s, d = h.shape
    hf = h.reshape(-1, d)
    n = hf.shape[0]
    g_prob = jax.nn.softmax((hf @ wg + bg).astype(jnp.float32), axis=-1)
    g_w, g_idx = lax.top_k(g_prob, 1)
    g_onehot = jax.nn.one_hot(g_idx[:, 0], N_GROUPS, dtype=jnp.float32)
    e_logits = (hf @ we + be).astype(jnp.float32).reshape(n, N_GROUPS, EXPERTS_PER_GROUP)
    e_logits = jnp.einsum('nge,ng->ne', e_logits, g_onehot)
    e_w, e_idx = lax.top_k(jax.nn.softmax(e_logits, axis=-1), TOP_K_IN_GROUP)
    e_w = e_w / jnp.sum(e_w, axis=-1, keepdims=True)
    e_gate = jnp.sum(jax.nn.one_hot(e_idx, EXPERTS_PER_GROUP, dtype=jnp.float32) * e_w[..., None], axis=1)
    comb = (g_onehot[:, :, None] * (g_w * e_gate)[:, None, :]).astype(h.dtype)
    out = jnp.zeros_like(hf)
    for g in range(N_GROUPS):
        a = jnp.einsum('nd,edf->nef', hf, w_gate[g])
        u = jnp.einsum('nd,edf->nef', hf, w_up[g])
        act = jax.nn.silu(a) * u * comb[:, g, :, None]
        out = out + jnp.einsum('nef,efd->nd', act, w_down[g])
    return out.reshape(b, s, d)


def setup_inputs(seed: int = 0) -> dict:
    key = jax.random.key(seed)
    ks = jax.random.split(key, 32)
    f32 = jnp.float32

    def nrm(k, shape, scale):
        return jax.random.normal(k, shape, f32) * scale

    def gain(k, shape):
        return 1.0 + 0.02 * jax.random.normal(k, shape, f32)

    G, E, F, D = N_GROUPS, EXPERTS_PER_GROUP, D_FF_EXPERT, D_MODEL
    offset = jax.random.randint(ks[2], (BATCH, 1), 0, MAX_POS_OFFSET, dtype=jnp.int32)
    positions = (offset + jnp.arange(SEQ, dtype=jnp.int32)[None, :]).astype(jnp.int32)
    return {
        "x": nrm(ks[0], (BATCH, SEQ, D), 1.0),
        "c": nrm(ks[1], (BATCH, D), 1.0),
        "positions": positions,
        "w_ada": nrm(ks[3], (DEPTH, D, 6 * D), 0.3 * D ** -0.5),
        "b_ada": nrm(ks[4], (DEPTH, 6 * D), 0.01),
        "norm_g": gain(ks[5], (DEPTH, 2, D)),
        "w_in": nrm(ks[6], (DEPTH, D, IN_WIDTH), D ** -0.5),
        "diff_qk_g": gain(ks[7], (DEPTH, 2, HEAD_DIM_A)),
        "diff_lambda": nrm(ks[8], (DEPTH, 4, HEAD_DIM_A), 0.1),
        "diff_out_g": gain(ks[9], (DEPTH, 2 * HEAD_DIM_A)),
        "rel_bias": nrm(ks[10], (N_REL_BUCKETS, N_HEADS_A), 0.5),
        "sgu_v_g": gain(ks[11], (DEPTH, SGU_WIDTH)),
        "sgu_w": nrm(ks[12], (DEPTH, SGU_GROUPS, SGU_CHUNK, SGU_CHUNK), 0.5 * SGU_CHUNK ** -0.5),
        "sgu_b": 1.0 + nrm(ks[13], (DEPTH, SGU_GROUPS, SGU_CHUNK), 0.05),
        "mla_lat_g": gain(ks[14], (DEPTH, Q_LORA + KV_LORA)),
        "mla_w_uq": nrm(ks[15], (DEPTH, Q_LORA, N_HEADS_C * (NOPE_DIM + ROPE_DIM)), Q_LORA ** -0.5),
        "mla_w_ukv": nrm(ks[16], (DEPTH, KV_LORA, N_HEADS_C * (NOPE_DIM + V_DIM_C)), KV_LORA ** -0.5),
        "mla_qk_g": gain(ks[17], (DEPTH, 2, NOPE_DIM + ROPE_DIM)),
        "w_branch": nrm(ks[18], (DEPTH, N_BRANCH, MIX_WIDTH, D), MIX_WIDTH ** -0.5),
        "w_out": nrm(ks[19], (DEPTH, D, D), D ** -0.5),
        "router_g_w": nrm(ks[20], (DEPTH, D, G), D ** -0.5),
        "router_g_b": nrm(ks[21], (DEPTH, G), 0.01),
        "router_e_w": nrm(ks[22], (DEPTH, D, G * E), D ** -0.5),
        "router_e_b": nrm(ks[23], (DEPTH, G * E), 0.01),
        "w_e_gate": nrm(ks[24], (DEPTH, G, E, D, F), D ** -0.5),
        "w_e_up": nrm(ks[25], (DEPTH, G, E, D, F), D ** -0.5),
        "w_e_down": nrm(ks[26], (DEPTH, G, E, F, D), F ** -0.5),
    }


def reference(x, c, positions, w_ada, b_ada, norm_g, w_in, diff_qk_g, diff_lambda, diff_out_g, rel_bias,
              sgu_v_g, sgu_w, sgu_b, mla_lat_g, mla_w_uq, mla_w_ukv, mla_qk_g, w_branch, w_out,
              router_g_w, router_g_b, router_e_w, router_e_b, w_e_gate, w_e_up, w_e_down):
    b, s, d = x.shape
    inv_freq = ROPE_THETA ** (-jnp.arange(0, ROPE_DIM, 2, dtype=jnp.float32) / ROPE_DIM)
    ang = positions.astype(jnp.float32)[..., None] * inv_freq
    cos = jnp.cos(ang).astype(x.dtype)
    sin = jnp.sin(ang).astype(x.dtype)
    c_act = jax.nn.silu(c)

    for l in range(DEPTH):
        mod = c_act @ w_ada[l] + b_ada[l]
        shift1, scale1, gate1, shift2, scale2, gate2 = [m[:, None, :] for m in jnp.split(mod, 6, axis=-1)]

        h = rms_norm(x, norm_g[l, 0]) * (1 + scale1) + shift1
        xa_q, xa_k, xa_v, xb, xc_q, xc_kv, xc_r, xg = jnp.split(h @ w_in[l], IN_SPLITS, axis=-1)

        lambda_init = LAMBDA_INIT_BASE - LAMBDA_INIT_SCALE * math.exp(-LAMBDA_INIT_DECAY * l)
        dl = diff_lambda[l].astype(jnp.float32)
        lam = jnp.exp(jnp.sum(dl[0] * dl[1])) - jnp.exp(jnp.sum(dl[2] * dl[3])) + lambda_init
        qa = rms_norm(xa_q.reshape(b, s, N_HEADS_A, 2, HEAD_DIM_A), diff_qk_g[l, 0])
        ka = rms_norm(xa_k.reshape(b, s, N_HEADS_A, 2, HEAD_DIM_A), diff_qk_g[l, 1])
        va = xa_v.reshape(b, s, N_HEADS_A, 2 * HEAD_DIM_A)
        o_a = differential_attention(qa, ka, va, lam, lambda_init, diff_out_g[l], rel_bias)

        o_b = spatial_gating(xb, sgu_v_g[l], sgu_w[l], sgu_b[l])

        o_c = latent_attention(xc_q, xc_kv, xc_r, cos, sin, mla_lat_g[l], mla_w_uq[l], mla_w_ukv[l], mla_qk_g[l])

        branches = jnp.stack([o_a, o_b, o_c], axis=2)
        gates = jax.nn.sigmoid(xg.reshape(b, s, N_BRANCH, d))
        merged = jnp.einsum('bsnd,bsnd->bsd', gates, jnp.einsum('bsnw,nwd->bsnd', branches, w_branch[l]))
        x = x + gate1 * (merged @ w_out[l])

        h = rms_norm(x, norm_g[l, 1]) * (1 + scale2) + shift2
        x = x + gate2 * hierarchical_moe(h, router_g_w[l], router_g_b[l], router_e_w[l], router_e_b[l],
                                         w_e_gate[l], w_e_up[l], w_e_down[l])
    return x
```

```python
import math
import os
from contextlib import ExitStack

import numpy as np
import concourse.bass as bass
import concourse.mybir as mybir
from concourse.bass_utils import run_bass_kernel_spmd

F32 = mybir.dt.float32
BF16 = mybir.dt.bfloat16
I32 = mybir.dt.int32
AF = mybir.ActivationFunctionType
ALU = mybir.AluOpType
AX = mybir.AxisListType

DEPTH = 4
S = 2048
D = 1024
NB = 4
TB = 512
EPS = 1e-6
IN_W = 6048
LAMBDA_INIT = [0.8 - 0.6 * math.exp(-0.3 * l) for l in range(DEPTH)]


class Eng:
    def __init__(self, name, handle, sem, step, issuer=None):
        self.name = name
        self.h = handle
        self.sem = sem
        self.step = step
        self.count = 0
        self.issuer = issuer or self
        self.waited = {}


class Buf:
    __slots__ = ("name", "w", "r", "excl")

    def __init__(self, name="", excl=False):
        self.name = name
        self.w = None
        self.r = []
        self.excl = excl


class FW:
    def __init__(self, nc, es):
        self.nc = nc
        self.es = es
        self.engs = {}
        self.ninstr = 0
        self.disabled = False

    def add_engine(self, name, handle, step=1, issuer=None):
        sem = self.es.enter_context(self.nc.semaphore("s_" + name))
        e = Eng(name, handle, sem, step, issuer)
        self.engs[name] = e
        return e

    def _wait(self, eng, dep):
        e2, cnt = dep
        iss = eng.issuer
        if iss.waited.get(e2.name, 0) >= cnt:
            return
        iss.h.wait_ge(e2.sem, cnt * e2.step)
        iss.waited[e2.name] = cnt
        self.ninstr += 1

    def op(self, eng, fn, reads=(), writes=(), inc=True):
        if self.disabled:
            return None
        iss = eng.issuer
        for b in reads:
            if b.excl:
                for r in b.r:
                    if r[0].issuer is not iss:
                        self._wait(eng, r)
            if b.w is not None:
                if b.w[0] is iss and iss.name == "pe":
                    continue
                self._wait(eng, b.w)
        for b in writes:
            if b.w is not None and (b.w[0].issuer is not iss or b.w[0].step == 16):
                self._wait(eng, b.w)
            for r in b.r:
                if r[0] is iss and iss.step == 1 and eng.step == 1:
                    continue
                self._wait(eng, r)
        ins = fn()
        self.ninstr += 1
        if inc:
            eng.count += 1
            ins.then_inc(eng.sem, eng.step)
            tag = (eng, eng.count)
        else:
            tag = (eng, eng.count + 1)
        for b in reads:
            b.r.append(tag)
            if len(b.r) > 48:
                best = {}
                for (e, c) in b.r:
                    if e.name not in best or best[e.name][1] < c:
                        best[e.name] = (e, c)
                b.r = list(best.values())
        for b in writes:
            b.w = tag
            b.r = []
        return ins

    def barrier(self):
        if self.disabled:
            return
        issuers = {}
        for e in self.engs.values():
            issuers[e.issuer.name] = e.issuer
        for iss in issuers.values():
            for e2 in self.engs.values():
                if e2 is iss or e2.count == 0:
                    continue
                if iss.waited.get(e2.name, 0) >= e2.count:
                    continue
                iss.h.wait_ge(e2.sem, e2.count * e2.step)
                iss.waited[e2.name] = e2.count
                self.ninstr += 1


class Ring:
    def __init__(self, K, name, shape, dt, n, psum=False):
        self.items = []
        for i in range(n):
            self.items.append(K.alloc(f"{name}{i}", shape, dt))
        self.i = 0

    def next(self):
        it = self.items[self.i % len(self.items)]
        self.i += 1
        return it


class StopBuild(Exception):
    pass


class K:
    def __init__(self, depth, dbg=None):
        self.stop = int(os.environ.get("K_STOP", 99))
        self.depth = depth
        self.dbg = dbg or {}
        self.nc = bass.Bass("TRN2", target_bir_lowering=False)
        self.dram = {}
        self.dbg_out = {}

    def din(self, name, shape, dt=F32):
        self.dram[name] = self.nc.dram_tensor(name, list(shape), dt, kind="ExternalInput").ap()
        return self.dram[name]

    def alloc(self, name, shape, dt, es=None):
        es = es or self.es
        self.uid = getattr(self, "uid", 0) + 1
        t = es.enter_context(self.nc.sbuf_tensor(f"sb{self.uid}_{name}", list(shape), dt))
        return t, Buf(name)

    def MM(self, out, lhsT, rhs, start, stop, rd, wr, inc=None):
        nc = self.nc
        if inc is None:
            inc = stop
        return self.fw.op(self.pe, lambda: nc.tensor.matmul(out, lhsT=lhsT, rhs=rhs, start=start, stop=stop),
                          rd, wr, inc)

    def ACT(self, out, in_, func, rd, wr, bias=None, scale=None):
        nc = self.nc
        kw = {}
        if bias is not None:
            kw["bias"] = bias
        if scale is not None:
            kw["scale"] = scale
        return self.fw.op(self.act, lambda: nc.scalar.activation(out=out, in_=in_, func=func, **kw), rd, wr)

    def TT(self, out, in0, in1, op, rd, wr, eng=None):
        nc = self.nc
        eng = eng or self.dve
        return self.fw.op(eng, lambda: eng.h.tensor_tensor(out=out, in0=in0, in1=in1, op=op), rd, wr)

    def STT(self, out, in0, scalar, in1, op0, op1, rd, wr):
        nc = self.nc
        return self.fw.op(self.dve, lambda: nc.vector.scalar_tensor_tensor(out=out, in0=in0, scalar=scalar, in1=in1,
                                                                           op0=op0, op1=op1), rd, wr)

    def TS(self, out, in0, s1, s2, op0, op1, rd, wr, eng=None):
        eng = eng or self.dve
        if op1 is None:
            return self.fw.op(eng, lambda: eng.h.tensor_scalar(out=out, in0=in0, scalar1=s1, scalar2=None, op0=op0),
                              rd, wr)
        return self.fw.op(eng, lambda: eng.h.tensor_scalar(out=out, in0=in0, scalar1=s1, scalar2=s2, op0=op0, op1=op1),
                          rd, wr)

    def CP(self, out, in_, rd, wr, eng=None):
        eng = eng or self.dve
        return self.fw.op(eng, lambda: eng.h.tensor_copy(out=out, in_=in_), rd, wr)

    def RECIP(self, out, in_, rd, wr):
        nc = self.nc
        return self.fw.op(self.dve, lambda: nc.vector.reciprocal(out=out, in_=in_), rd, wr)

    def MSET(self, ap, val, wr, eng=None):
        eng = eng or self.dve
        return self.fw.op(eng, lambda: eng.h.memset(ap, val), (), wr)

    def _dstream(self, wr, issuer, handle, pref):
        key = pref + (wr[0].name if len(wr) else "_out")
        if key not in self.dstreams:
            self.dstreams[key] = self.fw.add_engine(key, handle, step=16, issuer=issuer)
        return self.dstreams[key]

    def DMA(self, out, in_, rd, wr):
        nc = self.nc
        st = self._dstream(wr, self.sp, nc.sync, "dq_")
        return self.fw.op(st, lambda: nc.sync.dma_start(out=out, in_=in_), rd, wr)

    def DMAC(self, out, in_, rd, wr):
        nc = self.nc
        st = self._dstream(wr, self.pool, nc.gpsimd, "dg_")
        return self.fw.op(st, lambda: nc.gpsimd.dma_start(out=out, in_=in_), rd, wr)

    def rstd(self, out, ss_ps, inv_n, rd, wr):
        self.ACT(out, ss_ps, AF.Ln, rd, wr, bias=self.cst[:ss_ps.shape[0], 0:1], scale=inv_n)
        self.ACT(out, out, AF.Exp, wr, wr, scale=-0.5)

    def dump(self, name, ap, rd):
        if name not in self.dbg:
            return
        shape = list(ap.shape)
        o = self.nc.dram_tensor("dbg_" + name, shape, ap.dtype, kind="ExternalOutput").ap()
        self.dbg_out[name] = o
        self.DMA(o, ap, rd, ())

    def build(self):
        nc = self.nc
        din = self.din
        xT_d = din("xT", [D, S])
        outT_d = nc.dram_tensor("outT", [D, S], F32, kind="ExternalOutput").ap()
        cT_d = din("cT", [128, 8])
        pos_d = din("pos32", [32, S], I32)
        invf_d = din("invf", [32, 1])
        w_ada_d = din("w_ada", [DEPTH, D, 6 * D])
        b_ada_d = din("b_adaT", [128, DEPTH, 48])
        normg_d = din("norm_gT", [128, DEPTH, 2, 8])
        w_in_d = din("w_in", [DEPTH, D, IN_W])
        qkgA_d = din("qkgA", [128, DEPTH, 2])
        dlam_d = din("dlam", [128, DEPTH, 256])
        goutA_d = din("goutA", [128, DEPTH])
        biasA_d = din("biasA", [128, 8, 128])
        cfar_d = din("cfar", [128, 4])
        sgu_vg_d = din("sgu_vg", [DEPTH, 128, 512])
        sgu_wT_d = din("sgu_wT", [DEPTH, 128, 4, 128])
        sgu_bb_d = din("sgu_bb", [DEPTH, 128, 4, 128])
        latg_d = din("latg", [128, DEPTH, 3])
        w_uq_d = din("w_uq", [DEPTH, 256, 768])
        w_ukv_d = din("w_ukv", [DEPTH, 128, 1024])
        qkgCn_d = din("qkgCn", [128, DEPTH, 2])
        qkgCr_d = din("qkgCr", [32, DEPTH, 2])
        w_br_d = din("w_branch", [DEPTH, 3, 512, D])
        w_out_d = din("w_out", [DEPTH, D, D])
        w_r_d = din("w_r", [DEPTH, D, 36])
        b_r_d = din("b_r", [128, DEPTH, 36])
        weg_d = din("w_e_gate", [DEPTH, 32, D, 256])
        weu_d = din("w_e_up", [DEPTH, 32, D, 256])
        wed_d = din("w_e_down", [DEPTH, 32, 256, D])
        R32_d = din("R32T", [32, 32])
        ident_d = din("ident", [128, 128])

        with ExitStack() as es:
            self.es = es
            fw = self.fw = FW(nc, es)
            self.pe = fw.add_engine("pe", nc.tensor)
            self.act = fw.add_engine("act", nc.scalar)
            self.dve = fw.add_engine("dve", nc.vector)
            self.pool = fw.add_engine("pool", nc.gpsimd)
            self.sp = fw.add_engine("sp", nc.sync)
            self.dstreams = {}
            MM, ACT, TT, STT, TS, CP, RECIP, MSET, DMA, DMAC = (self.MM, self.ACT, self.TT, self.STT, self.TS,
                                                               self.CP, self.RECIP, self.MSET, self.DMA, self.DMAC)
            alloc = self.alloc

            PS = []
            PB = []
            for i in range(8):
                PS.append(es.enter_context(nc.psum_tensor(f"ps{i}", [128, 512], F32)))
                PB.append(Buf(f"ps{i}", excl=True))
            self.psg_i = 0
            self.psg_banks = [6, 7]
            ALLB = list(range(8))

            def psg():
                i = self.psg_banks[self.psg_i % len(self.psg_banks)]
                self.psg_i += 1
                return PS[i], PB[i]

            xT, xTb = alloc("xT", [128, 8, S], F32)
            xTB = [Buf(f"xT{b}") for b in range(NB)]
            ones_bf, ones_b = alloc("ones_bf", [128, 128], BF16)
            bones, bones_b = alloc("bones64", [128, 128], BF16)
            cst, cst_b = alloc("cst", [128, 4], F32)
            self.cst = cst
            ident, ident_b = alloc("ident", [128, 128], F32)
            R32, R32_b = alloc("R32", [32, 32], BF16)
            sinT, sin_b = alloc("sinT", [32, S], BF16)
            cosT, cos_b = alloc("cosT", [32, S], BF16)
            comb_scr = nc.dram_tensor("comb_scr", [32, S], F32, kind="Internal").ap()
            comb_scr_b = Buf("comb_scr")
            hbf, hbf_b = alloc("hbf", [128, 8, TB], BF16)
            self.h_r = alloc("h_r", [128, TB], F32)
            ht = alloc("h_t", [128, 2, TB], F32)
            self.h_t = (ht[0], [Buf("ht0"), Buf("ht1")])
            self.n_sq = Ring(self, "n_sq", [128, TB], BF16, 2)
            self.n_r = Ring(self, "n_r", [128, TB], F32, 4)
            Pr = Ring(self, "Pt", [128, TB], BF16, 4)
            MOD, MOD_b = alloc("MOD", [128, DEPTH, 48], F32)
            A1, A1_b = alloc("A1", [128, DEPTH, 8], F32)
            A2, A2_b = alloc("A2", [128, DEPTH, 8], F32)
            neglam, neglam_b = alloc("neglam", [128, DEPTH], F32)
            gout, gout_b = alloc("gout", [128, DEPTH], F32)
            qkgA, qkgA_b = alloc("qkgA", [128, DEPTH, 2], F32)
            latg, latg_b = alloc("latg", [128, DEPTH, 3], F32)
            qkgCn, qkgCn_b = alloc("qkgCn", [128, DEPTH, 2], F32)
            qkgCr, qkgCr_b = alloc("qkgCr", [32, DEPTH, 2], F32)
            Eb, Eb_b = alloc("Eb", [128, 8, 128], F32)
            cfar, cfar_b = alloc("cfar", [128, 4], F32)
            b_r, b_r_b = alloc("b_r", [128, DEPTH, 36], F32)

            MSET(ones_bf[:], 1.0, [ones_b])
            MSET(bones[:], 0.0, [bones_b])
            MSET(bones[0:64, 0:64], 1.0, [bones_b])
            MSET(bones[64:128, 64:128], 1.0, [bones_b])
            MSET(cst[:, 0:1], EPS, [cst_b])
            MSET(cst[:, 1:2], 0.0, [cst_b])
            DMA(ident[:], ident_d, (), [ident_b])
            DMAC(R32[:], R32_d, (), [R32_b])
            DMA(qkgA[:], qkgA_d, (), [qkgA_b])
            DMA(latg[:], latg_d, (), [latg_b])
            DMA(qkgCn[:], qkgCn_d, (), [qkgCn_b])
            DMA(qkgCr[:], qkgCr_d, (), [qkgCr_b])
            DMA(cfar[:], cfar_d, (), [cfar_b])
            DMA(b_r[:], b_r_d, (), [b_r_b])
            for c in range(8):
                DMA(xT[:, c, :], xT_d[c * 128:(c + 1) * 128, :], (), xTB)

            with ExitStack() as pes:
                pos_i, pos_ib = alloc("pos_i", [32, S], I32, pes)
                ang, ang_b = alloc("ang", [32, S], F32, pes)
                t1, t1_b = alloc("rt1", [32, S], F32, pes)
                t2, t2_b = alloc("rt2", [32, S], F32, pes)
                ki, ki_b = alloc("rki", [32, S], I32, pes)
                invf, invf_b = alloc("invf", [32, 1], F32, pes)
                DMA(pos_i[:], pos_d, (), [pos_ib])
                DMA(invf[:], invf_d, (), [invf_b])
                CP(ang[:], pos_i[:], [pos_ib], [ang_b])
                TS(ang[:], ang[:], invf[:, 0:1], None, ALU.mult, None, [ang_b, invf_b], [ang_b])
                TS(t1[:], ang[:], float(1.0 / (2 * np.pi)), None, ALU.mult, None, [ang_b], [t1_b])
                CP(ki[:], t1[:], [t1_b], [ki_b])
                CP(t1[:], ki[:], [ki_b], [t1_b])
                STT(t2[:], t1[:], -6.28125, ang[:], ALU.mult, ALU.add, [t1_b, ang_b], [t2_b])
                STT(t2[:], t1[:], -0.0019353071795864769, t2[:], ALU.mult, ALU.add, [t1_b, t2_b], [t2_b])
                TS(t1[:], t2[:], float(np.pi), -float(2 * np.pi), ALU.is_gt, ALU.mult, [t2_b], [t1_b])
                TT(t2[:], t2[:], t1[:], ALU.add, [t2_b, t1_b], [t2_b])
                TS(t1[:], t2[:], -float(np.pi), float(2 * np.pi), ALU.is_lt, ALU.mult, [t2_b], [t1_b])
                TT(t2[:], t2[:], t1[:], ALU.add, [t2_b, t1_b], [t2_b])
                ACT(sinT[:], t2[:], AF.Sin, [t2_b], [sin_b])
                TS(t2[:], t2[:], float(np.pi / 2), None, ALU.add, None, [t2_b], [t2_b])
                TS(t1[:], t2[:], float(np.pi), -float(2 * np.pi), ALU.is_gt, ALU.mult, [t2_b], [t1_b])
                TT(t2[:], t2[:], t1[:], ALU.add, [t2_b, t1_b], [t2_b])
                ACT(cosT[:], t2[:], AF.Sin, [t2_b], [cos_b])

                biasA, biasA_b = alloc("biasA", [128, 8, 128], F32, pes)
                ncf, ncf_b = alloc("ncf", [128, 4], F32, pes)
                DMA(biasA[:], biasA_d, (), [biasA_b])
                TS(ncf[:], cfar[:], -1.0, None, ALU.mult, None, [cfar_b], [ncf_b])
                for bi in range(8):
                    ACT(Eb[:, bi, :], biasA[:, bi, :], AF.Exp, [biasA_b, ncf_b], [Eb_b], bias=ncf[:, bi % 4:bi % 4 + 1], scale=1.0)
                c_sb, c_b = alloc("c_sb", [128, 8], F32, pes)
                c_act, cact_b = alloc("c_act", [128, 8], BF16, pes)
                b_ada, bada_b = alloc("b_ada", [128, DEPTH, 48], F32, pes)
                normg, normg_b = alloc("normg", [128, DEPTH, 2, 8], F32, pes)
                dlam, dlam_b = alloc("dlam", [128, DEPTH, 256], F32, pes)
                goutA, goutA_b = alloc("goutA", [128, DEPTH], F32, pes)
                DMA(c_sb[:], cT_d, (), [c_b])
                DMA(b_ada[:], b_ada_d, (), [bada_b])
                DMA(normg[:], normg_d, (), [normg_b])
                DMA(dlam[:], dlam_d, (), [dlam_b])
                DMA(goutA[:], goutA_d, (), [goutA_b])
                ACT(c_act[:], c_sb[:], AF.Silu, [c_b], [cact_b])
                wa_ring = Ring(self, "wada", [128, 8, 1024], BF16, 0)
                wa_ring.items = [alloc(f"wada{i}", [128, 8, 1024], BF16, pes) for i in range(2)]
                for l in range(self.depth):
                    ps, pb = PS[l % 2], PB[l % 2]
                    for piece in range(6):
                        wa, wab = wa_ring.next()
                        src = w_ada_d[l, :, piece * 1024:(piece + 1) * 1024].rearrange("(kc p) n -> p kc n", p=128)
                        DMAC(wa[:], src, (), [wab])
                        for jj in range(8):
                            j = piece * 8 + jj
                            for kc in range(8):
                                MM(ps[:, j:j + 1], wa[:, kc, jj * 128:(jj + 1) * 128], c_act[:, kc:kc + 1],
                                   kc == 0, kc == 7, [wab, cact_b], [pb], inc=(kc == 7 and jj == 7))
                    TT(MOD[:, l, :], ps[:, 0:48], b_ada[:, l, :], ALU.add, [pb, bada_b], [MOD_b])
                for l in range(self.depth):
                    STT(A1[:, l, :], MOD[:, l, 8:16], 1.0, normg[:, l, 0, :], ALU.add, ALU.mult, [MOD_b, normg_b], [A1_b])
                    STT(A2[:, l, :], MOD[:, l, 32:40], 1.0, normg[:, l, 1, :], ALU.add, ALU.mult, [MOD_b, normg_b], [A2_b])
                lt, lt_b = alloc("lam_t", [128, DEPTH, 2, 64], F32, pes)
                ls, ls_b = alloc("lam_s", [128, DEPTH, 2], F32, pes)
                dl4 = dlam[:].rearrange("p l (a d) -> p l a d", a=4)
                TT(lt[:, :, 0, :], dl4[:, :, 0, :], dl4[:, :, 1, :], ALU.mult, [dlam_b], [lt_b])
                TT(lt[:, :, 1, :], dl4[:, :, 2, :], dl4[:, :, 3, :], ALU.mult, [dlam_b], [lt_b])
                fw.op(self.dve, lambda: nc.vector.tensor_reduce(out=ls[:], in_=lt[:], axis=AX.X, op=ALU.add), [lt_b], [ls_b])
                ACT(ls[:], ls[:], AF.Exp, [ls_b], [ls_b])
                for l in range(self.depth):
                    STT(neglam[:, l:l + 1], ls[:, l, 0:1], -1.0, ls[:, l, 1:2], ALU.mult, ALU.add, [ls_b], [neglam_b])
                    TS(neglam[:, l:l + 1], neglam[:, l:l + 1], -LAMBDA_INIT[l], None, ALU.add, None, [neglam_b], [neglam_b])
                    TS(gout[:, l:l + 1], goutA[:, l:l + 1], 1.0 - LAMBDA_INIT[l], None, ALU.mult, None, [goutA_b], [gout_b])
                self.dump("MOD", MOD[:], [MOD_b])
                self.dump("sinT", sinT[:], [sin_b])
                self.dump("cosT", cosT[:], [cos_b])
                self.dump("neglam", neglam[:], [neglam_b])
                fw.barrier()

            def make_h(tb, A, B_ap_fn, hbf, hbf_b, tmp_es, l, hf=None, hf_b=None):
                blk = slice(tb * TB, (tb + 1) * TB)
                r_sb, r_b = self.h_r
                t_sb, t_b = self.h_t
                ps, pb = psg()
                for c in range(8):
                    sq, sq_b = self.n_sq.next()
                    ACT(sq[:], xT[:, c, blk], AF.Square, [xTB[tb]], [sq_b])
                    MM(ps[:], ones_bf[:], sq[:], c == 0, c == 7, [ones_b, sq_b], [pb], inc=True)
                self.rstd(r_sb[:], ps[:], 1.0 / D, [pb, cst_b], [r_b])
                for c in range(8):
                    STT(t_sb[:, c % 2, :], xT[:, c, blk], A[:, l, c:c + 1], r_sb[:], ALU.mult, ALU.mult,
                        [xTB[tb], r_b], [t_b[c % 2]])
                    if hf is not None:
                        ACT(hf[:, c, :], t_sb[:, c % 2, :], AF.Identity, [t_b[c % 2], MOD_b], [hf_b], bias=B_ap_fn(c))
                        CP(hbf[:, c, :], hf[:, c, :], [hf_b], [hbf_b], eng=self.pool)
                    else:
                        ACT(hbf[:, c, :], t_sb[:, c % 2, :], AF.Identity, [t_b[c % 2], MOD_b], [hbf_b], bias=B_ap_fn(c))

            def group_norm_fm(src_ps, src_pb, npart, ones_l, inv_n, g_ap, out_ap, out_bufs, g_bufs):
                sq, sq_b = self.n_sq.next()
                ACT(sq[:npart, :], src_ps, AF.Square, [src_pb], [sq_b])
                ps, pb = psg()
                MM(ps[:npart, :], ones_l, sq[:npart, :], True, True, [ones_b, bones_b, sq_b], [pb])
                r, r_b = self.n_r.next()
                self.rstd(r[:npart, :], ps[:npart, :], inv_n, [pb, cst_b], [r_b])
                STT(out_ap, src_ps, g_ap, r[:npart, :], ALU.mult, ALU.mult, [src_pb, r_b] + g_bufs, out_bufs)

            def rope(x_bf, x_b, blk, out_ap, out_bufs):
                ps, pb = psg()
                MM(ps[0:32, :], R32[:], x_bf, True, True, [R32_b, x_b], [pb])
                ta, ta_b = self.n_r.next()
                tb_, tb_b = self.n_r.next()
                TT(ta[0:32, :], x_bf, cosT[:, blk], ALU.mult, [x_b, cos_b], [ta_b])
                TT(tb_[0:32, :], ps[0:32, :], sinT[:, blk], ALU.mult, [pb, sin_b], [tb_b])
                TT(out_ap, ta[0:32, :], tb_[0:32, :], ALU.add, [ta_b, tb_b], out_bufs)

            def emit_output():
                for c in range(8):
                    DMA(outT_d[c * 128:(c + 1) * 128, :], xT[:, c, :], xTB, ())
                fw.barrier()

            self.sub = float(os.environ.get("K_SUB", 99))

            def sstop(n):
                if self.sub <= n and not fw.disabled:
                    fw.barrier()
                    emit_output()
                    fw.disabled = True

            def stop_at(n):
                if self.stop <= n and not fw.disabled:
                    fw.barrier()
                    emit_output()
                    fw.disabled = True

            if True:
              stop_at(0)
              for l in range(self.depth):
                with ExitStack() as les:
                  with ExitStack() as mes:
                    o_c, o_c_b = alloc("o_c", [128, 4, S], BF16, mes)
                    B1 = lambda c: MOD[:, l, c:c + 1]
                    B2 = lambda c: MOD[:, l, 24 + c:24 + c + 1]

                    with ExitStack() as ses:
                        w_mla, w_mla_b = alloc("w_mla", [128, 8, 416], BF16, ses)
                        w_uq, w_uq_b = alloc("w_uq", [128, 2, 768], BF16, ses)
                        w_ukv, w_ukv_b = alloc("w_ukv", [128, 1024], BF16, ses)
                        Kn, Kn_b = alloc("Kn", [128, 4, S], BF16, ses)
                        Vc, Vc_b = alloc("Vc", [128, 16, 512], BF16, ses)
                        Kr, Kr_b = alloc("Kr", [32, S], BF16, ses)
                        cqn, cqn_b = alloc("cqn", [128, 2, TB], BF16, ses)
                        cqf, cqf_b = alloc("cqf", [128, 2, TB], F32, ses)
                        cqs, cqs_b = alloc("cqs", [128, 2, TB], BF16, ses)
                        ckvn, ckvn_b = alloc("ckvn", [128, TB], BF16, ses)
                        qn, qn_b = alloc("qn", [128, 4, TB], BF16, ses)
                        qr, qr_b = alloc("qr", [32, 8, TB], BF16, ses)
                        xr, xr_b = alloc("xr", [32, TB], BF16, ses)
                        rc, rc_b = alloc("rc", [128, TB], F32, ses)
                        acc_r = [alloc(f"accC{i}", [128, TB], F32, ses) for i in range(2)]
                        accb_r = [alloc(f"accCb{i}", [128, TB], BF16, ses) for i in range(2)]
                        DMAC(w_mla[:], w_in_d[l, :, 2560:2976].rearrange("(kc p) n -> p kc n", p=128), (), [w_mla_b])
                        DMAC(w_uq[:], w_uq_d[l].rearrange("(kc p) n -> p kc n", p=128), (), [w_uq_b])
                        DMAC(w_ukv[:], w_ukv_d[l], (), [w_ukv_b])
                        sc_c = float(96 ** -0.5)
                        for tb in range(NB):
                            blk = slice(tb * TB, (tb + 1) * TB)
                            self.psg_banks = ALLB
                            make_h(tb, A1, B1, hbf, hbf_b, ses, l)
                            if l == 0 and tb == 0:
                                self.dump("h0", hbf[:], [hbf_b])
                            sstop(1)
                            for j in range(2):
                                ps, pb = psg()
                                for kc in range(8):
                                    MM(ps[:], w_mla[:, kc, j * 128:(j + 1) * 128], hbf[:, kc, :], kc == 0, kc == 7,
                                       [w_mla_b, hbf_b], [pb])
                                kvar = int(os.environ.get("K_VAR", 0))
                                if kvar in (0, 2):
                                    ACT(cqs[:, j, :], ps[:], AF.Square, [pb], [cqs_b])
                                if kvar in (0, 3):
                                    CP(cqf[:, j, :], ps[:], [pb], [cqf_b])
                            sstop(1.2)
                            ps, pb = psg()
                            for j in range(2):
                                MM(ps[:], ones_bf[:], cqs[:, j, :], j == 0, j == 1, [ones_b, cqs_b], [pb])
                            r, r_b = self.n_r.next()
                            self.rstd(r[:], ps[:], 1.0 / 256, [pb, cst_b], [r_b])
                            for j in range(2):
                                STT(cqn[:, j, :], cqf[:, j, :], latg[:, l, j:j + 1], r[:], ALU.mult, ALU.mult,
                                    [cqf_b, r_b, latg_b], [cqn_b])
                            sstop(1.5)
                            ps, pb = psg()
                            for kc in range(8):
                                MM(ps[:], w_mla[:, kc, 256:384], hbf[:, kc, :], kc == 0, kc == 7, [w_mla_b, hbf_b], [pb])
                            group_norm_fm(ps[:], pb, 128, ones_bf[:], 1.0 / 128, latg[:, l, 2:3], ckvn[:], [ckvn_b], [latg_b])
                            sstop(2)
                            ps, pb = psg()
                            for kc in range(8):
                                MM(ps[0:32, :], w_mla[:, kc, 384:416], hbf[:, kc, :], kc == 0, kc == 7, [w_mla_b, hbf_b], [pb])
                            group_norm_fm(ps[0:32, :], pb, 32, ones_bf[0:32, 0:32], 1.0 / 32, qkgCr[:, l, 1:2], xr[:], [xr_b], [qkgCr_b])
                            rope(xr[:], xr_b, blk, Kr[:, blk], [Kr_b])
                            sstop(3)
                            for j in range(4):
                                ps, pb = psg()
                                for kc in range(2):
                                    MM(ps[:], w_uq[:, kc, j * 128:(j + 1) * 128], cqn[:, kc, :], kc == 0, kc == 1,
                                       [w_uq_b, cqn_b], [pb])
                                group_norm_fm(ps[:], pb, 128, bones[:], 1.0 / 64, qkgCn[:, l, 0:1], qn[:, j, :], [qn_b], [qkgCn_b])
                            for h in range(8):
                                ps, pb = psg()
                                for kc in range(2):
                                    MM(ps[0:32, :], w_uq[:, kc, 512 + h * 32:512 + (h + 1) * 32], cqn[:, kc, :], kc == 0, kc == 1,
                                       [w_uq_b, cqn_b], [pb])
                                group_norm_fm(ps[0:32, :], pb, 32, ones_bf[0:32, 0:32], 1.0 / 32, qkgCr[:, l, 0:1], xr[:], [xr_b], [qkgCr_b])
                                rope(xr[:], xr_b, blk, qr[:, h, :], [qr_b])
                            for j in range(4):
                                ps, pb = psg()
                                MM(ps[:], w_ukv[:, j * 128:(j + 1) * 128], ckvn[:], True, True, [w_ukv_b, ckvn_b], [pb])
                                group_norm_fm(ps[:], pb, 128, bones[:], 1.0 / 64, qkgCn[:, l, 1:2], Kn[:, j, blk], [Kn_b], [qkgCn_b])
                            for tt in range(4):
                                ps, pb = psg()
                                MM(ps[:], ckvn[:, tt * 128:(tt + 1) * 128], w_ukv[:, 512:1024], True, True, [ckvn_b, w_ukv_b], [pb])
                                CP(Vc[:, tb * 4 + tt, :], ps[:], [pb], [Vc_b])
                            if l == 0 and tb == 0:
                                self.dump("qn0", qn[:], [qn_b])
                                self.dump("qr0", qr[:], [qr_b])
                                self.dump("Kr0", Kr[:, 0:TB], [Kr_b])
                                self.dump("Kn0", Kn[:, :, 0:TB], [Kn_b])
                                self.dump("Vc0", Vc[:, 0:4, :], [Vc_b])
                            sstop(4)
                            self.psg_banks = [6, 7]
                            nkt = 4 * (tb + 1)
                            steps = [(j, hh, kt) for j in range(4) for hh in range(2) for kt in range(nkt)]
                            sring = [0]

                            def emit_S(st, i):
                                j, hh, kt = st
                                h = 2 * j + hh
                                rows = slice(hh * 64, (hh + 1) * 64)
                                j0 = max(0, kt - 4 * tb)
                                cols = slice(j0 * 128, TB)
                                Sps, Spb = PS[i % 2], PB[i % 2]
                                MM(Sps[:, cols], Kn[rows, j, kt * 128:(kt + 1) * 128], qn[rows, j, cols], True, False,
                                   [Kn_b, qn_b], [Spb], inc=False)
                                MM(Sps[:, cols], Kr[:, kt * 128:(kt + 1) * 128], qr[:, h, cols], False, True,
                                   [Kr_b, qr_b], [Spb])

                            def emit_rest(st, i):
                                j, hh, kt = st
                                h = 2 * j + hh
                                rows = slice(hh * 64, (hh + 1) * 64)
                                j0 = max(0, kt - 4 * tb)
                                cols = slice(j0 * 128, TB)
                                Sps, Spb = PS[i % 2], PB[i % 2]
                                Ops, Opb = PS[2 + j % 2], PB[2 + j % 2]
                                Sms, Smb = PS[4 + j % 2], PB[4 + j % 2]
                                P, P_b = Pr.next()
                                ACT(P[:, cols], Sps[:, cols], AF.Exp, [Spb], [P_b], scale=sc_c)
                                if kt >= 4 * tb:
                                    MSET(P[64:128, j0 * 128:j0 * 128 + 64], 0.0, [P_b])
                                first = (kt == 0)
                                last = (kt == nkt - 1)
                                MM(Ops[rows, cols], Vc[:, kt, h * 64:(h + 1) * 64], P[:, cols], first, last, [Vc_b, P_b], [Opb],
                                   inc=True)
                                acc, acc_b = acc_r[h % 2]
                                if first:
                                    CP(acc[:, cols], P[:, cols], [P_b], [acc_b])
                                else:
                                    TT(acc[:, cols], acc[:, cols], P[:, cols], ALU.add, [acc_b, P_b], [acc_b])
                                if last:
                                    def fin(acc=acc, acc_b=acc_b, h=h, hh=hh, j=j, rows=rows, Ops=Ops, Opb=Opb, Sms=Sms, Smb=Smb):
                                        accb, accb_b = accb_r[h % 2]
                                        CP(accb[:], acc[:], [acc_b], [accb_b])
                                        MM(Sms[rows, :], ones_bf[:, 0:64], accb[:], True, True, [ones_b, accb_b], [Smb], inc=True)
                                        if hh == 1:
                                            ACT(rc[:], Sms[:], AF.Ln, [Smb], [rc_b])
                                            ACT(rc[:], rc[:], AF.Exp, [rc_b], [rc_b], scale=-1.0)
                                            TT(o_c[:, j, blk], Ops[:], rc[:], ALU.mult, [Opb, rc_b], [o_c_b])
                                    pending.append((i + 2, fin))

                            pending = []
                            emit_S(steps[0], 0)
                            for i, st in enumerate(steps):
                                if i + 1 < len(steps):
                                    emit_S(steps[i + 1], i + 1)
                                emit_rest(st, i)
                                while pending and pending[0][0] <= i:
                                    pending.pop(0)[1]()
                            while pending:
                                pending.pop(0)[1]()
                        self.dump("o_c", o_c[:], [o_c_b])
                        fw.barrier()
                    stop_at(1)

                    o_a, o_a_b = alloc("o_a", [128, 4, S], BF16, mes)
                    with ExitStack() as ses:
                        wa_r = [alloc(f"w_a{i}", [128, 8, 512], BF16, ses) for i in range(2)]
                        wa_i = [0]
                        Ka, Ka_b = alloc("Ka", [128, 4, S], BF16, ses)
                        Va, Va_b = alloc("Va", [128, 16, 512], BF16, ses)
                        Qa, Qa_b = alloc("Qa", [128, 4, TB], BF16, ses)
                        rc, rc_b = alloc("rca", [128, TB], F32, ses)
                        accA_r = [alloc(f"accA{i}", [128, TB], F32, ses) for i in range(2)]
                        accAb, accAb_b = alloc("accAb", [128, TB], BF16, ses)
                        o0, o0_b = alloc("o0", [128, TB], F32, ses)
                        o1, o1_b = alloc("o1", [128, TB], F32, ses)
                        dd, dd_b = alloc("dd", [128, TB], F32, ses)
                        sc_a = 0.125
                        for tb in range(NB):
                            blk = slice(tb * TB, (tb + 1) * TB)
                            self.psg_banks = ALLB
                            make_h(tb, A1, B1, hbf, hbf_b, ses, l)
                            wqkv = []
                            for q in range(3):
                                w_a, w_a_b = wa_r[wa_i[0] % 2]; wa_i[0] += 1
                                DMAC(w_a[:], w_in_d[l, :, q * 512:(q + 1) * 512].rearrange("(kc p) n -> p kc n", p=128), (), [w_a_b])
                                if q == 0:
                                    for hd in range(4):
                                        ps, pb = psg()
                                        for kc in range(8):
                                            MM(ps[:], w_a[:, kc, hd * 128:(hd + 1) * 128], hbf[:, kc, :], kc == 0, kc == 7, [w_a_b, hbf_b], [pb])
                                        group_norm_fm(ps[:], pb, 128, bones[:], 1.0 / 64, qkgA[:, l, 0:1], Qa[:, hd, :], [Qa_b], [qkgA_b])
                                elif q == 1:
                                    for hd in range(4):
                                        ps, pb = psg()
                                        for kc in range(8):
                                            MM(ps[:], w_a[:, kc, hd * 128:(hd + 1) * 128], hbf[:, kc, :], kc == 0, kc == 7, [w_a_b, hbf_b], [pb])
                                        group_norm_fm(ps[:], pb, 128, bones[:], 1.0 / 64, qkgA[:, l, 1:2], Ka[:, hd, blk], [Ka_b], [qkgA_b])
                                else:
                                    for tt in range(4):
                                        ps, pb = psg()
                                        for kc in range(8):
                                            MM(ps[:], hbf[:, kc, tt * 128:(tt + 1) * 128], w_a[:, kc, :], kc == 0, kc == 7, [hbf_b, w_a_b], [pb])
                                        CP(Va[:, tb * 4 + tt, :], ps[:], [pb], [Va_b])
                            if l == 0 and tb == 0:
                                self.dump("Qa0", Qa[:], [Qa_b])
                                self.dump("Va0", Va[:, 0:4, :], [Va_b])
                            self.psg_banks = [7]
                            SB3 = [0, 1, 6]
                            nkt = 4 * (tb + 1)
                            steps = [(hd, m, kt) for hd in range(4) for m in range(2) for kt in range(nkt)]

                            def emit_S(st, i):
                                hd, m, kt = st
                                rows = slice(m * 64, (m + 1) * 64)
                                j0 = max(0, kt - 4 * tb)
                                cols = slice(j0 * 128, TB)
                                Sps, Spb = PS[SB3[i % 3]], PB[SB3[i % 3]]
                                MM(Sps[:, cols], Ka[rows, hd, kt * 128:(kt + 1) * 128], Qa[rows, hd, cols], True, True,
                                   [Ka_b, Qa_b], [Spb])

                            def emit_rest(st, i):
                                hd, m, kt = st
                                sidx = hd * 2 + m
                                j0 = max(0, kt - 4 * tb)
                                cols = slice(j0 * 128, TB)
                                Sps, Spb = PS[SB3[i % 3]], PB[SB3[i % 3]]
                                Ops, Opb = PS[2 + sidx % 2], PB[2 + sidx % 2]
                                Sms, Smb = PS[4 + sidx % 2], PB[4 + sidx % 2]
                                P, P_b = Pr.next()
                                jq_far0 = max(j0, kt - 4 * tb + 2)
                                ACT(P[:, cols], Sps[:, cols], AF.Exp, [Spb, cfar_b], [P_b], bias=cfar[:, hd:hd + 1], scale=sc_a)
                                for jq in range(j0, min(4, jq_far0)):
                                    delta = kt - (4 * tb + jq)
                                    bi = (0 if delta == 0 else 1) * 4 + hd
                                    qc = slice(jq * 128, (jq + 1) * 128)
                                    TT(P[:, qc], P[:, qc], Eb[:, bi, :], ALU.mult, [P_b, Eb_b], [P_b], eng=self.pool)
                                first = (kt == 0)
                                last = (kt == nkt - 1)
                                MM(Ops[:, cols], Va[:, kt, hd * 128:(hd + 1) * 128], P[:, cols], first, last, [Va_b, P_b], [Opb],
                                   inc=True)
                                acc, acc_b = accA_r[sidx % 2]
                                if first:
                                    CP(acc[:, cols], P[:, cols], [P_b], [acc_b])
                                else:
                                    TT(acc[:, cols], acc[:, cols], P[:, cols], ALU.add, [acc_b, P_b], [acc_b])
                                if last:
                                    def fin(acc=acc, acc_b=acc_b, hd=hd, m=m, Ops=Ops, Opb=Opb, Sms=Sms, Smb=Smb):
                                        CP(accAb[:], acc[:], [acc_b], [accAb_b])
                                        MM(Sms[:], ones_bf[:], accAb[:], True, True, [ones_b, accAb_b], [Smb], inc=True)
                                        ACT(rc[:], Sms[:], AF.Ln, [Smb], [rc_b])
                                        ACT(rc[:], rc[:], AF.Exp, [rc_b], [rc_b], scale=-1.0)
                                        if m == 0:
                                            TT(o0[:], Ops[:], rc[:], ALU.mult, [Opb, rc_b], [o0_b])
                                        else:
                                            TT(o1[:], Ops[:], rc[:], ALU.mult, [Opb, rc_b], [o1_b])
                                            STT(dd[:], o1[:], neglam[:, l:l + 1], o0[:], ALU.mult, ALU.add, [o1_b, o0_b, neglam_b], [dd_b])
                                            sq, sq_b = self.n_sq.next()
                                            ACT(sq[:], dd[:], AF.Square, [dd_b], [sq_b])
                                            ps, pb = psg()
                                            MM(ps[:], ones_bf[:], sq[:], True, True, [ones_b, sq_b], [pb])
                                            r, r_b = self.n_r.next()
                                            self.rstd(r[:], ps[:], 1.0 / 128, [pb, cst_b], [r_b])
                                            STT(o_a[:, hd, blk], dd[:], gout[:, l:l + 1], r[:], ALU.mult, ALU.mult,
                                                [dd_b, r_b, gout_b], [o_a_b])
                                    pending.append((i + 2, fin))

                            pending = []
                            emit_S(steps[0], 0)
                            emit_S(steps[1], 1)
                            for i, st in enumerate(steps):
                                if i + 2 < len(steps):
                                    emit_S(steps[i + 2], i + 2)
                                emit_rest(st, i)
                                while pending and pending[0][0] <= i:
                                    pending.pop(0)[1]()
                            while pending:
                                pending.pop(0)[1]()
                        self.dump("o_a", o_a[:], [o_a_b])
                        fw.barrier()
                    stop_at(2)

                    with ExitStack() as ses:
                        w8_r = [alloc(f"w8_{i}", [128, 8, 512], BF16, ses) for i in range(2)]
                        wb_r = [alloc(f"wb{i}", [128, 4, 512], BF16, ses) for i in range(2)]
                        rings = {"w8": 0, "b": 0}
                        sgw_f, sgw_fb = alloc("sgw_f", [128, 4, 128], F32, ses)
                        sgw, sgw_b = alloc("sgw", [128, 4, 128], BF16, ses)
                        sgb, sgb_b = alloc("sgb", [128, 4, 128], F32, ses)
                        vg, vg_b = alloc("vg", [128, 512], F32, ses)
                        uT, uT_b = alloc("uT", [128, 4, TB], BF16, ses)
                        o_b, o_b_b = alloc("o_b", [128, 4, TB], BF16, ses)
                        vf, vf_b = alloc("vf", [128, 512], F32, ses)
                        vjunk, vjunk_b = alloc("vjunk", [128, 512], BF16, ses)
                        vss, vss_b = alloc("vss", [128, 2], F32, ses)
                        vn, vn_b = alloc("vn", [128, 512], BF16, ses)
                        vt, vt_b = alloc("vt", [128, 128], F32, ses)
                        gsb, gsb_b = alloc("gsb", [128, TB], F32, ses)
                        mt, mt_b = alloc("mt", [128, TB], F32, ses)
                        macc, macc_b = alloc("macc", [128, 4, TB], F32, ses)
                        merged, merged_b = alloc("merged", [128, 8, TB], BF16, ses)
                        DMA(sgw_f[:], sgu_wT_d[l], (), [sgw_fb])
                        DMA(sgb[:], sgu_bb_d[l], (), [sgb_b])
                        DMA(vg[:], sgu_vg_d[l], (), [vg_b])
                        MSET(sgw_f[64:128, :, 0:64], 0.0, [sgw_fb])
                        CP(sgw[:], sgw_f[:], [sgw_fb], [sgw_b])
                        self.psg_banks = ALLB
                        for tb in range(NB):
                            blk = slice(tb * TB, (tb + 1) * TB)
                            make_h(tb, A1, B1, hbf, hbf_b, ses, l)
                            wu, wu_b = w8_r[rings["w8"] % 2]; rings["w8"] += 1
                            DMAC(wu[:], w_in_d[l, :, 1536:2048].rearrange("(kc p) n -> p kc n", p=128), (), [wu_b])
                            wv, wv_b = w8_r[rings["w8"] % 2]; rings["w8"] += 1
                            DMAC(wv[:], w_in_d[l, :, 2048:2560].rearrange("(kc p) n -> p kc n", p=128), (), [wv_b])
                            for cu in range(4):
                                ps, pb = psg()
                                for kc in range(8):
                                    MM(ps[:], wu[:, kc, cu * 128:(cu + 1) * 128], hbf[:, kc, :], kc == 0, kc == 7, [wu_b, hbf_b], [pb])
                                ACT(uT[:, cu, :], ps[:], AF.Gelu_apprx_tanh, [pb], [uT_b])
                            for tt in range(4):
                                ps, pb = psg()
                                for kc in range(8):
                                    MM(ps[:], hbf[:, kc, tt * 128:(tt + 1) * 128], wv[:, kc, :], kc == 0, kc == 7, [hbf_b, wv_b], [pb])
                                ACT(vf[:], ps[:], AF.Gelu_apprx_tanh, [pb], [vf_b])
                                fw.op(self.act, lambda: nc.scalar.activation(out=vjunk[:], in_=vf[:], func=AF.Square,
                                                                             accum_out=vss[:, 0:1]),
                                      [vf_b], [vjunk_b, vss_b])
                                ACT(vss[:, 1:2], vss[:, 0:1], AF.Ln, [vss_b, cst_b], [vss_b], bias=cst[:, 0:1], scale=1.0 / 512)
                                ACT(vss[:, 1:2], vss[:, 1:2], AF.Exp, [vss_b], [vss_b], scale=-0.5)
                                STT(vn[:], vf[:], vss[:, 1:2], vg[:], ALU.mult, ALU.mult, [vf_b, vss_b, vg_b], [vn_b])
                                for g in range(4):
                                    ps2, pb2 = psg()
                                    MM(ps2[:, 0:128], vn[:, g * 128:(g + 1) * 128], sgw[:, g, :], True, True, [vn_b, sgw_b], [pb2])
                                    TT(vt[:], ps2[:, 0:128], sgb[:, g, :], ALU.add, [pb2, sgb_b], [vt_b])
                                    TT(o_b[:, g, tt * 128:(tt + 1) * 128], vt[:], uT[:, g, tt * 128:(tt + 1) * 128], ALU.mult,
                                       [vt_b, uT_b], [o_b_b])
                            if l == 0 and tb == 0:
                                self.dump("o_b0", o_b[:], [o_b_b])
                            for dcg in range(2):
                                for n in range(3):
                                    wg, wg_b = w8_r[rings["w8"] % 2]; rings["w8"] += 1
                                    c0 = 2976 + n * 1024 + dcg * 512
                                    DMAC(wg[:], w_in_d[l, :, c0:c0 + 512].rearrange("(kc p) n -> p kc n", p=128), (), [wg_b])
                                    wb, wb_b = wb_r[rings["b"] % 2]; rings["b"] += 1
                                    DMAC(wb[:], w_br_d[l, n, :, dcg * 512:(dcg + 1) * 512].rearrange("(kc p) n -> p kc n", p=128), (), [wb_b])
                                    src, src_b = [(o_a, o_a_b), (o_b, o_b_b), (o_c, o_c_b)][n]
                                    for dci in range(4):
                                        psa, pba = psg()
                                        for kc in range(8):
                                            MM(psa[:], wg[:, kc, dci * 128:(dci + 1) * 128], hbf[:, kc, :], kc == 0, kc == 7, [wg_b, hbf_b], [pba])
                                        ACT(gsb[:], psa[:], AF.Sigmoid, [pba], [gsb_b])
                                        psb, pbb = psg()
                                        for kc in range(4):
                                            rhs = src[:, kc, :] if n == 1 else src[:, kc, blk]
                                            MM(psb[:], wb[:, kc, dci * 128:(dci + 1) * 128], rhs, kc == 0, kc == 3, [wb_b, src_b], [pbb])
                                        if n == 0:
                                            TT(macc[:, dci, :], psb[:], gsb[:], ALU.mult, [pbb, gsb_b], [macc_b])
                                        else:
                                            TT(mt[:], psb[:], gsb[:], ALU.mult, [pbb, gsb_b], [mt_b])
                                            if n == 1:
                                                TT(macc[:, dci, :], macc[:, dci, :], mt[:], ALU.add, [macc_b, mt_b], [macc_b])
                                            else:
                                                TT(merged[:, dcg * 4 + dci, :], macc[:, dci, :], mt[:], ALU.add, [macc_b, mt_b], [merged_b])
                            if l == 0 and tb == 0:
                                self.dump("merged0", merged[:], [merged_b])
                            for dcg in range(2):
                                wo, wo_b = w8_r[rings["w8"] % 2]; rings["w8"] += 1
                                DMAC(wo[:], w_out_d[l, :, dcg * 512:(dcg + 1) * 512].rearrange("(kc p) n -> p kc n", p=128), (), [wo_b])
                                for dci in range(4):
                                    dc = dcg * 4 + dci
                                    ps, pb = psg()
                                    for kc in range(8):
                                        MM(ps[:], wo[:, kc, dci * 128:(dci + 1) * 128], merged[:, kc, :], kc == 0, kc == 7, [wo_b, merged_b], [pb])
                                    STT(xT[:, dc, blk], ps[:], MOD[:, l, 16 + dc:16 + dc + 1], xT[:, dc, blk], ALU.mult, ALU.add,
                                        [pb, MOD_b, xTB[tb]], [xTB[tb]])
                        fw.barrier()
                    if l == 0:
                        self.dump("x_mid", xT[:], xTB)
                    stop_at(3)

                    mes.close()
                    with ExitStack() as ses:
                        h2, h2_b = alloc("h2", [128, 8, S], BF16, ses)
                        rs = ExitStack()
                        h2f, h2f_b = alloc("h2f", [128, 8, TB], F32, rs)
                        combT, combT_b = alloc("combT", [32, S], F32, rs)
                        w_r, w_r_b = alloc("w_r", [128, 8, 36], F32, rs)
                        DMA(w_r[:], w_r_d[l].rearrange("(kc p) n -> p kc n", p=128), (), [w_r_b])
                        NT = 16
                        lgA, lg_b = alloc("lgA", [128, NT, 36], F32, rs)
                        ohg, ohg_b = alloc("ohg", [128, NT, 4], F32, rs)
                        ex4, ex4_b = alloc("ex4", [128, NT, 4], F32, rs)
                        v1, v1_b = alloc("v1", [128, 8, NT], F32, rs)
                        el3, el3_b = alloc("el3", [128, NT, 32], F32, rs)
                        els, els_b = alloc("els", [128, NT, 8], F32, rs)
                        els2, els2_b = alloc("els2", [128, NT, 8], F32, rs)
                        oh1, oh1_b = alloc("oh1", [128, NT, 8], F32, rs)
                        oh2, oh2_b = alloc("oh2", [128, NT, 8], F32, rs)
                        comb, comb_b = alloc("comb", [128, NT, 32], F32, rs)
                        h2B = [Buf(f"h2_{b}") for b in range(NB)]
                        self.psg_banks = ALLB
                        for tb in range(NB):
                            blk = slice(tb * TB, (tb + 1) * TB)
                            make_h(tb, A2, B2, h2[:, :, blk], h2B[tb], ses, l, hf=h2f, hf_b=h2f_b)
                            for tt in range(4):
                                ps, pb = psg()
                                for kc in range(8):
                                    MM(ps[:, 0:36], h2f[:, kc, tt * 128:(tt + 1) * 128], w_r[:, kc, :], kc == 0, kc == 7, [h2f_b, w_r_b], [pb])
                                TT(lgA[:, tb * 4 + tt, :], ps[:, 0:36], b_r[:, l, :], ALU.add, [pb, b_r_b], [lg_b])
                        def red(out, in_, op, rd, wr):
                            fw.op(self.dve, lambda: nc.vector.tensor_reduce(out=out, in_=in_, axis=AX.X, op=op), rd, wr)
                        gl = lgA[:, :, 0:4]
                        el4 = lgA[:, :, 4:36].rearrange("p t (g e) -> p t g e", g=4)
                        gmax, gsum, g_w, m1, m2, e2, w1, w2 = [v1[:, i, :] for i in range(8)]
                        bc4 = lambda a: a.unsqueeze(2).broadcast_to([128, NT, 4])
                        bc8 = lambda a: a.unsqueeze(2).broadcast_to([128, NT, 8])
                        red(gmax, gl, ALU.max, [lg_b], [v1_b])
                        TT(ohg[:], gl, bc4(gmax), ALU.is_equal, [lg_b, v1_b], [ohg_b])
                        TT(ex4[:], gl, bc4(gmax), ALU.subtract, [lg_b, v1_b], [ex4_b])
                        ACT(ex4[:], ex4[:], AF.Exp, [ex4_b], [ex4_b])
                        red(gsum, ex4[:], ALU.add, [ex4_b], [v1_b])
                        RECIP(g_w, gsum, [v1_b], [v1_b])
                        TT(el3[:].rearrange("p t (g e) -> p t g e", g=4), el4,
                           ohg[:].unsqueeze(3).broadcast_to([128, NT, 4, 8]), ALU.mult, [lg_b, ohg_b], [el3_b])
                        red(els[:], el3[:].rearrange("p t (g e) -> p t e g", g=4), ALU.add, [el3_b], [els_b])
                        red(m1, els[:], ALU.max, [els_b], [v1_b])
                        TT(oh1[:], els[:], bc8(m1), ALU.is_equal, [els_b, v1_b], [oh1_b])
                        STT(els2[:], oh1[:], -1.0e30, els[:], ALU.mult, ALU.add, [oh1_b, els_b], [els2_b])
                        red(m2, els2[:], ALU.max, [els2_b], [v1_b])
                        TT(oh2[:], els2[:], bc8(m2), ALU.is_equal, [els2_b, v1_b], [oh2_b])
                        TT(e2, m2, m1, ALU.subtract, [v1_b], [v1_b])
                        ACT(e2, e2, AF.Exp, [v1_b], [v1_b])
                        TS(w1, e2, 1.0, None, ALU.add, None, [v1_b], [v1_b])
                        RECIP(w1, w1, [v1_b], [v1_b])
                        TT(w2, e2, w1, ALU.mult, [v1_b], [v1_b])
                        TT(w1, w1, g_w, ALU.mult, [v1_b], [v1_b])
                        TT(w2, w2, g_w, ALU.mult, [v1_b], [v1_b])
                        TT(oh1[:], oh1[:], bc8(w1), ALU.mult, [oh1_b, v1_b], [oh1_b])
                        TT(oh2[:], oh2[:], bc8(w2), ALU.mult, [oh2_b, v1_b], [oh2_b])
                        TT(oh1[:], oh1[:], oh2[:], ALU.add, [oh1_b, oh2_b], [oh1_b])
                        TT(comb[:].rearrange("p t (g e) -> p t g e", g=4), ohg[:].unsqueeze(3).broadcast_to([128, NT, 4, 8]),
                           oh1[:].unsqueeze(2).broadcast_to([128, NT, 4, 8]), ALU.mult, [ohg_b, oh1_b], [comb_b])
                        for tb in range(NB):
                            ps2, pb2 = psg()
                            for tt in range(4):
                                fw.op(self.pe, lambda tt=tt: nc.tensor.transpose(ps2[0:32, tt * 128:(tt + 1) * 128], comb[:, tb * 4 + tt, :], ident[:]),
                                      [comb_b, ident_b], [pb2], inc=(tt == 3))
                            CP(combT[:, tb * TB:(tb + 1) * TB], ps2[0:32, :], [pb2], [combT_b])
                        self.dump("combT", combT[:], [combT_b])
                        if l == 0:
                            self.dump("h2", h2[:], h2B)
                        DMA(comb_scr, combT[:], [combT_b], [comb_scr_b])
                        fw.barrier()
                        rs.close()
                        stop_at(4)
                        self.psg_banks = [4, 5, 6, 7]
                        EE = 2
                        wgu_r = [alloc(f"wgu{i}", [128, 2, 8, 256], BF16, ses) for i in range(3)]
                        wd_r = [alloc(f"wd{i}", [128, 2, D], BF16, ses) for i in range(4)]
                        actT, actT_b0 = alloc("actT", [128, EE, 2, S], BF16, ses)
                        actB = [[Buf(f"act{e}_{b}") for b in range(NB)] for e in range(EE)]
                        sil_r = [alloc(f"sil{i}", [128, TB], BF16, ses) for i in range(2)]
                        ut_r = [alloc(f"ut{i}", [128, TB], BF16, ses) for i in range(2)]
                        cb_r = [alloc(f"cb{i}", [128, TB], F32, ses) for i in range(3)]
                        cnt = {"gu": 0, "d": 0, "s": 0, "cb": 0, "ps": 0}
                        for e0 in range(0, 32, EE):
                            wds = []
                            for ei in range(EE):
                                e = e0 + ei
                                wgu, wgu_b = wgu_r[cnt["gu"] % 3]; cnt["gu"] += 1
                                DMAC(wgu[:, 0, :, :], weg_d[l, e].rearrange("(kc p) n -> p kc n", p=128), (), [wgu_b])
                                DMAC(wgu[:, 1, :, :], weu_d[l, e].rearrange("(kc p) n -> p kc n", p=128), (), [wgu_b])
                                wd, wd_b = wd_r[cnt["d"] % 4]; cnt["d"] += 1
                                DMAC(wd[:], wed_d[l, e].rearrange("(fc p) n -> p fc n", p=128), (), [wd_b])
                                wds.append((wd, wd_b))
                                for tb in range(NB):
                                    blk = slice(tb * TB, (tb + 1) * TB)
                                    cb, cb_b = cb_r[cnt["cb"] % 3]; cnt["cb"] += 1
                                    DMA(cb[:], comb_scr[e:e + 1, blk].partition_broadcast(128), [comb_scr_b], [cb_b])
                                    for fc in range(2):
                                        k = cnt["ps"]; cnt["ps"] += 1
                                        aps, apb = PS[k % 2], PB[k % 2]
                                        ups, upb = PS[2 + k % 2], PB[2 + k % 2]
                                        for kc in range(8):
                                            MM(aps[:], wgu[:, 0, kc, fc * 128:(fc + 1) * 128], h2[:, kc, blk], kc == 0, kc == 7, [wgu_b, h2B[tb]], [apb])
                                        for kc in range(8):
                                            MM(ups[:], wgu[:, 1, kc, fc * 128:(fc + 1) * 128], h2[:, kc, blk], kc == 0, kc == 7, [wgu_b, h2B[tb]], [upb])
                                        sil, sil_b = sil_r[k % 2]
                                        ut, ut_b = ut_r[k % 2]
                                        ACT(sil[:], aps[:], AF.Silu, [apb], [sil_b])
                                        TT(ut[:], ups[:], cb[:], ALU.mult, [upb, cb_b], [ut_b])
                                        TT(actT[:, ei, fc, blk], sil[:], ut[:], ALU.mult, [sil_b, ut_b], [actB[ei][tb]], eng=self.pool)
                            for tb in range(NB):
                                blk = slice(tb * TB, (tb + 1) * TB)
                                for dc in range(8):
                                    ps, pb = psg()
                                    n_mm = EE * 2
                                    i_mm = 0
                                    for ei in range(EE):
                                        wd, wd_b = wds[ei]
                                        for fc in range(2):
                                            MM(ps[:], wd[:, fc, dc * 128:(dc + 1) * 128], actT[:, ei, fc, blk], i_mm == 0, i_mm == n_mm - 1,
                                               [wd_b, actB[ei][tb]], [pb])
                                            i_mm += 1
                                    STT(xT[:, dc, blk], ps[:], MOD[:, l, 40 + dc:40 + dc + 1], xT[:, dc, blk], ALU.mult, ALU.add,
                                        [pb, MOD_b, xTB[tb]], [xTB[tb]])
                        fw.barrier()

            emit_output()
            self.ninstr = fw.ninstr
        return nc


def _t5_bucket(rel):
    nb = 16
    max_exact = 8
    n = np.abs(rel)
    large = max_exact + (np.log(np.maximum(n, 1).astype(np.float32) / max_exact)
                         / math.log(128 / max_exact) * (nb - max_exact)).astype(np.int32)
    large = np.minimum(large, nb - 1)
    return np.where(rel > 0, nb, 0) + np.where(n < max_exact, n, large)


def _bucket_table():
    rel = np.arange(-300, 300)
    nb = 16
    max_exact = 8
    n = np.abs(rel)
    ratio = (np.maximum(n, 1).astype(np.float32) / np.float32(max_exact)).astype(np.float32)
    large = max_exact + (np.log(ratio).astype(np.float32) / np.float32(math.log(128 / max_exact))
                         * np.float32(nb - max_exact)).astype(np.int32)
    large = np.minimum(large, nb - 1)
    return np.where(rel > 0, nb, 0) + np.where(n < max_exact, n, large)


def prep_shared(inp, depth):
    f = np.float32
    g = {}
    g["w_ada"] = np.ascontiguousarray(inp["w_ada"], f)
    g["b_adaT"] = np.ascontiguousarray(inp["b_ada"].reshape(DEPTH, 48, 128).transpose(2, 0, 1), f)
    g["norm_gT"] = np.ascontiguousarray(inp["norm_g"].reshape(DEPTH, 2, 8, 128).transpose(3, 0, 1, 2), f)
    g["w_in"] = np.ascontiguousarray(inp["w_in"], f)
    qk = inp["diff_qk_g"]
    g["qkgA"] = np.ascontiguousarray(np.tile(qk.transpose(2, 0, 1), (2, 1, 1)), f)
    g["dlam"] = np.ascontiguousarray(np.broadcast_to(inp["diff_lambda"].reshape(1, DEPTH, 256), (128, DEPTH, 256)), f)
    g["goutA"] = np.ascontiguousarray(inp["diff_out_g"].T, f)
    bt = _bucket_table()
    ki = np.arange(128)[:, None]
    qi = np.arange(128)[None, :]
    tab = inp["rel_bias"]
    tiles = np.zeros((128, 8, 128), f)
    for di, delta in enumerate((0, -1)):
        rel = ki + 128 * delta - qi
        bidx = bt[rel + 300]
        for h in range(4):
            t = tab[bidx, h]
            if delta == 0:
                allowed = (ki // 64) <= (qi // 64)
                t = np.where(allowed, t, f(-30000.0))
            tiles[:, di * 4 + h, :] = t
    g["biasA"] = tiles
    g["cfar"] = np.ascontiguousarray(np.broadcast_to(tab[15][None, :], (128, 4)), f)
    g["sgu_vg"] = np.ascontiguousarray(np.broadcast_to(inp["sgu_v_g"][:, None, :], (DEPTH, 128, 512)), f)
    g["sgu_wT"] = np.ascontiguousarray(inp["sgu_w"].transpose(0, 3, 1, 2), f)
    g["sgu_bb"] = np.ascontiguousarray(np.broadcast_to(inp["sgu_b"][:, None, :, :], (DEPTH, 128, 4, 128)), f)
    lg = inp["mla_lat_g"]
    g["latg"] = np.ascontiguousarray(lg.reshape(DEPTH, 3, 128).transpose(2, 0, 1), f)
    wuq = inp["mla_w_uq"].reshape(DEPTH, 256, 8, 96)
    g["w_uq"] = np.ascontiguousarray(np.concatenate([wuq[..., :64].reshape(DEPTH, 256, 512),
                                                     wuq[..., 64:].reshape(DEPTH, 256, 256)], axis=-1), f)
    wukv = inp["mla_w_ukv"].reshape(DEPTH, 128, 8, 128)
    g["w_ukv"] = np.ascontiguousarray(np.concatenate([wukv[..., :64].reshape(DEPTH, 128, 512),
                                                      wukv[..., 64:].reshape(DEPTH, 128, 512)], axis=-1), f)
    qkc = inp["mla_qk_g"]
    g["qkgCn"] = np.ascontiguousarray(np.tile(qkc[:, :, :64].transpose(2, 0, 1), (2, 1, 1)), f)
    g["qkgCr"] = np.ascontiguousarray(qkc[:, :, 64:].transpose(2, 0, 1), f)
    g["w_branch"] = np.ascontiguousarray(inp["w_branch"], f)
    g["w_out"] = np.ascontiguousarray(inp["w_out"], f)
    g["w_r"] = np.ascontiguousarray(np.concatenate([inp["router_g_w"], inp["router_e_w"]], axis=-1), f)
    br = np.concatenate([inp["router_g_b"], inp["router_e_b"]], axis=-1)
    g["b_r"] = np.ascontiguousarray(np.broadcast_to(br[None], (128, DEPTH, 36)), f)
    g["w_e_gate"] = np.ascontiguousarray(inp["w_e_gate"].reshape(DEPTH, 32, D, 256), f)
    g["w_e_up"] = np.ascontiguousarray(inp["w_e_up"].reshape(DEPTH, 32, D, 256), f)
    g["w_e_down"] = np.ascontiguousarray(inp["w_e_down"].reshape(DEPTH, 32, 256, D), f)
    inv_freq = (10000.0 ** (-np.arange(0, 32, 2, dtype=np.float32) / np.float32(32))).astype(f)
    g["invf"] = np.ascontiguousarray(np.concatenate([inv_freq, inv_freq])[:, None], f)
    R = np.zeros((32, 32), f)
    for i in range(16):
        R[i + 16, i] = -1.0
        R[i, i + 16] = 1.0
    g["R32T"] = R
    g["ident"] = np.eye(128, dtype=f)
    return g


def prep_core(inp, b):
    f = np.float32
    m = {}
    m["xT"] = np.ascontiguousarray(np.asarray(inp["x"][b], f).T)
    m["cT"] = np.ascontiguousarray(np.asarray(inp["c"][b], f).reshape(8, 128).T)
    m["pos32"] = np.ascontiguousarray(np.broadcast_to(np.asarray(inp["positions"][b], np.int32)[None, :], (32, S)))
    return m


_CACHE = {}


def kernel(**inputs):
    inp = {k: np.asarray(v) for k, v in inputs.items()}
    depth = int(os.environ.get("K_DEPTH", DEPTH))
    if depth not in _CACHE:
        kb = K(depth)
        _CACHE[depth] = kb.build()
    nc = _CACHE[depth]
    shared = prep_shared(inp, depth)
    in_maps = []
    for b in range(8):
        m = dict(shared)
        m.update(prep_core(inp, b))
        in_maps.append(m)
    res = run_bass_kernel_spmd(nc, in_maps, core_ids=list(range(8)))
    out = np.stack([np.asarray(r["outT"], np.float32).T for r in res.results], axis=0)
    return np.ascontiguousarray(out)
```

```python
import math
import os
from contextlib import ExitStack

import numpy as np
import concourse.bass as bass
import concourse.mybir as mybir
from concourse.bass_utils import run_bass_kernel_spmd

F32 = mybir.dt.float32
BF16 = mybir.dt.bfloat16
I32 = mybir.dt.int32
AF = mybir.ActivationFunctionType
ALU = mybir.AluOpType
AX = mybir.AxisListType

DEPTH = 4
S = 2048
D = 1024
NB = 4
TB = 512
EPS = 1e-6
IN_W = 6048
LAMBDA_INIT = [0.8 - 0.6 * math.exp(-0.3 * l) for l in range(DEPTH)]


class Eng:
    def __init__(self, name, handle, sem, step, issuer=None):
        self.name = name
        self.h = handle
        self.sem = sem
        self.step = step
        self.count = 0
        self.issuer = issuer or self
        self.waited = {}


class Buf:
    __slots__ = ("name", "w", "r", "excl")

    def __init__(self, name="", excl=False):
        self.name = name
        self.w = None
        self.r = []
        self.excl = excl


class FW:
    def __init__(self, nc, es):
        self.nc = nc
        self.es = es
        self.engs = {}
        self.ninstr = 0
        self.disabled = False

    def add_engine(self, name, handle, step=1, issuer=None):
        sem = self.es.enter_context(self.nc.semaphore("s_" + name))
        e = Eng(name, handle, sem, step, issuer)
        self.engs[name] = e
        return e

    def _wait(self, eng, dep):
        e2, cnt = dep
        iss = eng.issuer
        if iss.waited.get(e2.name, 0) >= cnt:
            return
        iss.h.wait_ge(e2.sem, cnt * e2.step)
        iss.waited[e2.name] = cnt
        self.ninstr += 1

    def op(self, eng, fn, reads=(), writes=(), inc=True):
        if self.disabled:
            return None
        iss = eng.issuer
        for b in reads:
            if b.excl:
                for r in b.r:
                    if r[0].issuer is not iss:
                        self._wait(eng, r)
            if b.w is not None:
                if b.w[0] is iss and iss.name == "pe":
                    continue
                self._wait(eng, b.w)
        for b in writes:
            if b.w is not None and (b.w[0].issuer is not iss or b.w[0].step == 16):
                self._wait(eng, b.w)
            for r in b.r:
                if r[0] is iss and iss.step == 1 and eng.step == 1:
                    continue
                self._wait(eng, r)
        ins = fn()
        self.ninstr += 1
        if inc:
            eng.count += 1
            ins.then_inc(eng.sem, eng.step)
            tag = (eng, eng.count)
        else:
            tag = (eng, eng.count + 1)
        for b in reads:
            b.r.append(tag)
            if len(b.r) > 48:
                best = {}
                for (e, c) in b.r:
                    if e.name not in best or best[e.name][1] < c:
                        best[e.name] = (e, c)
                b.r = list(best.values())
        for b in writes:
            b.w = tag
            b.r = []
        return ins

    def barrier(self):
        if self.disabled:
            return
        issuers = {}
        for e in self.engs.values():
            issuers[e.issuer.name] = e.issuer
        for iss in issuers.values():
            for e2 in self.engs.values():
                if e2 is iss or e2.count == 0:
                    continue
                if iss.waited.get(e2.name, 0) >= e2.count:
                    continue
                iss.h.wait_ge(e2.sem, e2.count * e2.step)
                iss.waited[e2.name] = e2.count
                self.ninstr += 1


class Ring:
    def __init__(self, K, name, shape, dt, n, psum=False):
        self.items = []
        for i in range(n):
            self.items.append(K.alloc(f"{name}{i}", shape, dt))
        self.i = 0

    def next(self):
        it = self.items[self.i % len(self.items)]
        self.i += 1
        return it


class StopBuild(Exception):
    pass


class K:
    def __init__(self, depth, dbg=None):
        self.stop = int(os.environ.get("K_STOP", 99))
        self.depth = depth
        self.dbg = dbg or {}
        self.nc = bass.Bass("TRN2", target_bir_lowering=False)
        self.dram = {}
        self.dbg_out = {}

    def din(self, name, shape, dt=F32):
        self.dram[name] = self.nc.dram_tensor(name, list(shape), dt, kind="ExternalInput").ap()
        return self.dram[name]

    def alloc(self, name, shape, dt, es=None):
        es = es or self.es
        self.uid = getattr(self, "uid", 0) + 1
        t = es.enter_context(self.nc.sbuf_tensor(f"sb{self.uid}_{name}", list(shape), dt))
        return t, Buf(name)

    def MM(self, out, lhsT, rhs, start, stop, rd, wr, inc=None):
        nc = self.nc
        if inc is None:
            inc = stop
        return self.fw.op(self.pe, lambda: nc.tensor.matmul(out, lhsT=lhsT, rhs=rhs, start=start, stop=stop),
                          rd, wr, inc)

    def ACT(self, out, in_, func, rd, wr, bias=None, scale=None):
        nc = self.nc
        kw = {}
        if bias is not None:
            kw["bias"] = bias
        if scale is not None:
            kw["scale"] = scale
        return self.fw.op(self.act, lambda: nc.scalar.activation(out=out, in_=in_, func=func, **kw), rd, wr)

    def TT(self, out, in0, in1, op, rd, wr, eng=None):
        nc = self.nc
        eng = eng or self.dve
        return self.fw.op(eng, lambda: eng.h.tensor_tensor(out=out, in0=in0, in1=in1, op=op), rd, wr)

    def STT(self, out, in0, scalar, in1, op0, op1, rd, wr):
        nc = self.nc
        return self.fw.op(self.dve, lambda: nc.vector.scalar_tensor_tensor(out=out, in0=in0, scalar=scalar, in1=in1,
                                                                           op0=op0, op1=op1), rd, wr)

    def TS(self, out, in0, s1, s2, op0, op1, rd, wr, eng=None):
        eng = eng or self.dve
        if op1 is None:
            return self.fw.op(eng, lambda: eng.h.tensor_scalar(out=out, in0=in0, scalar1=s1, scalar2=None, op0=op0),
                              rd, wr)
        return self.fw.op(eng, lambda: eng.h.tensor_scalar(out=out, in0=in0, scalar1=s1, scalar2=s2, op0=op0, op1=op1),
                          rd, wr)

    def CP(self, out, in_, rd, wr, eng=None):
        eng = eng or self.dve
        return self.fw.op(eng, lambda: eng.h.tensor_copy(out=out, in_=in_), rd, wr)

    def RECIP(self, out, in_, rd, wr):
        nc = self.nc
        return self.fw.op(self.dve, lambda: nc.vector.reciprocal(out=out, in_=in_), rd, wr)

    def MSET(self, ap, val, wr, eng=None):
        eng = eng or self.dve
        return self.fw.op(eng, lambda: eng.h.memset(ap, val), (), wr)

    def _dstream(self, wr, issuer, handle, pref):
        key = pref + (wr[0].name if len(wr) else "_out")
        if key not in self.dstreams:
            self.dstreams[key] = self.fw.add_engine(key, handle, step=16, issuer=issuer)
        return self.dstreams[key]

    def DMA(self, out, in_, rd, wr):
        nc = self.nc
        st = self._dstream(wr, self.sp, nc.sync, "dq_")
        return self.fw.op(st, lambda: nc.sync.dma_start(out=out, in_=in_), rd, wr)

    def DMAC(self, out, in_, rd, wr):
        nc = self.nc
        st = self._dstream(wr, self.pool, nc.gpsimd, "dg_")
        return self.fw.op(st, lambda: nc.gpsimd.dma_start(out=out, in_=in_), rd, wr)

    def rstd(self, out, ss_ps, inv_n, rd, wr):
        self.ACT(out, ss_ps, AF.Ln, rd, wr, bias=self.cst[:ss_ps.shape[0], 0:1], scale=inv_n)
        self.ACT(out, out, AF.Exp, wr, wr, scale=-0.5)

    def dump(self, name, ap, rd):
        if name not in self.dbg:
            return
        shape = list(ap.shape)
        o = self.nc.dram_tensor("dbg_" + name, shape, ap.dtype, kind="ExternalOutput").ap()
        self.dbg_out[name] = o
        self.DMA(o, ap, rd, ())

    def build(self):
        nc = self.nc
        din = self.din
        xT_d = din("xT", [D, S])
        outT_d = nc.dram_tensor("outT", [D, S], F32, kind="ExternalOutput").ap()
        cT_d = din("cT", [128, 8])
        pos_d = din("pos32", [32, S], I32)
        invf_d = din("invf", [32, 1])
        w_ada_d = din("w_ada", [DEPTH, D, 6 * D])
        b_ada_d = din("b_adaT", [128, DEPTH, 48])
        normg_d = din("norm_gT", [128, DEPTH, 2, 8])
        w_in_d = din("w_in", [DEPTH, D, IN_W])
        qkgA_d = din("qkgA", [128, DEPTH, 2])
        dlam_d = din("dlam", [128, DEPTH, 256])
        goutA_d = din("goutA", [128, DEPTH])
        biasA_d = din("biasA", [128, 8, 128])
        cfar_d = din("cfar", [128, 4])
        sgu_vg_d = din("sgu_vg", [DEPTH, 128, 512])
        sgu_wT_d = din("sgu_wT", [DEPTH, 128, 4, 128])
        sgu_bb_d = din("sgu_bb", [DEPTH, 128, 4, 128])
        latg_d = din("latg", [128, DEPTH, 3])
        w_uq_d = din("w_uq", [DEPTH, 256, 768])
        w_ukv_d = din("w_ukv", [DEPTH, 128, 1024])
        qkgCn_d = din("qkgCn", [128, DEPTH, 2])
        qkgCr_d = din("qkgCr", [32, DEPTH, 2])
        w_br_d = din("w_branch", [DEPTH, 3, 512, D])
        w_out_d = din("w_out", [DEPTH, D, D])
        w_r_d = din("w_r", [DEPTH, D, 36])
        b_r_d = din("b_r", [128, DEPTH, 36])
        weg_d = din("w_e_gate", [DEPTH, 32, D, 256])
        weu_d = din("w_e_up", [DEPTH, 32, D, 256])
        wed_d = din("w_e_down", [DEPTH, 32, 256, D])
        R32_d = din("R32T", [32, 32])
        ident_d = din("ident", [128, 128])

        with ExitStack() as es:
            self.es = es
            fw = self.fw = FW(nc, es)
            self.pe = fw.add_engine("pe", nc.tensor)
            self.act = fw.add_engine("act", nc.scalar)
            self.dve = fw.add_engine("dve", nc.vector)
            self.pool = fw.add_engine("pool", nc.gpsimd)
            self.sp = fw.add_engine("sp", nc.sync)
            self.dstreams = {}
            MM, ACT, TT, STT, TS, CP, RECIP, MSET, DMA, DMAC = (self.MM, self.ACT, self.TT, self.STT, self.TS,
                                                               self.CP, self.RECIP, self.MSET, self.DMA, self.DMAC)
            alloc = self.alloc

            PS = []
            PB = []
            for i in range(8):
                PS.append(es.enter_context(nc.psum_tensor(f"ps{i}", [128, 512], F32)))
                PB.append(Buf(f"ps{i}", excl=True))
            self.psg_i = 0
            self.psg_banks = [6, 7]
            ALLB = list(range(8))

            def psg():
                i = self.psg_banks[self.psg_i % len(self.psg_banks)]
                self.psg_i += 1
                return PS[i], PB[i]

            xT, xTb = alloc("xT", [128, 8, S], F32)
            xTB = [Buf(f"xT{b}") for b in range(NB)]
            ones_bf, ones_b = alloc("ones_bf", [128, 128], BF16)
            bones, bones_b = alloc("bones64", [128, 128], BF16)
            cst, cst_b = alloc("cst", [128, 4], F32)
            self.cst = cst
            ident, ident_b = alloc("ident", [128, 128], F32)
            R32, R32_b = alloc("R32", [32, 32], BF16)
            sinT, sin_b = alloc("sinT", [32, S], BF16)
            cosT, cos_b = alloc("cosT", [32, S], BF16)
            comb_scr = nc.dram_tensor("comb_scr", [32, S], F32, kind="Internal").ap()
            comb_scr_b = Buf("comb_scr")
            hbf, hbf_b = alloc("hbf", [128, 8, TB], BF16)
            self.h_r = alloc("h_r", [128, TB], F32)
            ht = alloc("h_t", [128, 2, TB], F32)
            self.h_t = (ht[0], [Buf("ht0"), Buf("ht1")])
            self.n_sq = Ring(self, "n_sq", [128, TB], BF16, 2)
            self.n_r = Ring(self, "n_r", [128, TB], F32, 4)
            Pr = Ring(self, "Pt", [128, TB], BF16, 4)
            MOD, MOD_b = alloc("MOD", [128, DEPTH, 48], F32)
            A1, A1_b = alloc("A1", [128, DEPTH, 8], F32)
            A2, A2_b = alloc("A2", [128, DEPTH, 8], F32)
            neglam, neglam_b = alloc("neglam", [128, DEPTH], F32)
            gout, gout_b = alloc("gout", [128, DEPTH], F32)
            qkgA, qkgA_b = alloc("qkgA", [128, DEPTH, 2], F32)
            latg, latg_b = alloc("latg", [128, DEPTH, 3], F32)
            qkgCn, qkgCn_b = alloc("qkgCn", [128, DEPTH, 2], F32)
            qkgCr, qkgCr_b = alloc("qkgCr", [32, DEPTH, 2], F32)
            Eb, Eb_b = alloc("Eb", [128, 8, 128], F32)
            cfar, cfar_b = alloc("cfar", [128, 4], F32)
            b_r, b_r_b = alloc("b_r", [128, DEPTH, 36], F32)

            MSET(ones_bf[:], 1.0, [ones_b])
            MSET(bones[:], 0.0, [bones_b])
            MSET(bones[0:64, 0:64], 1.0, [bones_b])
            MSET(bones[64:128, 64:128], 1.0, [bones_b])
            MSET(cst[:, 0:1], EPS, [cst_b])
            MSET(cst[:, 1:2], 0.0, [cst_b])
            DMA(ident[:], ident_d, (), [ident_b])
            DMAC(R32[:], R32_d, (), [R32_b])
            DMA(qkgA[:], qkgA_d, (), [qkgA_b])
            DMA(latg[:], latg_d, (), [latg_b])
            DMA(qkgCn[:], qkgCn_d, (), [qkgCn_b])
            DMA(qkgCr[:], qkgCr_d, (), [qkgCr_b])
            DMA(cfar[:], cfar_d, (), [cfar_b])
            DMA(b_r[:], b_r_d, (), [b_r_b])
            for c in range(8):
                DMA(xT[:, c, :], xT_d[c * 128:(c + 1) * 128, :], (), xTB)

            with ExitStack() as pes:
                pos_i, pos_ib = alloc("pos_i", [32, S], I32, pes)
                ang, ang_b = alloc("ang", [32, S], F32, pes)
                t1, t1_b = alloc("rt1", [32, S], F32, pes)
                t2, t2_b = alloc("rt2", [32, S], F32, pes)
                ki, ki_b = alloc("rki", [32, S], I32, pes)
                invf, invf_b = alloc("invf", [32, 1], F32, pes)
                DMA(pos_i[:], pos_d, (), [pos_ib])
                DMA(invf[:], invf_d, (), [invf_b])
                CP(ang[:], pos_i[:], [pos_ib], [ang_b])
                TS(ang[:], ang[:], invf[:, 0:1], None, ALU.mult, None, [ang_b, invf_b], [ang_b])
                TS(t1[:], ang[:], float(1.0 / (2 * np.pi)), None, ALU.mult, None, [ang_b], [t1_b])
                CP(ki[:], t1[:], [t1_b], [ki_b])
                CP(t1[:], ki[:], [ki_b], [t1_b])
                STT(t2[:], t1[:], -6.28125, ang[:], ALU.mult, ALU.add, [t1_b, ang_b], [t2_b])
                STT(t2[:], t1[:], -0.0019353071795864769, t2[:], ALU.mult, ALU.add, [t1_b, t2_b], [t2_b])
                TS(t1[:], t2[:], float(np.pi), -float(2 * np.pi), ALU.is_gt, ALU.mult, [t2_b], [t1_b])
                TT(t2[:], t2[:], t1[:], ALU.add, [t2_b, t1_b], [t2_b])
                TS(t1[:], t2[:], -float(np.pi), float(2 * np.pi), ALU.is_lt, ALU.mult, [t2_b], [t1_b])
                TT(t2[:], t2[:], t1[:], ALU.add, [t2_b, t1_b], [t2_b])
                ACT(sinT[:], t2[:], AF.Sin, [t2_b], [sin_b])
                TS(t2[:], t2[:], float(np.pi / 2), None, ALU.add, None, [t2_b], [t2_b])
                TS(t1[:], t2[:], float(np.pi), -float(2 * np.pi), ALU.is_gt, ALU.mult, [t2_b], [t1_b])
                TT(t2[:], t2[:], t1[:], ALU.add, [t2_b, t1_b], [t2_b])
                ACT(cosT[:], t2[:], AF.Sin, [t2_b], [cos_b])

                biasA, biasA_b = alloc("biasA", [128, 8, 128], F32, pes)
                ncf, ncf_b = alloc("ncf", [128, 4], F32, pes)
                DMA(biasA[:], biasA_d, (), [biasA_b])
                TS(ncf[:], cfar[:], -1.0, None, ALU.mult, None, [cfar_b], [ncf_b])
                for bi in range(8):
                    ACT(Eb[:, bi, :], biasA[:, bi, :], AF.Exp, [biasA_b, ncf_b], [Eb_b], bias=ncf[:, bi % 4:bi % 4 + 1], scale=1.0)
                c_sb, c_b = alloc("c_sb", [128, 8], F32, pes)
                c_act, cact_b = alloc("c_act", [128, 8], BF16, pes)
                b_ada, bada_b = alloc("b_ada", [128, DEPTH, 48], F32, pes)
                normg, normg_b = alloc("normg", [128, DEPTH, 2, 8], F32, pes)
                dlam, dlam_b = alloc("dlam", [128, DEPTH, 256], F32, pes)
                goutA, goutA_b = alloc("goutA", [128, DEPTH], F32, pes)
                DMA(c_sb[:], cT_d, (), [c_b])
                DMA(b_ada[:], b_ada_d, (), [bada_b])
                DMA(normg[:], normg_d, (), [normg_b])
                DMA(dlam[:], dlam_d, (), [dlam_b])
                DMA(goutA[:], goutA_d, (), [goutA_b])
                ACT(c_act[:], c_sb[:], AF.Silu, [c_b], [cact_b])
                wa_ring = Ring(self, "wada", [128, 8, 1024], BF16, 0)
                wa_ring.items = [alloc(f"wada{i}", [128, 8, 1024], BF16, pes) for i in range(2)]
                for l in range(self.depth):
                    ps, pb = PS[l % 2], PB[l % 2]
                    for piece in range(6):
                        wa, wab = wa_ring.next()
                        src = w_ada_d[l, :, piece * 1024:(piece + 1) * 1024].rearrange("(kc p) n -> p kc n", p=128)
                        DMAC(wa[:], src, (), [wab])
                        for jj in range(8):
                            j = piece * 8 + jj
                            for kc in range(8):
                                MM(ps[:, j:j + 1], wa[:, kc, jj * 128:(jj + 1) * 128], c_act[:, kc:kc + 1],
                                   kc == 0, kc == 7, [wab, cact_b], [pb], inc=(kc == 7 and jj == 7))
                    TT(MOD[:, l, :], ps[:, 0:48], b_ada[:, l, :], ALU.add, [pb, bada_b], [MOD_b])
                for l in range(self.depth):
                    STT(A1[:, l, :], MOD[:, l, 8:16], 1.0, normg[:, l, 0, :], ALU.add, ALU.mult, [MOD_b, normg_b], [A1_b])
                    STT(A2[:, l, :], MOD[:, l, 32:40], 1.0, normg[:, l, 1, :], ALU.add, ALU.mult, [MOD_b, normg_b], [A2_b])
                lt, lt_b = alloc("lam_t", [128, DEPTH, 2, 64], F32, pes)
                ls, ls_b = alloc("lam_s", [128, DEPTH, 2], F32, pes)
                dl4 = dlam[:].rearrange("p l (a d) -> p l a d", a=4)
                TT(lt[:, :, 0, :], dl4[:, :, 0, :], dl4[:, :, 1, :], ALU.mult, [dlam_b], [lt_b])
                TT(lt[:, :, 1, :], dl4[:, :, 2, :], dl4[:, :, 3, :], ALU.mult, [dlam_b], [lt_b])
                fw.op(self.dve, lambda: nc.vector.tensor_reduce(out=ls[:], in_=lt[:], axis=AX.X, op=ALU.add), [lt_b], [ls_b])
                ACT(ls[:], ls[:], AF.Exp, [ls_b], [ls_b])
                for l in range(self.depth):
                    STT(neglam[:, l:l + 1], ls[:, l, 0:1], -1.0, ls[:, l, 1:2], ALU.mult, ALU.add, [ls_b], [neglam_b])
                    TS(neglam[:, l:l + 1], neglam[:, l:l + 1], -LAMBDA_INIT[l], None, ALU.add, None, [neglam_b], [neglam_b])
                    TS(gout[:, l:l + 1], goutA[:, l:l + 1], 1.0 - LAMBDA_INIT[l], None, ALU.mult, None, [goutA_b], [gout_b])
                self.dump("MOD", MOD[:], [MOD_b])
                self.dump("sinT", sinT[:], [sin_b])
                self.dump("cosT", cosT[:], [cos_b])
                self.dump("neglam", neglam[:], [neglam_b])
                fw.barrier()

            def make_h(tb, A, B_ap_fn, hbf, hbf_b, tmp_es, l, hf=None, hf_b=None):
                blk = slice(tb * TB, (tb + 1) * TB)
                r_sb, r_b = self.h_r
                t_sb, t_b = self.h_t
                ps, pb = psg()
                for c in range(8):
                    sq, sq_b = self.n_sq.next()
                    ACT(sq[:], xT[:, c, blk], AF.Square, [xTB[tb]], [sq_b])
                    MM(ps[:], ones_bf[:], sq[:], c == 0, c == 7, [ones_b, sq_b], [pb], inc=True)
                self.rstd(r_sb[:], ps[:], 1.0 / D, [pb, cst_b], [r_b])
                for c in range(8):
                    STT(t_sb[:, c % 2, :], xT[:, c, blk], A[:, l, c:c + 1], r_sb[:], ALU.mult, ALU.mult,
                        [xTB[tb], r_b], [t_b[c % 2]])
                    if hf is not None:
                        ACT(hf[:, c, :], t_sb[:, c % 2, :], AF.Identity, [t_b[c % 2], MOD_b], [hf_b], bias=B_ap_fn(c))
                        CP(hbf[:, c, :], hf[:, c, :], [hf_b], [hbf_b], eng=self.pool)
                    else:
                        ACT(hbf[:, c, :], t_sb[:, c % 2, :], AF.Identity, [t_b[c % 2], MOD_b], [hbf_b], bias=B_ap_fn(c))

            def group_norm_fm(src_ps, src_pb, npart, ones_l, inv_n, g_ap, out_ap, out_bufs, g_bufs):
                sq, sq_b = self.n_sq.next()
                ACT(sq[:npart, :], src_ps, AF.Square, [src_pb], [sq_b])
                ps, pb = psg()
                MM(ps[:npart, :], ones_l, sq[:npart, :], True, True, [ones_b, bones_b, sq_b], [pb])
                r, r_b = self.n_r.next()
                self.rstd(r[:npart, :], ps[:npart, :], inv_n, [pb, cst_b], [r_b])
                STT(out_ap, src_ps, g_ap, r[:npart, :], ALU.mult, ALU.mult, [src_pb, r_b] + g_bufs, out_bufs)

            def rope(x_bf, x_b, blk, out_ap, out_bufs):
                ps, pb = psg()
                MM(ps[0:32, :], R32[:], x_bf, True, True, [R32_b, x_b], [pb])
                ta, ta_b = self.n_r.next()
                tb_, tb_b = self.n_r.next()
                TT(ta[0:32, :], x_bf, cosT[:, blk], ALU.mult, [x_b, cos_b], [ta_b])
                TT(tb_[0:32, :], ps[0:32, :], sinT[:, blk], ALU.mult, [pb, sin_b], [tb_b])
                TT(out_ap, ta[0:32, :], tb_[0:32, :], ALU.add, [ta_b, tb_b], out_bufs)

            class Lane:
                pass
            lanes = []
            for k in range(2):
                ln = Lane()
                ln.sq = self.n_sq.items[k]
                ln.nr = [self.n_r.items[2 * k], self.n_r.items[2 * k + 1]]
                ln.banks = [4 * k, 4 * k + 1, 4 * k + 2, 4 * k + 3]
                ln.bi = 0
                lanes.append(ln)

            def lane_ps(ln):
                b = ln.banks[ln.bi % 4]
                ln.bi += 1
                return PS[b], PB[b]

            def g_group_norm(ln, src_ps, src_pb, npart, ones_l, inv_n, g_ap, out_ap, out_bufs, g_bufs):
                sq, sq_b = ln.sq
                ACT(sq[:npart, :], src_ps, AF.Square, [src_pb], [sq_b])
                yield
                ps, pb = lane_ps(ln)
                MM(ps[:npart, :], ones_l, sq[:npart, :], True, True, [ones_b, bones_b, sq_b], [pb])
                yield
                r, r_b = ln.nr[0]
                ACT(r[:npart, :], ps[:npart, :], AF.Ln, [pb, cst_b], [r_b], bias=cst[:npart, 0:1], scale=inv_n)
                yield
                ACT(r[:npart, :], r[:npart, :], AF.Exp, [r_b], [r_b], scale=-0.5)
                yield
                STT(out_ap, src_ps, g_ap, r[:npart, :], ALU.mult, ALU.mult, [src_pb, r_b] + g_bufs, out_bufs)
                yield

            def g_rope(ln, x_bf, x_b, blk, out_ap, out_bufs):
                ps, pb = lane_ps(ln)
                MM(ps[0:32, :], R32[:], x_bf, True, True, [R32_b, x_b], [pb])
                yield
                ta, ta_b = ln.nr[0]
                tb_, tb_b = ln.nr[1]
                TT(ta[0:32, :], x_bf, cosT[:, blk], ALU.mult, [x_b, cos_b], [ta_b])
                yield
                TT(tb_[0:32, :], ps[0:32, :], sinT[:, blk], ALU.mult, [pb, sin_b], [tb_b])
                yield
                TT(out_ap, ta[0:32, :], tb_[0:32, :], ALU.add, [ta_b, tb_b], out_bufs)
                yield

            def run_chains(chain_fns):
                todo = list(chain_fns)
                active = [None, None]
                while todo or any(a is not None for a in active):
                    for k in range(2):
                        if active[k] is None and todo:
                            active[k] = todo.pop(0)(lanes[k])
                        if active[k] is not None:
                            try:
                                next(active[k])
                            except StopIteration:
                                active[k] = None

            def emit_output():
                for c in range(8):
                    DMA(outT_d[c * 128:(c + 1) * 128, :], xT[:, c, :], xTB, ())
                fw.barrier()

            self.sub = float(os.environ.get("K_SUB", 99))

            def sstop(n):
                if self.sub <= n and not fw.disabled:
                    fw.barrier()
                    emit_output()
                    fw.disabled = True

            def stop_at(n):
                if self.stop <= n and not fw.disabled:
                    fw.barrier()
                    emit_output()
                    fw.disabled = True

            if True:
              stop_at(0)
              for l in range(self.depth):
                with ExitStack() as les:
                  with ExitStack() as mes:
                    o_c, o_c_b = alloc("o_c", [128, 4, S], BF16, mes)
                    B1 = lambda c: MOD[:, l, c:c + 1]
                    B2 = lambda c: MOD[:, l, 24 + c:24 + c + 1]

                    with ExitStack() as ses:
                        w_mla, w_mla_b = alloc("w_mla", [128, 8, 416], BF16, ses)
                        w_uq, w_uq_b = alloc("w_uq", [128, 2, 768], BF16, ses)
                        w_ukv, w_ukv_b = alloc("w_ukv", [128, 1024], BF16, ses)
                        Kn, Kn_b = alloc("Kn", [128, 4, S], BF16, ses)
                        Vc, Vc_b = alloc("Vc", [128, 16, 512], BF16, ses)
                        Kr, Kr_b = alloc("Kr", [32, S], BF16, ses)
                        cqn, cqn_b = alloc("cqn", [128, 2, TB], BF16, ses)
                        cqf, cqf_b = alloc("cqf", [128, 2, TB], F32, ses)
                        cqs, cqs_b = alloc("cqs", [128, 2, TB], BF16, ses)
                        ckvn, ckvn_b = alloc("ckvn", [128, TB], BF16, ses)
                        qn, qn_b = alloc("qn", [128, 4, TB], BF16, ses)
                        qr, qr_b = alloc("qr", [32, 8, TB], BF16, ses)
                        xr_r = [alloc(f"xr{i}", [32, TB], BF16, ses) for i in range(2)]
                        rc, rc_b = alloc("rc", [128, TB], F32, ses)
                        acc_r = [alloc(f"accC{i}", [128, TB], F32, ses) for i in range(2)]
                        accb_r = [alloc(f"accCb{i}", [128, TB], BF16, ses) for i in range(2)]
                        DMAC(w_mla[:], w_in_d[l, :, 2560:2976].rearrange("(kc p) n -> p kc n", p=128), (), [w_mla_b])
                        DMAC(w_uq[:], w_uq_d[l].rearrange("(kc p) n -> p kc n", p=128), (), [w_uq_b])
                        DMAC(w_ukv[:], w_ukv_d[l], (), [w_ukv_b])
                        sc_c = float(96 ** -0.5)
                        for tb in range(NB):
                            blk = slice(tb * TB, (tb + 1) * TB)
                            self.psg_banks = ALLB
                            make_h(tb, A1, B1, hbf, hbf_b, ses, l)
                            if l == 0 and tb == 0:
                                self.dump("h0", hbf[:], [hbf_b])
                            sstop(1)
                            for j in range(2):
                                ps, pb = psg()
                                for kc in range(8):
                                    MM(ps[:], w_mla[:, kc, j * 128:(j + 1) * 128], hbf[:, kc, :], kc == 0, kc == 7,
                                       [w_mla_b, hbf_b], [pb])
                                kvar = int(os.environ.get("K_VAR", 0))
                                if kvar in (0, 2):
                                    ACT(cqs[:, j, :], ps[:], AF.Square, [pb], [cqs_b])
                                if kvar in (0, 3):
                                    CP(cqf[:, j, :], ps[:], [pb], [cqf_b])
                            sstop(1.2)
                            ps, pb = psg()
                            for j in range(2):
                                MM(ps[:], ones_bf[:], cqs[:, j, :], j == 0, j == 1, [ones_b, cqs_b], [pb])
                            r, r_b = self.n_r.next()
                            self.rstd(r[:], ps[:], 1.0 / 256, [pb, cst_b], [r_b])
                            for j in range(2):
                                STT(cqn[:, j, :], cqf[:, j, :], latg[:, l, j:j + 1], r[:], ALU.mult, ALU.mult,
                                    [cqf_b, r_b, latg_b], [cqn_b])
                            sstop(1.5)

                            def ch_ckv(ln):
                                ps, pb = lane_ps(ln)
                                for kc in range(8):
                                    MM(ps[:], w_mla[:, kc, 256:384], hbf[:, kc, :], kc == 0, kc == 7, [w_mla_b, hbf_b], [pb])
                                yield
                                yield from g_group_norm(ln, ps[:], pb, 128, ones_bf[:], 1.0 / 128, latg[:, l, 2:3], ckvn[:], [ckvn_b], [latg_b])

                            def ch_kr(ln):
                                ps, pb = lane_ps(ln)
                                for kc in range(8):
                                    MM(ps[0:32, :], w_mla[:, kc, 384:416], hbf[:, kc, :], kc == 0, kc == 7, [w_mla_b, hbf_b], [pb])
                                yield
                                x, x_b = xr_r[lanes.index(ln)]
                                yield from g_group_norm(ln, ps[0:32, :], pb, 32, ones_bf[0:32, 0:32], 1.0 / 32, qkgCr[:, l, 1:2], x[:], [x_b], [qkgCr_b])
                                yield from g_rope(ln, x[:], x_b, blk, Kr[:, blk], [Kr_b])

                            def ch_qnope(j):
                                def f(ln):
                                    ps, pb = lane_ps(ln)
                                    for kc in range(2):
                                        MM(ps[:], w_uq[:, kc, j * 128:(j + 1) * 128], cqn[:, kc, :], kc == 0, kc == 1, [w_uq_b, cqn_b], [pb])
                                    yield
                                    yield from g_group_norm(ln, ps[:], pb, 128, bones[:], 1.0 / 64, qkgCn[:, l, 0:1], qn[:, j, :], [qn_b], [qkgCn_b])
                                return f

                            def ch_qrope(h):
                                def f(ln):
                                    ps, pb = lane_ps(ln)
                                    for kc in range(2):
                                        MM(ps[0:32, :], w_uq[:, kc, 512 + h * 32:512 + (h + 1) * 32], cqn[:, kc, :], kc == 0, kc == 1, [w_uq_b, cqn_b], [pb])
                                    yield
                                    x, x_b = xr_r[lanes.index(ln)]
                                    yield from g_group_norm(ln, ps[0:32, :], pb, 32, ones_bf[0:32, 0:32], 1.0 / 32, qkgCr[:, l, 0:1], x[:], [x_b], [qkgCr_b])
                                    yield from g_rope(ln, x[:], x_b, blk, qr[:, h, :], [qr_b])
                                return f

                            def ch_knope(j):
                                def f(ln):
                                    ps, pb = lane_ps(ln)
                                    MM(ps[:], w_ukv[:, j * 128:(j + 1) * 128], ckvn[:], True, True, [w_ukv_b, ckvn_b], [pb])
                                    yield
                                    yield from g_group_norm(ln, ps[:], pb, 128, bones[:], 1.0 / 64, qkgCn[:, l, 1:2], Kn[:, j, blk], [Kn_b], [qkgCn_b])
                                return f

                            def ch_v(tt):
                                def f(ln):
                                    ps, pb = lane_ps(ln)
                                    MM(ps[:], ckvn[:, tt * 128:(tt + 1) * 128], w_ukv[:, 512:1024], True, True, [ckvn_b, w_ukv_b], [pb])
                                    yield
                                    CP(Vc[:, tb * 4 + tt, :], ps[:], [pb], [Vc_b])
                                    yield
                                return f

                            run_chains([ch_ckv, ch_kr])
                            sstop(3)
                            run_chains([ch_qnope(j) for j in range(4)] + [ch_qrope(h) for h in range(8)]
                                       + [ch_knope(j) for j in range(4)] + [ch_v(tt) for tt in range(4)])
                            if l == 0 and tb == 0:
                                self.dump("qn0", qn[:], [qn_b])
                                self.dump("qr0", qr[:], [qr_b])
                                self.dump("Kr0", Kr[:, 0:TB], [Kr_b])
                                self.dump("Kn0", Kn[:, :, 0:TB], [Kn_b])
                                self.dump("Vc0", Vc[:, 0:4, :], [Vc_b])
                            sstop(4)
                            self.psg_banks = [6, 7]
                            nkt = 4 * (tb + 1)
                            steps = [(j, hh, kt) for j in range(4) for hh in range(2) for kt in range(nkt)]
                            sring = [0]

                            def emit_S(st, i):
                                j, hh, kt = st
                                h = 2 * j + hh
                                rows = slice(hh * 64, (hh + 1) * 64)
                                j0 = max(0, kt - 4 * tb)
                                cols = slice(j0 * 128, TB)
                                Sps, Spb = PS[i % 2], PB[i % 2]
                                MM(Sps[:, cols], Kn[rows, j, kt * 128:(kt + 1) * 128], qn[rows, j, cols], True, False,
                                   [Kn_b, qn_b], [Spb], inc=False)
                                MM(Sps[:, cols], Kr[:, kt * 128:(kt + 1) * 128], qr[:, h, cols], False, True,
                                   [Kr_b, qr_b], [Spb])

                            def emit_rest(st, i):
                                j, hh, kt = st
                                h = 2 * j + hh
                                rows = slice(hh * 64, (hh + 1) * 64)
                                j0 = max(0, kt - 4 * tb)
                                cols = slice(j0 * 128, TB)
                                Sps, Spb = PS[i % 2], PB[i % 2]
                                Ops, Opb = PS[2 + j % 2], PB[2 + j % 2]
                                Sms, Smb = PS[4 + j % 2], PB[4 + j % 2]
                                P, P_b = Pr.next()
                                ACT(P[:, cols], Sps[:, cols], AF.Exp, [Spb], [P_b], scale=sc_c)
                                if kt >= 4 * tb:
                                    MSET(P[64:128, j0 * 128:j0 * 128 + 64], 0.0, [P_b])
                                first = (kt == 0)
                                last = (kt == nkt - 1)
                                MM(Ops[rows, cols], Vc[:, kt, h * 64:(h + 1) * 64], P[:, cols], first, last, [Vc_b, P_b], [Opb],
                                   inc=True)
                                acc, acc_b = acc_r[h % 2]
                                if first:
                                    CP(acc[:, cols], P[:, cols], [P_b], [acc_b])
                                else:
                                    TT(acc[:, cols], acc[:, cols], P[:, cols], ALU.add, [acc_b, P_b], [acc_b])
                                if last:
                                    def fin(acc=acc, acc_b=acc_b, h=h, hh=hh, j=j, rows=rows, Ops=Ops, Opb=Opb, Sms=Sms, Smb=Smb):
                                        accb, accb_b = accb_r[h % 2]
                                        CP(accb[:], acc[:], [acc_b], [accb_b])
                                        MM(Sms[rows, :], ones_bf[:, 0:64], accb[:], True, True, [ones_b, accb_b], [Smb], inc=True)
                                        if hh == 1:
                                            ACT(rc[:], Sms[:], AF.Ln, [Smb], [rc_b])
                                            ACT(rc[:], rc[:], AF.Exp, [rc_b], [rc_b], scale=-1.0)
                                            TT(o_c[:, j, blk], Ops[:], rc[:], ALU.mult, [Opb, rc_b], [o_c_b])
                                    pending.append((i + 2, fin))

                            pending = []
                            emit_S(steps[0], 0)
                            for i, st in enumerate(steps):
                                if i + 1 < len(steps):
                                    emit_S(steps[i + 1], i + 1)
                                emit_rest(st, i)
                                while pending and pending[0][0] <= i:
                                    pending.pop(0)[1]()
                            while pending:
                                pending.pop(0)[1]()
                        self.dump("o_c", o_c[:], [o_c_b])
                        fw.barrier()
                    stop_at(1)

                    o_a, o_a_b = alloc("o_a", [128, 4, S], BF16, mes)
                    with ExitStack() as ses:
                        wa_r = [alloc(f"w_a{i}", [128, 8, 512], BF16, ses) for i in range(2)]
                        wa_i = [0]
                        Ka, Ka_b = alloc("Ka", [128, 4, S], BF16, ses)
                        Va, Va_b = alloc("Va", [128, 16, 512], BF16, ses)
                        Qa, Qa_b = alloc("Qa", [128, 4, TB], BF16, ses)
                        rc, rc_b = alloc("rca", [128, TB], F32, ses)
                        accA_r = [alloc(f"accA{i}", [128, TB], F32, ses) for i in range(2)]
                        accAb, accAb_b = alloc("accAb", [128, TB], BF16, ses)
                        o0, o0_b = alloc("o0", [128, TB], F32, ses)
                        o1, o1_b = alloc("o1", [128, TB], F32, ses)
                        dd, dd_b = alloc("dd", [128, TB], F32, ses)
                        sc_a = 0.125
                        for tb in range(NB):
                            blk = slice(tb * TB, (tb + 1) * TB)
                            self.psg_banks = ALLB
                            make_h(tb, A1, B1, hbf, hbf_b, ses, l)
                            wqkv = []
                            for q in range(3):
                                w_a, w_a_b = wa_r[wa_i[0] % 2]; wa_i[0] += 1
                                DMAC(w_a[:], w_in_d[l, :, q * 512:(q + 1) * 512].rearrange("(kc p) n -> p kc n", p=128), (), [w_a_b])
                                if q <= 1:
                                    def ch_qk(hd, q=q, w_a=w_a, w_a_b=w_a_b):
                                        def f(ln):
                                            ps, pb = lane_ps(ln)
                                            for kc in range(8):
                                                MM(ps[:], w_a[:, kc, hd * 128:(hd + 1) * 128], hbf[:, kc, :], kc == 0, kc == 7, [w_a_b, hbf_b], [pb])
                                            yield
                                            if q == 0:
                                                yield from g_group_norm(ln, ps[:], pb, 128, bones[:], 1.0 / 64, qkgA[:, l, 0:1], Qa[:, hd, :], [Qa_b], [qkgA_b])
                                            else:
                                                yield from g_group_norm(ln, ps[:], pb, 128, bones[:], 1.0 / 64, qkgA[:, l, 1:2], Ka[:, hd, blk], [Ka_b], [qkgA_b])
                                        return f
                                    run_chains([ch_qk(hd) for hd in range(4)])
                                else:
                                    for tt in range(4):
                                        ps, pb = psg()
                                        for kc in range(8):
                                            MM(ps[:], hbf[:, kc, tt * 128:(tt + 1) * 128], w_a[:, kc, :], kc == 0, kc == 7, [hbf_b, w_a_b], [pb])
                                        CP(Va[:, tb * 4 + tt, :], ps[:], [pb], [Va_b])
                            if l == 0 and tb == 0:
                                self.dump("Qa0", Qa[:], [Qa_b])
                                self.dump("Va0", Va[:, 0:4, :], [Va_b])
                            self.psg_banks = [7]
                            SB3 = [0, 1, 6]
                            nkt = 4 * (tb + 1)
                            steps = [(hd, m, kt) for hd in range(4) for m in range(2) for kt in range(nkt)]

                            def emit_S(st, i):
                                hd, m, kt = st
                                rows = slice(m * 64, (m + 1) * 64)
                                j0 = max(0, kt - 4 * tb)
                                cols = slice(j0 * 128, TB)
                                Sps, Spb = PS[SB3[i % 3]], PB[SB3[i % 3]]
                                MM(Sps[:, cols], Ka[rows, hd, kt * 128:(kt + 1) * 128], Qa[rows, hd, cols], True, True,
                                   [Ka_b, Qa_b], [Spb])

                            def emit_rest(st, i):
                                hd, m, kt = st
                                sidx = hd * 2 + m
                                j0 = max(0, kt - 4 * tb)
                                cols = slice(j0 * 128, TB)
                                Sps, Spb = PS[SB3[i % 3]], PB[SB3[i % 3]]
                                Ops, Opb = PS[2 + sidx % 2], PB[2 + sidx % 2]
                                Sms, Smb = PS[4 + sidx % 2], PB[4 + sidx % 2]
                                P, P_b = Pr.next()
                                jq_far0 = max(j0, kt - 4 * tb + 2)
                                ACT(P[:, cols], Sps[:, cols], AF.Exp, [Spb, cfar_b], [P_b], bias=cfar[:, hd:hd + 1], scale=sc_a)
                                for jq in range(j0, min(4, jq_far0)):
                                    delta = kt - (4 * tb + jq)
                                    bi = (0 if delta == 0 else 1) * 4 + hd
                                    qc = slice(jq * 128, (jq + 1) * 128)
                                    TT(P[:, qc], P[:, qc], Eb[:, bi, :], ALU.mult, [P_b, Eb_b], [P_b], eng=self.pool)
                                first = (kt == 0)
                                last = (kt == nkt - 1)
                                MM(Ops[:, cols], Va[:, kt, hd * 128:(hd + 1) * 128], P[:, cols], first, last, [Va_b, P_b], [Opb],
                                   inc=True)
                                acc, acc_b = accA_r[sidx % 2]
                                if first:
                                    CP(acc[:, cols], P[:, cols], [P_b], [acc_b])
                                else:
                                    TT(acc[:, cols], acc[:, cols], P[:, cols], ALU.add, [acc_b, P_b], [acc_b])
                                if last:
                                    def fin(acc=acc, acc_b=acc_b, hd=hd, m=m, Ops=Ops, Opb=Opb, Sms=Sms, Smb=Smb):
                                        CP(accAb[:], acc[:], [acc_b], [accAb_b])
                                        MM(Sms[:], ones_bf[:], accAb[:], True, True, [ones_b, accAb_b], [Smb], inc=True)
                                        ACT(rc[:], Sms[:], AF.Ln, [Smb], [rc_b])
                                        ACT(rc[:], rc[:], AF.Exp, [rc_b], [rc_b], scale=-1.0)
                                        if m == 0:
                                            TT(o0[:], Ops[:], rc[:], ALU.mult, [Opb, rc_b], [o0_b])
                                        else:
                                            TT(o1[:], Ops[:], rc[:], ALU.mult, [Opb, rc_b], [o1_b])
                                            STT(dd[:], o1[:], neglam[:, l:l + 1], o0[:], ALU.mult, ALU.add, [o1_b, o0_b, neglam_b], [dd_b])
                                            sq, sq_b = self.n_sq.next()
                                            ACT(sq[:], dd[:], AF.Square, [dd_b], [sq_b])
                                            ps, pb = psg()
                                            MM(ps[:], ones_bf[:], sq[:], True, True, [ones_b, sq_b], [pb])
                                            r, r_b = self.n_r.next()
                                            self.rstd(r[:], ps[:], 1.0 / 128, [pb, cst_b], [r_b])
                                            STT(o_a[:, hd, blk], dd[:], gout[:, l:l + 1], r[:], ALU.mult, ALU.mult,
                                                [dd_b, r_b, gout_b], [o_a_b])
                                    pending.append((i + 2, fin))

                            pending = []
                            emit_S(steps[0], 0)
                            emit_S(steps[1], 1)
                            for i, st in enumerate(steps):
                                if i + 2 < len(steps):
                                    emit_S(steps[i + 2], i + 2)
                                emit_rest(st, i)
                                while pending and pending[0][0] <= i:
                                    pending.pop(0)[1]()
                            while pending:
                                pending.pop(0)[1]()
                        self.dump("o_a", o_a[:], [o_a_b])
                        fw.barrier()
                    stop_at(2)

                    with ExitStack() as ses:
                        w8_r = [alloc(f"w8_{i}", [128, 8, 512], BF16, ses) for i in range(2)]
                        wb_r = [alloc(f"wb{i}", [128, 4, 512], BF16, ses) for i in range(2)]
                        rings = {"w8": 0, "b": 0}
                        sgw_f, sgw_fb = alloc("sgw_f", [128, 4, 128], F32, ses)
                        sgw, sgw_b = alloc("sgw", [128, 4, 128], BF16, ses)
                        sgb, sgb_b = alloc("sgb", [128, 4, 128], F32, ses)
                        vg, vg_b = alloc("vg", [128, 512], F32, ses)
                        uT, uT_b = alloc("uT", [128, 4, TB], BF16, ses)
                        o_b, o_b_b = alloc("o_b", [128, 4, TB], BF16, ses)
                        vf, vf_b = alloc("vf", [128, 512], F32, ses)
                        vjunk, vjunk_b = alloc("vjunk", [128, 512], BF16, ses)
                        vss, vss_b = alloc("vss", [128, 2], F32, ses)
                        vn, vn_b = alloc("vn", [128, 512], BF16, ses)
                        vt, vt_b = alloc("vt", [128, 128], F32, ses)
                        gsb, gsb_b = alloc("gsb", [128, TB], F32, ses)
                        mt, mt_b = alloc("mt", [128, TB], F32, ses)
                        macc, macc_b = alloc("macc", [128, 4, TB], F32, ses)
                        merged, merged_b = alloc("merged", [128, 8, TB], BF16, ses)
                        DMA(sgw_f[:], sgu_wT_d[l], (), [sgw_fb])
                        DMA(sgb[:], sgu_bb_d[l], (), [sgb_b])
                        DMA(vg[:], sgu_vg_d[l], (), [vg_b])
                        MSET(sgw_f[64:128, :, 0:64], 0.0, [sgw_fb])
                        CP(sgw[:], sgw_f[:], [sgw_fb], [sgw_b])
                        self.psg_banks = ALLB
                        for tb in range(NB):
                            blk = slice(tb * TB, (tb + 1) * TB)
                            make_h(tb, A1, B1, hbf, hbf_b, ses, l)
                            wu, wu_b = w8_r[rings["w8"] % 2]; rings["w8"] += 1
                            DMAC(wu[:], w_in_d[l, :, 1536:2048].rearrange("(kc p) n -> p kc n", p=128), (), [wu_b])
                            wv, wv_b = w8_r[rings["w8"] % 2]; rings["w8"] += 1
                            DMAC(wv[:], w_in_d[l, :, 2048:2560].rearrange("(kc p) n -> p kc n", p=128), (), [wv_b])
                            for cu in range(4):
                                ps, pb = psg()
                                for kc in range(8):
                                    MM(ps[:], wu[:, kc, cu * 128:(cu + 1) * 128], hbf[:, kc, :], kc == 0, kc == 7, [wu_b, hbf_b], [pb])
                                ACT(uT[:, cu, :], ps[:], AF.Gelu_apprx_tanh, [pb], [uT_b])
                            for tt in range(4):
                                ps, pb = psg()
                                for kc in range(8):
                                    MM(ps[:], hbf[:, kc, tt * 128:(tt + 1) * 128], wv[:, kc, :], kc == 0, kc == 7, [hbf_b, wv_b], [pb])
                                ACT(vf[:], ps[:], AF.Gelu_apprx_tanh, [pb], [vf_b])
                                fw.op(self.act, lambda: nc.scalar.activation(out=vjunk[:], in_=vf[:], func=AF.Square,
                                                                             accum_out=vss[:, 0:1]),
                                      [vf_b], [vjunk_b, vss_b])
                                ACT(vss[:, 1:2], vss[:, 0:1], AF.Ln, [vss_b, cst_b], [vss_b], bias=cst[:, 0:1], scale=1.0 / 512)
                                ACT(vss[:, 1:2], vss[:, 1:2], AF.Exp, [vss_b], [vss_b], scale=-0.5)
                                STT(vn[:], vf[:], vss[:, 1:2], vg[:], ALU.mult, ALU.mult, [vf_b, vss_b, vg_b], [vn_b])
                                for g in range(4):
                                    ps2, pb2 = psg()
                                    MM(ps2[:, 0:128], vn[:, g * 128:(g + 1) * 128], sgw[:, g, :], True, True, [vn_b, sgw_b], [pb2])
                                    TT(vt[:], ps2[:, 0:128], sgb[:, g, :], ALU.add, [pb2, sgb_b], [vt_b])
                                    TT(o_b[:, g, tt * 128:(tt + 1) * 128], vt[:], uT[:, g, tt * 128:(tt + 1) * 128], ALU.mult,
                                       [vt_b, uT_b], [o_b_b])
                            if l == 0 and tb == 0:
                                self.dump("o_b0", o_b[:], [o_b_b])
                            for dcg in range(2):
                                for n in range(3):
                                    wg, wg_b = w8_r[rings["w8"] % 2]; rings["w8"] += 1
                                    c0 = 2976 + n * 1024 + dcg * 512
                                    DMAC(wg[:], w_in_d[l, :, c0:c0 + 512].rearrange("(kc p) n -> p kc n", p=128), (), [wg_b])
                                    wb, wb_b = wb_r[rings["b"] % 2]; rings["b"] += 1
                                    DMAC(wb[:], w_br_d[l, n, :, dcg * 512:(dcg + 1) * 512].rearrange("(kc p) n -> p kc n", p=128), (), [wb_b])
                                    src, src_b = [(o_a, o_a_b), (o_b, o_b_b), (o_c, o_c_b)][n]
                                    for dci in range(4):
                                        psa, pba = psg()
                                        for kc in range(8):
                                            MM(psa[:], wg[:, kc, dci * 128:(dci + 1) * 128], hbf[:, kc, :], kc == 0, kc == 7, [wg_b, hbf_b], [pba])
                                        ACT(gsb[:], psa[:], AF.Sigmoid, [pba], [gsb_b])
                                        psb, pbb = psg()
                                        for kc in range(4):
                                            rhs = src[:, kc, :] if n == 1 else src[:, kc, blk]
                                            MM(psb[:], wb[:, kc, dci * 128:(dci + 1) * 128], rhs, kc == 0, kc == 3, [wb_b, src_b], [pbb])
                                        if n == 0:
                                            TT(macc[:, dci, :], psb[:], gsb[:], ALU.mult, [pbb, gsb_b], [macc_b])
                                        else:
                                            TT(mt[:], psb[:], gsb[:], ALU.mult, [pbb, gsb_b], [mt_b])
                                            if n == 1:
                                                TT(macc[:, dci, :], macc[:, dci, :], mt[:], ALU.add, [macc_b, mt_b], [macc_b])
                                            else:
                                                TT(merged[:, dcg * 4 + dci, :], macc[:, dci, :], mt[:], ALU.add, [macc_b, mt_b], [merged_b])
                            if l == 0 and tb == 0:
                                self.dump("merged0", merged[:], [merged_b])
                            for dcg in range(2):
                                wo, wo_b = w8_r[rings["w8"] % 2]; rings["w8"] += 1
                                DMAC(wo[:], w_out_d[l, :, dcg * 512:(dcg + 1) * 512].rearrange("(kc p) n -> p kc n", p=128), (), [wo_b])
                                for dci in range(4):
                                    dc = dcg * 4 + dci
                                    ps, pb = psg()
                                    for kc in range(8):
                                        MM(ps[:], wo[:, kc, dci * 128:(dci + 1) * 128], merged[:, kc, :], kc == 0, kc == 7, [wo_b, merged_b], [pb])
                                    STT(xT[:, dc, blk], ps[:], MOD[:, l, 16 + dc:16 + dc + 1], xT[:, dc, blk], ALU.mult, ALU.add,
                                        [pb, MOD_b, xTB[tb]], [xTB[tb]])
                        fw.barrier()
                    if l == 0:
                        self.dump("x_mid", xT[:], xTB)
                    stop_at(3)

                    mes.close()
                    with ExitStack() as ses:
                        h2, h2_b = alloc("h2", [128, 8, S], BF16, ses)
                        rs = ExitStack()
                        h2f, h2f_b = alloc("h2f", [128, 8, TB], F32, rs)
                        combT, combT_b = alloc("combT", [32, S], F32, rs)
                        w_r, w_r_b = alloc("w_r", [128, 8, 36], F32, rs)
                        DMA(w_r[:], w_r_d[l].rearrange("(kc p) n -> p kc n", p=128), (), [w_r_b])
                        NT = 16
                        lgA, lg_b = alloc("lgA", [128, NT, 36], F32, rs)
                        ohg, ohg_b = alloc("ohg", [128, NT, 4], F32, rs)
                        ex4, ex4_b = alloc("ex4", [128, NT, 4], F32, rs)
                        v1, v1_b = alloc("v1", [128, 8, NT], F32, rs)
                        el3, el3_b = alloc("el3", [128, NT, 32], F32, rs)
                        els, els_b = alloc("els", [128, NT, 8], F32, rs)
                        els2, els2_b = alloc("els2", [128, NT, 8], F32, rs)
                        oh1, oh1_b = alloc("oh1", [128, NT, 8], F32, rs)
                        oh2, oh2_b = alloc("oh2", [128, NT, 8], F32, rs)
                        comb, comb_b = alloc("comb", [128, NT, 32], F32, rs)
                        h2B = [Buf(f"h2_{b}") for b in range(NB)]
                        self.psg_banks = ALLB
                        for tb in range(NB):
                            blk = slice(tb * TB, (tb + 1) * TB)
                            make_h(tb, A2, B2, h2[:, :, blk], h2B[tb], ses, l, hf=h2f, hf_b=h2f_b)
                            for tt in range(4):
                                ps, pb = psg()
                                for kc in range(8):
                                    MM(ps[:, 0:36], h2f[:, kc, tt * 128:(tt + 1) * 128], w_r[:, kc, :], kc == 0, kc == 7, [h2f_b, w_r_b], [pb])
                                TT(lgA[:, tb * 4 + tt, :], ps[:, 0:36], b_r[:, l, :], ALU.add, [pb, b_r_b], [lg_b])
                        def red(out, in_, op, rd, wr):
                            fw.op(self.dve, lambda: nc.vector.tensor_reduce(out=out, in_=in_, axis=AX.X, op=op), rd, wr)
                        gl = lgA[:, :, 0:4]
                        el4 = lgA[:, :, 4:36].rearrange("p t (g e) -> p t g e", g=4)
                        gmax, gsum, g_w, m1, m2, e2, w1, w2 = [v1[:, i, :] for i in range(8)]
                        bc4 = lambda a: a.unsqueeze(2).broadcast_to([128, NT, 4])
                        bc8 = lambda a: a.unsqueeze(2).broadcast_to([128, NT, 8])
                        red(gmax, gl, ALU.max, [lg_b], [v1_b])
                        TT(ohg[:], gl, bc4(gmax), ALU.is_equal, [lg_b, v1_b], [ohg_b])
                        TT(ex4[:], gl, bc4(gmax), ALU.subtract, [lg_b, v1_b], [ex4_b])
                        ACT(ex4[:], ex4[:], AF.Exp, [ex4_b], [ex4_b])
                        red(gsum, ex4[:], ALU.add, [ex4_b], [v1_b])
                        RECIP(g_w, gsum, [v1_b], [v1_b])
                        TT(el3[:].rearrange("p t (g e) -> p t g e", g=4), el4,
                           ohg[:].unsqueeze(3).broadcast_to([128, NT, 4, 8]), ALU.mult, [lg_b, ohg_b], [el3_b])
                        red(els[:], el3[:].rearrange("p t (g e) -> p t e g", g=4), ALU.add, [el3_b], [els_b])
                        red(m1, els[:], ALU.max, [els_b], [v1_b])
                        TT(oh1[:], els[:], bc8(m1), ALU.is_equal, [els_b, v1_b], [oh1_b])
                        STT(els2[:], oh1[:], -1.0e30, els[:], ALU.mult, ALU.add, [oh1_b, els_b], [els2_b])
                        red(m2, els2[:], ALU.max, [els2_b], [v1_b])
                        TT(oh2[:], els2[:], bc8(m2), ALU.is_equal, [els2_b, v1_b], [oh2_b])
                        TT(e2, m2, m1, ALU.subtract, [v1_b], [v1_b])
                        ACT(e2, e2, AF.Exp, [v1_b], [v1_b])
                        TS(w1, e2, 1.0, None, ALU.add, None, [v1_b], [v1_b])
                        RECIP(w1, w1, [v1_b], [v1_b])
                        TT(w2, e2, w1, ALU.mult, [v1_b], [v1_b])
                        TT(w1, w1, g_w, ALU.mult, [v1_b], [v1_b])
                        TT(w2, w2, g_w, ALU.mult, [v1_b], [v1_b])
                        TT(oh1[:], oh1[:], bc8(w1), ALU.mult, [oh1_b, v1_b], [oh1_b])
                        TT(oh2[:], oh2[:], bc8(w2), ALU.mult, [oh2_b, v1_b], [oh2_b])
                        TT(oh1[:], oh1[:], oh2[:], ALU.add, [oh1_b, oh2_b], [oh1_b])
                        TT(comb[:].rearrange("p t (g e) -> p t g e", g=4), ohg[:].unsqueeze(3).broadcast_to([128, NT, 4, 8]),
                           oh1[:].unsqueeze(2).broadcast_to([128, NT, 4, 8]), ALU.mult, [ohg_b, oh1_b], [comb_b])
                        for tb in range(NB):
                            ps2, pb2 = psg()
                            for tt in range(4):
                                fw.op(self.pe, lambda tt=tt: nc.tensor.transpose(ps2[0:32, tt * 128:(tt + 1) * 128], comb[:, tb * 4 + tt, :], ident[:]),
                                      [comb_b, ident_b], [pb2], inc=(tt == 3))
                            CP(combT[:, tb * TB:(tb + 1) * TB], ps2[0:32, :], [pb2], [combT_b])
                        self.dump("combT", combT[:], [combT_b])
                        if l == 0:
                            self.dump("h2", h2[:], h2B)
                        DMA(comb_scr, combT[:], [combT_b], [comb_scr_b])
                        fw.barrier()
                        rs.close()
                        stop_at(4)
                        self.psg_banks = [4, 5, 6, 7]
                        EE = 2
                        wgu_r = [alloc(f"wgu{i}", [128, 2, 8, 256], BF16, ses) for i in range(3)]
                        wd_r = [alloc(f"wd{i}", [128, 2, D], BF16, ses) for i in range(4)]
                        actT, actT_b0 = alloc("actT", [128, EE, 2, S], BF16, ses)
                        actB = [[Buf(f"act{e}_{b}") for b in range(NB)] for e in range(EE)]
                        sil_r = [alloc(f"sil{i}", [128, TB], BF16, ses) for i in range(2)]
                        ut_r = [alloc(f"ut{i}", [128, TB], BF16, ses) for i in range(2)]
                        cb_r = [alloc(f"cb{i}", [128, TB], F32, ses) for i in range(3)]
                        cnt = {"gu": 0, "d": 0, "s": 0, "cb": 0, "ps": 0}
                        for e0 in range(0, 32, EE):
                            wds = []
                            for ei in range(EE):
                                e = e0 + ei
                                wgu, wgu_b = wgu_r[cnt["gu"] % 3]; cnt["gu"] += 1
                                DMAC(wgu[:, 0, :, :], weg_d[l, e].rearrange("(kc p) n -> p kc n", p=128), (), [wgu_b])
                                DMAC(wgu[:, 1, :, :], weu_d[l, e].rearrange("(kc p) n -> p kc n", p=128), (), [wgu_b])
                                wd, wd_b = wd_r[cnt["d"] % 4]; cnt["d"] += 1
                                DMAC(wd[:], wed_d[l, e].rearrange("(fc p) n -> p fc n", p=128), (), [wd_b])
                                wds.append((wd, wd_b))
                                for tb in range(NB):
                                    blk = slice(tb * TB, (tb + 1) * TB)
                                    cb, cb_b = cb_r[cnt["cb"] % 3]; cnt["cb"] += 1
                                    DMA(cb[:], comb_scr[e:e + 1, blk].partition_broadcast(128), [comb_scr_b], [cb_b])
                                    for fc in range(2):
                                        k = cnt["ps"]; cnt["ps"] += 1
                                        aps, apb = PS[k % 2], PB[k % 2]
                                        ups, upb = PS[2 + k % 2], PB[2 + k % 2]
                                        for kc in range(8):
                                            MM(aps[:], wgu[:, 0, kc, fc * 128:(fc + 1) * 128], h2[:, kc, blk], kc == 0, kc == 7, [wgu_b, h2B[tb]], [apb])
                                        for kc in range(8):
                                            MM(ups[:], wgu[:, 1, kc, fc * 128:(fc + 1) * 128], h2[:, kc, blk], kc == 0, kc == 7, [wgu_b, h2B[tb]], [upb])
                                        sil, sil_b = sil_r[k % 2]
                                        ut, ut_b = ut_r[k % 2]
                                        ACT(sil[:], aps[:], AF.Silu, [apb], [sil_b])
                                        TT(ut[:], ups[:], cb[:], ALU.mult, [upb, cb_b], [ut_b])
                                        TT(actT[:, ei, fc, blk], sil[:], ut[:], ALU.mult, [sil_b, ut_b], [actB[ei][tb]], eng=self.pool)
                            for tb in range(NB):
                                blk = slice(tb * TB, (tb + 1) * TB)
                                for dc in range(8):
                                    ps, pb = psg()
                                    n_mm = EE * 2
                                    i_mm = 0
                                    for ei in range(EE):
                                        wd, wd_b = wds[ei]
                                        for fc in range(2):
                                            MM(ps[:], wd[:, fc, dc * 128:(dc + 1) * 128], actT[:, ei, fc, blk], i_mm == 0, i_mm == n_mm - 1,
                                               [wd_b, actB[ei][tb]], [pb])
                                            i_mm += 1
                                    STT(xT[:, dc, blk], ps[:], MOD[:, l, 40 + dc:40 + dc + 1], xT[:, dc, blk], ALU.mult, ALU.add,
                                        [pb, MOD_b, xTB[tb]], [xTB[tb]])
                        fw.barrier()

            emit_output()
            self.ninstr = fw.ninstr
        return nc


def _t5_bucket(rel):
    nb = 16
    max_exact = 8
    n = np.abs(rel)
    large = max_exact + (np.log(np.maximum(n, 1).astype(np.float32) / max_exact)
                         / math.log(128 / max_exact) * (nb - max_exact)).astype(np.int32)
    large = np.minimum(large, nb - 1)
    return np.where(rel > 0, nb, 0) + np.where(n < max_exact, n, large)


def _bucket_table():
    rel = np.arange(-300, 300)
    nb = 16
    max_exact = 8
    n = np.abs(rel)
    ratio = (np.maximum(n, 1).astype(np.float32) / np.float32(max_exact)).astype(np.float32)
    large = max_exact + (np.log(ratio).astype(np.float32) / np.float32(math.log(128 / max_exact))
                         * np.float32(nb - max_exact)).astype(np.int32)
    large = np.minimum(large, nb - 1)
    return np.where(rel > 0, nb, 0) + np.where(n < max_exact, n, large)


def prep_shared(inp, depth):
    f = np.float32
    g = {}
    g["w_ada"] = np.ascontiguousarray(inp["w_ada"], f)
    g["b_adaT"] = np.ascontiguousarray(inp["b_ada"].reshape(DEPTH, 48, 128).transpose(2, 0, 1), f)
    g["norm_gT"] = np.ascontiguousarray(inp["norm_g"].reshape(DEPTH, 2, 8, 128).transpose(3, 0, 1, 2), f)
    g["w_in"] = np.ascontiguousarray(inp["w_in"], f)
    qk = inp["diff_qk_g"]
    g["qkgA"] = np.ascontiguousarray(np.tile(qk.transpose(2, 0, 1), (2, 1, 1)), f)
    g["dlam"] = np.ascontiguousarray(np.broadcast_to(inp["diff_lambda"].reshape(1, DEPTH, 256), (128, DEPTH, 256)), f)
    g["goutA"] = np.ascontiguousarray(inp["diff_out_g"].T, f)
    bt = _bucket_table()
    ki = np.arange(128)[:, None]
    qi = np.arange(128)[None, :]
    tab = inp["rel_bias"]
    tiles = np.zeros((128, 8, 128), f)
    for di, delta in enumerate((0, -1)):
        rel = ki + 128 * delta - qi
        bidx = bt[rel + 300]
        for h in range(4):
            t = tab[bidx, h]
            if delta == 0:
                allowed = (ki // 64) <= (qi // 64)
                t = np.where(allowed, t, f(-30000.0))
            tiles[:, di * 4 + h, :] = t
    g["biasA"] = tiles
    g["cfar"] = np.ascontiguousarray(np.broadcast_to(tab[15][None, :], (128, 4)), f)
    g["sgu_vg"] = np.ascontiguousarray(np.broadcast_to(inp["sgu_v_g"][:, None, :], (DEPTH, 128, 512)), f)
    g["sgu_wT"] = np.ascontiguousarray(inp["sgu_w"].transpose(0, 3, 1, 2), f)
    g["sgu_bb"] = np.ascontiguousarray(np.broadcast_to(inp["sgu_b"][:, None, :, :], (DEPTH, 128, 4, 128)), f)
    lg = inp["mla_lat_g"]
    g["latg"] = np.ascontiguousarray(lg.reshape(DEPTH, 3, 128).transpose(2, 0, 1), f)
    wuq = inp["mla_w_uq"].reshape(DEPTH, 256, 8, 96)
    g["w_uq"] = np.ascontiguousarray(np.concatenate([wuq[..., :64].reshape(DEPTH, 256, 512),
                                                     wuq[..., 64:].reshape(DEPTH, 256, 256)], axis=-1), f)
    wukv = inp["mla_w_ukv"].reshape(DEPTH, 128, 8, 128)
    g["w_ukv"] = np.ascontiguousarray(np.concatenate([wukv[..., :64].reshape(DEPTH, 128, 512),
                                                      wukv[..., 64:].reshape(DEPTH, 128, 512)], axis=-1), f)
    qkc = inp["mla_qk_g"]
    g["qkgCn"] = np.ascontiguousarray(np.tile(qkc[:, :, :64].transpose(2, 0, 1), (2, 1, 1)), f)
    g["qkgCr"] = np.ascontiguousarray(qkc[:, :, 64:].transpose(2, 0, 1), f)
    g["w_branch"] = np.ascontiguousarray(inp["w_branch"], f)
    g["w_out"] = np.ascontiguousarray(inp["w_out"], f)
    g["w_r"] = np.ascontiguousarray(np.concatenate([inp["router_g_w"], inp["router_e_w"]], axis=-1), f)
    br = np.concatenate([inp["router_g_b"], inp["router_e_b"]], axis=-1)
    g["b_r"] = np.ascontiguousarray(np.broadcast_to(br[None], (128, DEPTH, 36)), f)
    g["w_e_gate"] = np.ascontiguousarray(inp["w_e_gate"].reshape(DEPTH, 32, D, 256), f)
    g["w_e_up"] = np.ascontiguousarray(inp["w_e_up"].reshape(DEPTH, 32, D, 256), f)
    g["w_e_down"] = np.ascontiguousarray(inp["w_e_down"].reshape(DEPTH, 32, 256, D), f)
    inv_freq = (10000.0 ** (-np.arange(0, 32, 2, dtype=np.float32) / np.float32(32))).astype(f)
    g["invf"] = np.ascontiguousarray(np.concatenate([inv_freq, inv_freq])[:, None], f)
    R = np.zeros((32, 32), f)
    for i in range(16):
        R[i + 16, i] = -1.0
        R[i, i + 16] = 1.0
    g["R32T"] = R
    g["ident"] = np.eye(128, dtype=f)
    return g


def prep_core(inp, b):
    f = np.float32
    m = {}
    m["xT"] = np.ascontiguousarray(np.asarray(inp["x"][b], f).T)
    m["cT"] = np.ascontiguousarray(np.asarray(inp["c"][b], f).reshape(8, 128).T)
    m["pos32"] = np.ascontiguousarray(np.broadcast_to(np.asarray(inp["positions"][b], np.int32)[None, :], (32, S)))
    return m


_CACHE = {}


def kernel(**inputs):
    inp = {k: np.asarray(v) for k, v in inputs.items()}
    depth = int(os.environ.get("K_DEPTH", DEPTH))
    if depth not in _CACHE:
        kb = K(depth)
        _CACHE[depth] = kb.build()
    nc = _CACHE[depth]
    shared = prep_shared(inp, depth)
    in_maps = []
    for b in range(8):
        m = dict(shared)
        m.update(prep_core(inp, b))
        in_maps.append(m)
    res = run_bass_kernel_spmd(nc, in_maps, core_ids=list(range(8)))
    out = np.stack([np.asarray(r["outT"], np.float32).T for r in res.results], axis=0)
    return np.ascontiguousarray(out)
```

```python
import math
import os
from contextlib import ExitStack

import numpy as np
import concourse.bass as bass
import concourse.mybir as mybir
from concourse.bass_utils import run_bass_kernel_spmd

F32 = mybir.dt.float32
BF16 = mybir.dt.bfloat16
I32 = mybir.dt.int32
AF = mybir.ActivationFunctionType
ALU = mybir.AluOpType
AX = mybir.AxisListType

DEPTH = 4
S = 2048
D = 1024
NB = 4
TB = 512
EPS = 1e-6
IN_W = 6048
LAMBDA_INIT = [0.8 - 0.6 * math.exp(-0.3 * l) for l in range(DEPTH)]


class Eng:
    def __init__(self, name, handle, sem, step, issuer=None):
        self.name = name
        self.h = handle
        self.sem = sem
        self.step = step
        self.count = 0
        self.issuer = issuer or self
        self.waited = {}


class Buf:
    __slots__ = ("name", "w", "r", "excl")

    def __init__(self, name="", excl=False):
        self.name = name
        self.w = None
        self.r = []
        self.excl = excl


class FW:
    def __init__(self, nc, es):
        self.nc = nc
        self.es = es
        self.engs = {}
        self.ninstr = 0
        self.disabled = False

    def add_engine(self, name, handle, step=1, issuer=None):
        sem = self.es.enter_context(self.nc.semaphore("s_" + name))
        e = Eng(name, handle, sem, step, issuer)
        self.engs[name] = e
        return e

    def _wait(self, eng, dep):
        e2, cnt = dep
        iss = eng.issuer
        if iss.waited.get(e2.name, 0) >= cnt:
            return
        iss.h.wait_ge(e2.sem, cnt * e2.step)
        iss.waited[e2.name] = cnt
        self.ninstr += 1

    def op(self, eng, fn, reads=(), writes=(), inc=True):
        if self.disabled:
            return None
        iss = eng.issuer
        for b in reads:
            if b.excl:
                for r in b.r:
                    if r[0].issuer is not iss:
                        self._wait(eng, r)
            if b.w is not None:
                if b.w[0] is iss and iss.name == "pe":
                    continue
                self._wait(eng, b.w)
        for b in writes:
            if b.w is not None and (b.w[0].issuer is not iss or b.w[0].step == 16):
                self._wait(eng, b.w)
            for r in b.r:
                if r[0] is iss and iss.step == 1 and eng.step == 1:
                    continue
                self._wait(eng, r)
        ins = fn()
        self.ninstr += 1
        if inc:
            eng.count += 1
            ins.then_inc(eng.sem, eng.step)
            tag = (eng, eng.count)
        else:
            tag = (eng, eng.count + 1)
        for b in reads:
            b.r.append(tag)
            if len(b.r) > 48:
                best = {}
                for (e, c) in b.r:
                    if e.name not in best or best[e.name][1] < c:
                        best[e.name] = (e, c)
                b.r = list(best.values())
        for b in writes:
            b.w = tag
            b.r = []
        return ins

    def barrier(self):
        if self.disabled:
            return
        issuers = {}
        for e in self.engs.values():
            issuers[e.issuer.name] = e.issuer
        for iss in issuers.values():
            for e2 in self.engs.values():
                if e2 is iss or e2.count == 0:
                    continue
                if iss.waited.get(e2.name, 0) >= e2.count:
                    continue
                iss.h.wait_ge(e2.sem, e2.count * e2.step)
                iss.waited[e2.name] = e2.count
                self.ninstr += 1


class Ring:
    def __init__(self, K, name, shape, dt, n, psum=False):
        self.items = []
        for i in range(n):
            self.items.append(K.alloc(f"{name}{i}", shape, dt))
        self.i = 0

    def next(self):
        it = self.items[self.i % len(self.items)]
        self.i += 1
        return it


class StopBuild(Exception):
    pass


class K:
    def __init__(self, depth, dbg=None):
        self.stop = int(os.environ.get("K_STOP", 99))
        self.depth = depth
        self.dbg = dbg or {}
        self.nc = bass.Bass("TRN2", target_bir_lowering=False)
        self.dram = {}
        self.dbg_out = {}

    def din(self, name, shape, dt=F32):
        self.dram[name] = self.nc.dram_tensor(name, list(shape), dt, kind="ExternalInput").ap()
        return self.dram[name]

    def alloc(self, name, shape, dt, es=None):
        es = es or self.es
        self.uid = getattr(self, "uid", 0) + 1
        t = es.enter_context(self.nc.sbuf_tensor(f"sb{self.uid}_{name}", list(shape), dt))
        return t, Buf(name)

    def MM(self, out, lhsT, rhs, start, stop, rd, wr, inc=None):
        nc = self.nc
        if inc is None:
            inc = stop
        return self.fw.op(self.pe, lambda: nc.tensor.matmul(out, lhsT=lhsT, rhs=rhs, start=start, stop=stop),
                          rd, wr, inc)

    def ACT(self, out, in_, func, rd, wr, bias=None, scale=None):
        nc = self.nc
        kw = {}
        if bias is not None:
            kw["bias"] = bias
        if scale is not None:
            kw["scale"] = scale
        return self.fw.op(self.act, lambda: nc.scalar.activation(out=out, in_=in_, func=func, **kw), rd, wr)

    def TT(self, out, in0, in1, op, rd, wr, eng=None):
        nc = self.nc
        eng = eng or self.dve
        return self.fw.op(eng, lambda: eng.h.tensor_tensor(out=out, in0=in0, in1=in1, op=op), rd, wr)

    def STT(self, out, in0, scalar, in1, op0, op1, rd, wr):
        nc = self.nc
        return self.fw.op(self.dve, lambda: nc.vector.scalar_tensor_tensor(out=out, in0=in0, scalar=scalar, in1=in1,
                                                                           op0=op0, op1=op1), rd, wr)

    def TS(self, out, in0, s1, s2, op0, op1, rd, wr, eng=None):
        eng = eng or self.dve
        if op1 is None:
            return self.fw.op(eng, lambda: eng.h.tensor_scalar(out=out, in0=in0, scalar1=s1, scalar2=None, op0=op0),
                              rd, wr)
        return self.fw.op(eng, lambda: eng.h.tensor_scalar(out=out, in0=in0, scalar1=s1, scalar2=s2, op0=op0, op1=op1),
                          rd, wr)

    def CP(self, out, in_, rd, wr, eng=None):
        eng = eng or self.dve
        return self.fw.op(eng, lambda: eng.h.tensor_copy(out=out, in_=in_), rd, wr)

    def RECIP(self, out, in_, rd, wr):
        nc = self.nc
        return self.fw.op(self.dve, lambda: nc.vector.reciprocal(out=out, in_=in_), rd, wr)

    def MSET(self, ap, val, wr, eng=None):
        eng = eng or self.dve
        return self.fw.op(eng, lambda: eng.h.memset(ap, val), (), wr)

    def _dstream(self, wr, issuer, handle, pref):
        key = pref + (wr[0].name if len(wr) else "_out")
        if key not in self.dstreams:
            self.dstreams[key] = self.fw.add_engine(key, handle, step=16, issuer=issuer)
        return self.dstreams[key]

    def DMA(self, out, in_, rd, wr):
        nc = self.nc
        st = self._dstream(wr, self.sp, nc.sync, "dq_")
        return self.fw.op(st, lambda: nc.sync.dma_start(out=out, in_=in_), rd, wr)

    def DMAC(self, out, in_, rd, wr):
        nc = self.nc
        st = self._dstream(wr, self.pool, nc.gpsimd, "dg_")
        return self.fw.op(st, lambda: nc.gpsimd.dma_start(out=out, in_=in_), rd, wr)

    def rstd(self, out, ss_ps, inv_n, rd, wr):
        self.ACT(out, ss_ps, AF.Ln, rd, wr, bias=self.cst[:ss_ps.shape[0], 0:1], scale=inv_n)
        self.ACT(out, out, AF.Exp, wr, wr, scale=-0.5)

    def dump(self, name, ap, rd):
        if name not in self.dbg:
            return
        shape = list(ap.shape)
        o = self.nc.dram_tensor("dbg_" + name, shape, ap.dtype, kind="ExternalOutput").ap()
        self.dbg_out[name] = o
        self.DMA(o, ap, rd, ())

    def build(self):
        nc = self.nc
        din = self.din
        xT_d = din("xT", [D, S])
        outT_d = nc.dram_tensor("outT", [D, S], F32, kind="ExternalOutput").ap()
        cT_d = din("cT", [128, 8])
        pos_d = din("pos32", [32, S], I32)
        invf_d = din("invf", [32, 1])
        w_ada_d = din("w_ada", [DEPTH, D, 6 * D])
        b_ada_d = din("b_adaT", [128, DEPTH, 48])
        normg_d = din("norm_gT", [128, DEPTH, 2, 8])
        w_in_d = din("w_in", [DEPTH, D, IN_W])
        qkgA_d = din("qkgA", [128, DEPTH, 2])
        dlam_d = din("dlam", [128, DEPTH, 256])
        goutA_d = din("goutA", [128, DEPTH])
        biasA_d = din("biasA", [128, 8, 128])
        cfar_d = din("cfar", [128, 4])
        sgu_vg_d = din("sgu_vg", [DEPTH, 128, 512])
        sgu_wT_d = din("sgu_wT", [DEPTH, 128, 4, 128])
        sgu_bb_d = din("sgu_bb", [DEPTH, 128, 4, 128])
        latg_d = din("latg", [128, DEPTH, 3])
        w_uq_d = din("w_uq", [DEPTH, 256, 768])
        w_ukv_d = din("w_ukv", [DEPTH, 128, 1024])
        qkgCn_d = din("qkgCn", [128, DEPTH, 2])
        qkgCr_d = din("qkgCr", [32, DEPTH, 2])
        w_br_d = din("w_branch", [DEPTH, 3, 512, D])
        w_out_d = din("w_out", [DEPTH, D, D])
        w_r_d = din("w_r", [DEPTH, D, 36])
        b_r_d = din("b_r", [128, DEPTH, 36])
        weg_d = din("w_e_gate", [DEPTH, 32, D, 256])
        weu_d = din("w_e_up", [DEPTH, 32, D, 256])
        wed_d = din("w_e_down", [DEPTH, 32, 256, D])
        R32_d = din("R32T", [32, 32])
        ident_d = din("ident", [128, 128])

        with ExitStack() as es:
            self.es = es
            fw = self.fw = FW(nc, es)
            self.pe = fw.add_engine("pe", nc.tensor)
            self.act = fw.add_engine("act", nc.scalar)
            self.dve = fw.add_engine("dve", nc.vector)
            self.pool = fw.add_engine("pool", nc.gpsimd)
            self.sp = fw.add_engine("sp", nc.sync)
            self.dstreams = {}
            MM, ACT, TT, STT, TS, CP, RECIP, MSET, DMA, DMAC = (self.MM, self.ACT, self.TT, self.STT, self.TS,
                                                               self.CP, self.RECIP, self.MSET, self.DMA, self.DMAC)
            alloc = self.alloc

            PS = []
            PB = []
            for i in range(8):
                PS.append(es.enter_context(nc.psum_tensor(f"ps{i}", [128, 512], F32)))
                PB.append(Buf(f"ps{i}", excl=True))
            self.psg_i = 0
            self.psg_banks = [6, 7]
            ALLB = list(range(8))

            def psg():
                i = self.psg_banks[self.psg_i % len(self.psg_banks)]
                self.psg_i += 1
                return PS[i], PB[i]

            xT, xTb = alloc("xT", [128, 8, S], F32)
            xTB = [Buf(f"xT{b}") for b in range(NB)]
            ones_bf, ones_b = alloc("ones_bf", [128, 128], BF16)
            bones, bones_b = alloc("bones64", [128, 128], BF16)
            cst, cst_b = alloc("cst", [128, 4], F32)
            self.cst = cst
            ident, ident_b = alloc("ident", [128, 128], F32)
            R32, R32_b = alloc("R32", [32, 32], BF16)
            sinT, sin_b = alloc("sinT", [32, S], BF16)
            cosT, cos_b = alloc("cosT", [32, S], BF16)
            comb_scr = nc.dram_tensor("comb_scr", [32, S], F32, kind="Internal").ap()
            comb_scr_b = Buf("comb_scr")
            hbf, hbf_b = alloc("hbf", [128, 8, TB], BF16)
            h_scr = nc.dram_tensor("h_scr", [NB, 128, 8 * TB], BF16, kind="Internal").ap()
            h_scr_b = [Buf(f"h_scr{b}") for b in range(NB)]
            hbf_flat = hbf[:].rearrange("p c t -> p (c t)")
            self.h_r = alloc("h_r", [128, TB], F32)
            ht = alloc("h_t", [128, 2, TB], F32)
            self.h_t = (ht[0], [Buf("ht0"), Buf("ht1")])
            self.n_sq = Ring(self, "n_sq", [128, TB], BF16, 2)
            self.n_r = Ring(self, "n_r", [128, TB], F32, 4)
            Pr = Ring(self, "Pt", [128, TB], BF16, 4)
            MOD, MOD_b = alloc("MOD", [128, DEPTH, 48], F32)
            A1, A1_b = alloc("A1", [128, DEPTH, 8], F32)
            A2, A2_b = alloc("A2", [128, DEPTH, 8], F32)
            neglam, neglam_b = alloc("neglam", [128, DEPTH], F32)
            gout, gout_b = alloc("gout", [128, DEPTH], F32)
            qkgA, qkgA_b = alloc("qkgA", [128, DEPTH, 2], F32)
            latg, latg_b = alloc("latg", [128, DEPTH, 3], F32)
            qkgCn, qkgCn_b = alloc("qkgCn", [128, DEPTH, 2], F32)
            qkgCr, qkgCr_b = alloc("qkgCr", [32, DEPTH, 2], F32)
            Eb, Eb_b = alloc("Eb", [128, 8, 128], F32)
            cfar, cfar_b = alloc("cfar", [128, 4], F32)
            b_r, b_r_b = alloc("b_r", [128, DEPTH, 36], F32)

            MSET(ones_bf[:], 1.0, [ones_b])
            MSET(bones[:], 0.0, [bones_b])
            MSET(bones[0:64, 0:64], 1.0, [bones_b])
            MSET(bones[64:128, 64:128], 1.0, [bones_b])
            MSET(cst[:, 0:1], EPS, [cst_b])
            MSET(cst[:, 1:2], 0.0, [cst_b])
            DMA(ident[:], ident_d, (), [ident_b])
            DMAC(R32[:], R32_d, (), [R32_b])
            DMA(qkgA[:], qkgA_d, (), [qkgA_b])
            DMA(latg[:], latg_d, (), [latg_b])
            DMA(qkgCn[:], qkgCn_d, (), [qkgCn_b])
            DMA(qkgCr[:], qkgCr_d, (), [qkgCr_b])
            DMA(cfar[:], cfar_d, (), [cfar_b])
            DMA(b_r[:], b_r_d, (), [b_r_b])
            for c in range(8):
                DMA(xT[:, c, :], xT_d[c * 128:(c + 1) * 128, :], (), xTB)

            with ExitStack() as pes:
                pos_i, pos_ib = alloc("pos_i", [32, S], I32, pes)
                ang, ang_b = alloc("ang", [32, S], F32, pes)
                t1, t1_b = alloc("rt1", [32, S], F32, pes)
                t2, t2_b = alloc("rt2", [32, S], F32, pes)
                ki, ki_b = alloc("rki", [32, S], I32, pes)
                invf, invf_b = alloc("invf", [32, 1], F32, pes)
                DMA(pos_i[:], pos_d, (), [pos_ib])
                DMA(invf[:], invf_d, (), [invf_b])
                CP(ang[:], pos_i[:], [pos_ib], [ang_b])
                TS(ang[:], ang[:], invf[:, 0:1], None, ALU.mult, None, [ang_b, invf_b], [ang_b])
                TS(t1[:], ang[:], float(1.0 / (2 * np.pi)), None, ALU.mult, None, [ang_b], [t1_b])
                CP(ki[:], t1[:], [t1_b], [ki_b])
                CP(t1[:], ki[:], [ki_b], [t1_b])
                STT(t2[:], t1[:], -6.28125, ang[:], ALU.mult, ALU.add, [t1_b, ang_b], [t2_b])
                STT(t2[:], t1[:], -0.0019353071795864769, t2[:], ALU.mult, ALU.add, [t1_b, t2_b], [t2_b])
                TS(t1[:], t2[:], float(np.pi), -float(2 * np.pi), ALU.is_gt, ALU.mult, [t2_b], [t1_b])
                TT(t2[:], t2[:], t1[:], ALU.add, [t2_b, t1_b], [t2_b])
                TS(t1[:], t2[:], -float(np.pi), float(2 * np.pi), ALU.is_lt, ALU.mult, [t2_b], [t1_b])
                TT(t2[:], t2[:], t1[:], ALU.add, [t2_b, t1_b], [t2_b])
                ACT(sinT[:], t2[:], AF.Sin, [t2_b], [sin_b])
                TS(t2[:], t2[:], float(np.pi / 2), None, ALU.add, None, [t2_b], [t2_b])
                TS(t1[:], t2[:], float(np.pi), -float(2 * np.pi), ALU.is_gt, ALU.mult, [t2_b], [t1_b])
                TT(t2[:], t2[:], t1[:], ALU.add, [t2_b, t1_b], [t2_b])
                ACT(cosT[:], t2[:], AF.Sin, [t2_b], [cos_b])

                biasA, biasA_b = alloc("biasA", [128, 8, 128], F32, pes)
                ncf, ncf_b = alloc("ncf", [128, 4], F32, pes)
                DMA(biasA[:], biasA_d, (), [biasA_b])
                TS(ncf[:], cfar[:], -1.0, None, ALU.mult, None, [cfar_b], [ncf_b])
                for bi in range(8):
                    ACT(Eb[:, bi, :], biasA[:, bi, :], AF.Exp, [biasA_b, ncf_b], [Eb_b], bias=ncf[:, bi % 4:bi % 4 + 1], scale=1.0)
                c_sb, c_b = alloc("c_sb", [128, 8], F32, pes)
                c_act, cact_b = alloc("c_act", [128, 8], BF16, pes)
                b_ada, bada_b = alloc("b_ada", [128, DEPTH, 48], F32, pes)
                normg, normg_b = alloc("normg", [128, DEPTH, 2, 8], F32, pes)
                dlam, dlam_b = alloc("dlam", [128, DEPTH, 256], F32, pes)
                goutA, goutA_b = alloc("goutA", [128, DEPTH], F32, pes)
                DMA(c_sb[:], cT_d, (), [c_b])
                DMA(b_ada[:], b_ada_d, (), [bada_b])
                DMA(normg[:], normg_d, (), [normg_b])
                DMA(dlam[:], dlam_d, (), [dlam_b])
                DMA(goutA[:], goutA_d, (), [goutA_b])
                ACT(c_act[:], c_sb[:], AF.Silu, [c_b], [cact_b])
                wa_ring = Ring(self, "wada", [128, 8, 1024], BF16, 0)
                wa_ring.items = [alloc(f"wada{i}", [128, 8, 1024], BF16, pes) for i in range(2)]
                for l in range(self.depth):
                    ps, pb = PS[l % 2], PB[l % 2]
                    for piece in range(6):
                        wa, wab = wa_ring.next()
                        src = w_ada_d[l, :, piece * 1024:(piece + 1) * 1024].rearrange("(kc p) n -> p kc n", p=128)
                        DMAC(wa[:], src, (), [wab])
                        for jj in range(8):
                            j = piece * 8 + jj
                            for kc in range(8):
                                MM(ps[:, j:j + 1], wa[:, kc, jj * 128:(jj + 1) * 128], c_act[:, kc:kc + 1],
                                   kc == 0, kc == 7, [wab, cact_b], [pb], inc=(kc == 7 and jj == 7))
                    TT(MOD[:, l, :], ps[:, 0:48], b_ada[:, l, :], ALU.add, [pb, bada_b], [MOD_b])
                for l in range(self.depth):
                    STT(A1[:, l, :], MOD[:, l, 8:16], 1.0, normg[:, l, 0, :], ALU.add, ALU.mult, [MOD_b, normg_b], [A1_b])
                    STT(A2[:, l, :], MOD[:, l, 32:40], 1.0, normg[:, l, 1, :], ALU.add, ALU.mult, [MOD_b, normg_b], [A2_b])
                lt, lt_b = alloc("lam_t", [128, DEPTH, 2, 64], F32, pes)
                ls, ls_b = alloc("lam_s", [128, DEPTH, 2], F32, pes)
                dl4 = dlam[:].rearrange("p l (a d) -> p l a d", a=4)
                TT(lt[:, :, 0, :], dl4[:, :, 0, :], dl4[:, :, 1, :], ALU.mult, [dlam_b], [lt_b])
                TT(lt[:, :, 1, :], dl4[:, :, 2, :], dl4[:, :, 3, :], ALU.mult, [dlam_b], [lt_b])
                fw.op(self.dve, lambda: nc.vector.tensor_reduce(out=ls[:], in_=lt[:], axis=AX.X, op=ALU.add), [lt_b], [ls_b])
                ACT(ls[:], ls[:], AF.Exp, [ls_b], [ls_b])
                for l in range(self.depth):
                    STT(neglam[:, l:l + 1], ls[:, l, 0:1], -1.0, ls[:, l, 1:2], ALU.mult, ALU.add, [ls_b], [neglam_b])
                    TS(neglam[:, l:l + 1], neglam[:, l:l + 1], -LAMBDA_INIT[l], None, ALU.add, None, [neglam_b], [neglam_b])
                    TS(gout[:, l:l + 1], goutA[:, l:l + 1], 1.0 - LAMBDA_INIT[l], None, ALU.mult, None, [goutA_b], [gout_b])
                self.dump("MOD", MOD[:], [MOD_b])
                self.dump("sinT", sinT[:], [sin_b])
                self.dump("cosT", cosT[:], [cos_b])
                self.dump("neglam", neglam[:], [neglam_b])
                fw.barrier()

            def make_h(tb, A, B_ap_fn, hbf, hbf_b, tmp_es, l, hf=None, hf_b=None):
                blk = slice(tb * TB, (tb + 1) * TB)
                r_sb, r_b = self.h_r
                t_sb, t_b = self.h_t
                ps, pb = psg()
                for c in range(8):
                    sq, sq_b = self.n_sq.next()
                    ACT(sq[:], xT[:, c, blk], AF.Square, [xTB[tb]], [sq_b])
                    MM(ps[:], ones_bf[:], sq[:], c == 0, c == 7, [ones_b, sq_b], [pb], inc=True)
                self.rstd(r_sb[:], ps[:], 1.0 / D, [pb, cst_b], [r_b])
                for c in range(8):
                    STT(t_sb[:, c % 2, :], xT[:, c, blk], A[:, l, c:c + 1], r_sb[:], ALU.mult, ALU.mult,
                        [xTB[tb], r_b], [t_b[c % 2]])
                    if hf is not None:
                        ACT(hf[:, c, :], t_sb[:, c % 2, :], AF.Identity, [t_b[c % 2], MOD_b], [hf_b], bias=B_ap_fn(c))
                        CP(hbf[:, c, :], hf[:, c, :], [hf_b], [hbf_b], eng=self.pool)
                    else:
                        ACT(hbf[:, c, :], t_sb[:, c % 2, :], AF.Identity, [t_b[c % 2], MOD_b], [hbf_b], bias=B_ap_fn(c))

            def group_norm_fm(src_ps, src_pb, npart, ones_l, inv_n, g_ap, out_ap, out_bufs, g_bufs):
                sq, sq_b = self.n_sq.next()
                ACT(sq[:npart, :], src_ps, AF.Square, [src_pb], [sq_b])
                ps, pb = psg()
                MM(ps[:npart, :], ones_l, sq[:npart, :], True, True, [ones_b, bones_b, sq_b], [pb])
                r, r_b = self.n_r.next()
                self.rstd(r[:npart, :], ps[:npart, :], inv_n, [pb, cst_b], [r_b])
                STT(out_ap, src_ps, g_ap, r[:npart, :], ALU.mult, ALU.mult, [src_pb, r_b] + g_bufs, out_bufs)

            def rope(x_bf, x_b, blk, out_ap, out_bufs):
                ps, pb = psg()
                MM(ps[0:32, :], R32[:], x_bf, True, True, [R32_b, x_b], [pb])
                ta, ta_b = self.n_r.next()
                tb_, tb_b = self.n_r.next()
                TT(ta[0:32, :], x_bf, cosT[:, blk], ALU.mult, [x_b, cos_b], [ta_b])
                TT(tb_[0:32, :], ps[0:32, :], sinT[:, blk], ALU.mult, [pb, sin_b], [tb_b])
                TT(out_ap, ta[0:32, :], tb_[0:32, :], ALU.add, [ta_b, tb_b], out_bufs)

            class Lane:
                pass
            lanes = []
            for k in range(2):
                ln = Lane()
                ln.sq = self.n_sq.items[k]
                ln.nr = [self.n_r.items[2 * k], self.n_r.items[2 * k + 1]]
                ln.banks = [4 * k, 4 * k + 1, 4 * k + 2, 4 * k + 3]
                ln.bi = 0
                lanes.append(ln)

            def lane_ps(ln):
                b = ln.banks[ln.bi % 4]
                ln.bi += 1
                return PS[b], PB[b]

            def g_group_norm(ln, src_ps, src_pb, npart, ones_l, inv_n, g_ap, out_ap, out_bufs, g_bufs):
                sq, sq_b = ln.sq
                ACT(sq[:npart, :], src_ps, AF.Square, [src_pb], [sq_b])
                yield
                ps, pb = lane_ps(ln)
                MM(ps[:npart, :], ones_l, sq[:npart, :], True, True, [ones_b, bones_b, sq_b], [pb])
                yield
                r, r_b = ln.nr[0]
                ACT(r[:npart, :], ps[:npart, :], AF.Ln, [pb, cst_b], [r_b], bias=cst[:npart, 0:1], scale=inv_n)
                yield
                ACT(r[:npart, :], r[:npart, :], AF.Exp, [r_b], [r_b], scale=-0.5)
                yield
                STT(out_ap, src_ps, g_ap, r[:npart, :], ALU.mult, ALU.mult, [src_pb, r_b] + g_bufs, out_bufs)
                yield

            def g_rope(ln, x_bf, x_b, blk, out_ap, out_bufs):
                ps, pb = lane_ps(ln)
                MM(ps[0:32, :], R32[:], x_bf, True, True, [R32_b, x_b], [pb])
                yield
                ta, ta_b = ln.nr[0]
                tb_, tb_b = ln.nr[1]
                TT(ta[0:32, :], x_bf, cosT[:, blk], ALU.mult, [x_b, cos_b], [ta_b])
                yield
                TT(tb_[0:32, :], ps[0:32, :], sinT[:, blk], ALU.mult, [pb, sin_b], [tb_b])
                yield
                TT(out_ap, ta[0:32, :], tb_[0:32, :], ALU.add, [ta_b, tb_b], out_bufs)
                yield

            def run_chains(chain_fns):
                todo = list(chain_fns)
                active = [None, None]
                while todo or any(a is not None for a in active):
                    for k in range(2):
                        if active[k] is None and todo:
                            active[k] = todo.pop(0)(lanes[k])
                        if active[k] is not None:
                            try:
                                next(active[k])
                            except StopIteration:
                                active[k] = None

            def emit_output():
                for c in range(8):
                    DMA(outT_d[c * 128:(c + 1) * 128, :], xT[:, c, :], xTB, ())
                fw.barrier()

            self.sub = float(os.environ.get("K_SUB", 99))

            def sstop(n):
                if self.sub <= n and not fw.disabled:
                    fw.barrier()
                    emit_output()
                    fw.disabled = True

            def stop_at(n):
                if self.stop <= n and not fw.disabled:
                    fw.barrier()
                    emit_output()
                    fw.disabled = True

            if True:
              stop_at(0)
              for l in range(self.depth):
                with ExitStack() as les:
                  with ExitStack() as mes:
                    o_c, o_c_b = alloc("o_c", [128, 4, S], BF16, mes)
                    B1 = lambda c: MOD[:, l, c:c + 1]
                    B2 = lambda c: MOD[:, l, 24 + c:24 + c + 1]

                    with ExitStack() as ses:
                        w_mla, w_mla_b = alloc("w_mla", [128, 8, 416], BF16, ses)
                        w_uq, w_uq_b = alloc("w_uq", [128, 2, 768], BF16, ses)
                        w_ukv, w_ukv_b = alloc("w_ukv", [128, 1024], BF16, ses)
                        Kn, Kn_b = alloc("Kn", [128, 4, S], BF16, ses)
                        Vc, Vc_b = alloc("Vc", [128, 16, 512], BF16, ses)
                        Kr, Kr_b = alloc("Kr", [32, S], BF16, ses)
                        cqn, cqn_b = alloc("cqn", [128, 2, TB], BF16, ses)
                        cqf, cqf_b = alloc("cqf", [128, 2, TB], F32, ses)
                        cqs, cqs_b = alloc("cqs", [128, 2, TB], BF16, ses)
                        ckvn, ckvn_b = alloc("ckvn", [128, TB], BF16, ses)
                        qn, qn_b = alloc("qn", [128, 4, TB], BF16, ses)
                        qr, qr_b = alloc("qr", [32, 8, TB], BF16, ses)
                        xr_r = [alloc(f"xr{i}", [32, TB], BF16, ses) for i in range(2)]
                        rc, rc_b = alloc("rc", [128, TB], F32, ses)
                        acc_r = [alloc(f"accC{i}", [128, TB], F32, ses) for i in range(2)]
                        accb_r = [alloc(f"accCb{i}", [128, TB], BF16, ses) for i in range(2)]
                        DMAC(w_mla[:], w_in_d[l, :, 2560:2976].rearrange("(kc p) n -> p kc n", p=128), (), [w_mla_b])
                        DMAC(w_uq[:], w_uq_d[l].rearrange("(kc p) n -> p kc n", p=128), (), [w_uq_b])
                        DMAC(w_ukv[:], w_ukv_d[l], (), [w_ukv_b])
                        sc_c = float(96 ** -0.5)
                        for tb in range(NB):
                            blk = slice(tb * TB, (tb + 1) * TB)
                            self.psg_banks = ALLB
                            make_h(tb, A1, B1, hbf, hbf_b, ses, l)
                            DMA(h_scr[tb], hbf_flat, [hbf_b], [h_scr_b[tb]])
                            if l == 0 and tb == 0:
                                self.dump("h0", hbf[:], [hbf_b])
                            sstop(1)
                            for j in range(2):
                                ps, pb = psg()
                                for kc in range(8):
                                    MM(ps[:], w_mla[:, kc, j * 128:(j + 1) * 128], hbf[:, kc, :], kc == 0, kc == 7,
                                       [w_mla_b, hbf_b], [pb])
                                kvar = int(os.environ.get("K_VAR", 0))
                                if kvar in (0, 2):
                                    ACT(cqs[:, j, :], ps[:], AF.Square, [pb], [cqs_b])
                                if kvar in (0, 3):
                                    CP(cqf[:, j, :], ps[:], [pb], [cqf_b])
                            sstop(1.2)
                            ps, pb = psg()
                            for j in range(2):
                                MM(ps[:], ones_bf[:], cqs[:, j, :], j == 0, j == 1, [ones_b, cqs_b], [pb])
                            r, r_b = self.n_r.next()
                            self.rstd(r[:], ps[:], 1.0 / 256, [pb, cst_b], [r_b])
                            for j in range(2):
                                STT(cqn[:, j, :], cqf[:, j, :], latg[:, l, j:j + 1], r[:], ALU.mult, ALU.mult,
                                    [cqf_b, r_b, latg_b], [cqn_b])
                            sstop(1.5)

                            def ch_ckv(ln):
                                ps, pb = lane_ps(ln)
                                for kc in range(8):
                                    MM(ps[:], w_mla[:, kc, 256:384], hbf[:, kc, :], kc == 0, kc == 7, [w_mla_b, hbf_b], [pb])
                                yield
                                yield from g_group_norm(ln, ps[:], pb, 128, ones_bf[:], 1.0 / 128, latg[:, l, 2:3], ckvn[:], [ckvn_b], [latg_b])

                            def ch_kr(ln):
                                ps, pb = lane_ps(ln)
                                for kc in range(8):
                                    MM(ps[0:32, :], w_mla[:, kc, 384:416], hbf[:, kc, :], kc == 0, kc == 7, [w_mla_b, hbf_b], [pb])
                                yield
                                x, x_b = xr_r[lanes.index(ln)]
                                yield from g_group_norm(ln, ps[0:32, :], pb, 32, ones_bf[0:32, 0:32], 1.0 / 32, qkgCr[:, l, 1:2], x[:], [x_b], [qkgCr_b])
                                yield from g_rope(ln, x[:], x_b, blk, Kr[:, blk], [Kr_b])

                            def ch_qnope(j):
                                def f(ln):
                                    ps, pb = lane_ps(ln)
                                    for kc in range(2):
                                        MM(ps[:], w_uq[:, kc, j * 128:(j + 1) * 128], cqn[:, kc, :], kc == 0, kc == 1, [w_uq_b, cqn_b], [pb])
                                    yield
                                    yield from g_group_norm(ln, ps[:], pb, 128, bones[:], 1.0 / 64, qkgCn[:, l, 0:1], qn[:, j, :], [qn_b], [qkgCn_b])
                                return f

                            def ch_qrope(h):
                                def f(ln):
                                    ps, pb = lane_ps(ln)
                                    for kc in range(2):
                                        MM(ps[0:32, :], w_uq[:, kc, 512 + h * 32:512 + (h + 1) * 32], cqn[:, kc, :], kc == 0, kc == 1, [w_uq_b, cqn_b], [pb])
                                    yield
                                    x, x_b = xr_r[lanes.index(ln)]
                                    yield from g_group_norm(ln, ps[0:32, :], pb, 32, ones_bf[0:32, 0:32], 1.0 / 32, qkgCr[:, l, 0:1], x[:], [x_b], [qkgCr_b])
                                    yield from g_rope(ln, x[:], x_b, blk, qr[:, h, :], [qr_b])
                                return f

                            def ch_knope(j):
                                def f(ln):
                                    ps, pb = lane_ps(ln)
                                    MM(ps[:], w_ukv[:, j * 128:(j + 1) * 128], ckvn[:], True, True, [w_ukv_b, ckvn_b], [pb])
                                    yield
                                    yield from g_group_norm(ln, ps[:], pb, 128, bones[:], 1.0 / 64, qkgCn[:, l, 1:2], Kn[:, j, blk], [Kn_b], [qkgCn_b])
                                return f

                            def ch_v(tt):
                                def f(ln):
                                    ps, pb = lane_ps(ln)
                                    MM(ps[:], ckvn[:, tt * 128:(tt + 1) * 128], w_ukv[:, 512:1024], True, True, [ckvn_b, w_ukv_b], [pb])
                                    yield
                                    CP(Vc[:, tb * 4 + tt, :], ps[:], [pb], [Vc_b])
                                    yield
                                return f

                            run_chains([ch_ckv, ch_kr])
                            sstop(3)
                            run_chains([ch_qnope(j) for j in range(4)] + [ch_qrope(h) for h in range(8)]
                                       + [ch_knope(j) for j in range(4)] + [ch_v(tt) for tt in range(4)])
                            if l == 0 and tb == 0:
                                self.dump("qn0", qn[:], [qn_b])
                                self.dump("qr0", qr[:], [qr_b])
                                self.dump("Kr0", Kr[:, 0:TB], [Kr_b])
                                self.dump("Kn0", Kn[:, :, 0:TB], [Kn_b])
                                self.dump("Vc0", Vc[:, 0:4, :], [Vc_b])
                            sstop(4)
                            self.psg_banks = [6, 7]
                            nkt = 4 * (tb + 1)
                            steps = [(j, hh, kt) for j in range(4) for hh in range(2) for kt in range(nkt)]
                            sring = [0]

                            def emit_S(st, i):
                                j, hh, kt = st
                                h = 2 * j + hh
                                rows = slice(hh * 64, (hh + 1) * 64)
                                j0 = max(0, kt - 4 * tb)
                                cols = slice(j0 * 128, TB)
                                Sps, Spb = PS[i % 2], PB[i % 2]
                                MM(Sps[:, cols], Kn[rows, j, kt * 128:(kt + 1) * 128], qn[rows, j, cols], True, False,
                                   [Kn_b, qn_b], [Spb], inc=False)
                                MM(Sps[:, cols], Kr[:, kt * 128:(kt + 1) * 128], qr[:, h, cols], False, True,
                                   [Kr_b, qr_b], [Spb])

                            def emit_rest(st, i):
                                j, hh, kt = st
                                h = 2 * j + hh
                                rows = slice(hh * 64, (hh + 1) * 64)
                                j0 = max(0, kt - 4 * tb)
                                cols = slice(j0 * 128, TB)
                                Sps, Spb = PS[i % 2], PB[i % 2]
                                Ops, Opb = PS[2 + j % 2], PB[2 + j % 2]
                                Sms, Smb = PS[4 + j % 2], PB[4 + j % 2]
                                P, P_b = Pr.next()
                                ACT(P[:, cols], Sps[:, cols], AF.Exp, [Spb], [P_b], scale=sc_c)
                                if kt >= 4 * tb:
                                    MSET(P[64:128, j0 * 128:j0 * 128 + 64], 0.0, [P_b])
                                first = (kt == 0)
                                last = (kt == nkt - 1)
                                MM(Ops[rows, cols], Vc[:, kt, h * 64:(h + 1) * 64], P[:, cols], first, last, [Vc_b, P_b], [Opb],
                                   inc=True)
                                acc, acc_b = acc_r[h % 2]
                                if first:
                                    CP(acc[:, cols], P[:, cols], [P_b], [acc_b])
                                else:
                                    TT(acc[:, cols], acc[:, cols], P[:, cols], ALU.add, [acc_b, P_b], [acc_b])
                                if last:
                                    def fin(acc=acc, acc_b=acc_b, h=h, hh=hh, j=j, rows=rows, Ops=Ops, Opb=Opb, Sms=Sms, Smb=Smb):
                                        accb, accb_b = accb_r[h % 2]
                                        CP(accb[:], acc[:], [acc_b], [accb_b])
                                        MM(Sms[rows, :], ones_bf[:, 0:64], accb[:], True, True, [ones_b, accb_b], [Smb], inc=True)
                                        if hh == 1:
                                            ACT(rc[:], Sms[:], AF.Ln, [Smb], [rc_b])
                                            ACT(rc[:], rc[:], AF.Exp, [rc_b], [rc_b], scale=-1.0)
                                            TT(o_c[:, j, blk], Ops[:], rc[:], ALU.mult, [Opb, rc_b], [o_c_b])
                                    pending.append((i + 2, fin))

                            pending = []
                            emit_S(steps[0], 0)
                            for i, st in enumerate(steps):
                                if i + 1 < len(steps):
                                    emit_S(steps[i + 1], i + 1)
                                emit_rest(st, i)
                                while pending and pending[0][0] <= i:
                                    pending.pop(0)[1]()
                            while pending:
                                pending.pop(0)[1]()
                        self.dump("o_c", o_c[:], [o_c_b])
                        fw.barrier()
                    stop_at(1)

                    o_a, o_a_b = alloc("o_a", [128, 4, S], BF16, mes)
                    with ExitStack() as ses:
                        wa_r = [alloc(f"w_a{i}", [128, 8, 512], BF16, ses) for i in range(2)]
                        wa_i = [0]
                        Ka, Ka_b = alloc("Ka", [128, 4, S], BF16, ses)
                        Va, Va_b = alloc("Va", [128, 16, 512], BF16, ses)
                        Qa, Qa_b = alloc("Qa", [128, 4, TB], BF16, ses)
                        rc, rc_b = alloc("rca", [128, TB], F32, ses)
                        accA_r = [alloc(f"accA{i}", [128, TB], F32, ses) for i in range(2)]
                        accAb, accAb_b = alloc("accAb", [128, TB], BF16, ses)
                        o0, o0_b = alloc("o0", [128, TB], F32, ses)
                        o1, o1_b = alloc("o1", [128, TB], F32, ses)
                        dd, dd_b = alloc("dd", [128, TB], F32, ses)
                        sc_a = 0.125
                        for tb in range(NB):
                            blk = slice(tb * TB, (tb + 1) * TB)
                            self.psg_banks = ALLB
                            if tb == 0:
                                DMA(hbf_flat, h_scr[0], [h_scr_b[0]], [hbf_b])
                            wqkv = []
                            for q in range(3):
                                w_a, w_a_b = wa_r[wa_i[0] % 2]; wa_i[0] += 1
                                DMAC(w_a[:], w_in_d[l, :, q * 512:(q + 1) * 512].rearrange("(kc p) n -> p kc n", p=128), (), [w_a_b])
                                if q <= 1:
                                    def ch_qk(hd, q=q, w_a=w_a, w_a_b=w_a_b):
                                        def f(ln):
                                            ps, pb = lane_ps(ln)
                                            for kc in range(8):
                                                MM(ps[:], w_a[:, kc, hd * 128:(hd + 1) * 128], hbf[:, kc, :], kc == 0, kc == 7, [w_a_b, hbf_b], [pb])
                                            yield
                                            if q == 0:
                                                yield from g_group_norm(ln, ps[:], pb, 128, bones[:], 1.0 / 64, qkgA[:, l, 0:1], Qa[:, hd, :], [Qa_b], [qkgA_b])
                                            else:
                                                yield from g_group_norm(ln, ps[:], pb, 128, bones[:], 1.0 / 64, qkgA[:, l, 1:2], Ka[:, hd, blk], [Ka_b], [qkgA_b])
                                        return f
                                    run_chains([ch_qk(hd) for hd in range(4)])
                                else:
                                    for tt in range(4):
                                        ps, pb = psg()
                                        for kc in range(8):
                                            MM(ps[:], hbf[:, kc, tt * 128:(tt + 1) * 128], w_a[:, kc, :], kc == 0, kc == 7, [hbf_b, w_a_b], [pb])
                                        CP(Va[:, tb * 4 + tt, :], ps[:], [pb], [Va_b])
                            if l == 0 and tb == 0:
                                self.dump("Qa0", Qa[:], [Qa_b])
                                self.dump("Va0", Va[:, 0:4, :], [Va_b])
                            if tb + 1 < NB:
                                DMA(hbf_flat, h_scr[tb + 1], [h_scr_b[tb + 1]], [hbf_b])
                            self.psg_banks = [7]
                            SB3 = [0, 1, 6]
                            nkt = 4 * (tb + 1)
                            steps = [(hd, m, kt) for hd in range(4) for m in range(2) for kt in range(nkt)]

                            def emit_S(st, i):
                                hd, m, kt = st
                                rows = slice(m * 64, (m + 1) * 64)
                                j0 = max(0, kt - 4 * tb)
                                cols = slice(j0 * 128, TB)
                                Sps, Spb = PS[SB3[i % 3]], PB[SB3[i % 3]]
                                MM(Sps[:, cols], Ka[rows, hd, kt * 128:(kt + 1) * 128], Qa[rows, hd, cols], True, True,
                                   [Ka_b, Qa_b], [Spb])

                            def emit_rest(st, i):
                                hd, m, kt = st
                                sidx = hd * 2 + m
                                j0 = max(0, kt - 4 * tb)
                                cols = slice(j0 * 128, TB)
                                Sps, Spb = PS[SB3[i % 3]], PB[SB3[i % 3]]
                                Ops, Opb = PS[2 + sidx % 2], PB[2 + sidx % 2]
                                Sms, Smb = PS[4 + sidx % 2], PB[4 + sidx % 2]
                                P, P_b = Pr.next()
                                jq_far0 = max(j0, kt - 4 * tb + 2)
                                ACT(P[:, cols], Sps[:, cols], AF.Exp, [Spb, cfar_b], [P_b], bias=cfar[:, hd:hd + 1], scale=sc_a)
                                for jq in range(j0, min(4, jq_far0)):
                                    delta = kt - (4 * tb + jq)
                                    bi = (0 if delta == 0 else 1) * 4 + hd
                                    qc = slice(jq * 128, (jq + 1) * 128)
                                    TT(P[:, qc], P[:, qc], Eb[:, bi, :], ALU.mult, [P_b, Eb_b], [P_b], eng=self.pool)
                                first = (kt == 0)
                                last = (kt == nkt - 1)
                                MM(Ops[:, cols], Va[:, kt, hd * 128:(hd + 1) * 128], P[:, cols], first, last, [Va_b, P_b], [Opb],
                                   inc=True)
                                acc, acc_b = accA_r[sidx % 2]
                                if first:
                                    CP(acc[:, cols], P[:, cols], [P_b], [acc_b])
                                else:
                                    TT(acc[:, cols], acc[:, cols], P[:, cols], ALU.add, [acc_b, P_b], [acc_b])
                                if last:
                                    def fin(acc=acc, acc_b=acc_b, hd=hd, m=m, Ops=Ops, Opb=Opb, Sms=Sms, Smb=Smb):
                                        CP(accAb[:], acc[:], [acc_b], [accAb_b])
                                        MM(Sms[:], ones_bf[:], accAb[:], True, True, [ones_b, accAb_b], [Smb], inc=True)
                                        ACT(rc[:], Sms[:], AF.Ln, [Smb], [rc_b])
                                        ACT(rc[:], rc[:], AF.Exp, [rc_b], [rc_b], scale=-1.0)
                                        if m == 0:
                                            TT(o0[:], Ops[:], rc[:], ALU.mult, [Opb, rc_b], [o0_b])
                                        else:
                                            TT(o1[:], Ops[:], rc[:], ALU.mult, [Opb, rc_b], [o1_b])
                                            STT(dd[:], o1[:], neglam[:, l:l + 1], o0[:], ALU.mult, ALU.add, [o1_b, o0_b, neglam_b], [dd_b])
                                            sq, sq_b = self.n_sq.next()
                                            ACT(sq[:], dd[:], AF.Square, [dd_b], [sq_b])
                                            ps, pb = psg()
                                            MM(ps[:], ones_bf[:], sq[:], True, True, [ones_b, sq_b], [pb])
                                            r, r_b = self.n_r.next()
                                            self.rstd(r[:], ps[:], 1.0 / 128, [pb, cst_b], [r_b])
                                            STT(o_a[:, hd, blk], dd[:], gout[:, l:l + 1], r[:], ALU.mult, ALU.mult,
                                                [dd_b, r_b, gout_b], [o_a_b])
                                    pending.append((i + 2, fin))

                            pending = []
                            emit_S(steps[0], 0)
                            emit_S(steps[1], 1)
                            for i, st in enumerate(steps):
                                if i + 2 < len(steps):
                                    emit_S(steps[i + 2], i + 2)
                                emit_rest(st, i)
                                while pending and pending[0][0] <= i:
                                    pending.pop(0)[1]()
                            while pending:
                                pending.pop(0)[1]()
                        self.dump("o_a", o_a[:], [o_a_b])
                        fw.barrier()
                    stop_at(2)

                    with ExitStack() as ses:
                        w8_r = [alloc(f"w8_{i}", [128, 8, 512], BF16, ses) for i in range(2)]
                        wb_r = [alloc(f"wb{i}", [128, 4, 512], BF16, ses) for i in range(2)]
                        rings = {"w8": 0, "b": 0}
                        sgw_f, sgw_fb = alloc("sgw_f", [128, 4, 128], F32, ses)
                        sgw, sgw_b = alloc("sgw", [128, 4, 128], BF16, ses)
                        sgb, sgb_b = alloc("sgb", [128, 4, 128], F32, ses)
                        vg, vg_b = alloc("vg", [128, 512], F32, ses)
                        uT, uT_b = alloc("uT", [128, 4, TB], BF16, ses)
                        o_b, o_b_b = alloc("o_b", [128, 4, TB], BF16, ses)
                        vf, vf_b = alloc("vf", [128, 512], F32, ses)
                        vjunk, vjunk_b = alloc("vjunk", [128, 512], BF16, ses)
                        vss, vss_b = alloc("vss", [128, 2], F32, ses)
                        vn, vn_b = alloc("vn", [128, 512], BF16, ses)
                        vt, vt_b = alloc("vt", [128, 128], F32, ses)
                        gsb, gsb_b = alloc("gsb", [128, TB], F32, ses)
                        mt, mt_b = alloc("mt", [128, TB], F32, ses)
                        macc, macc_b = alloc("macc", [128, 4, TB], F32, ses)
                        merged, merged_b = alloc("merged", [128, 8, TB], BF16, ses)
                        DMA(sgw_f[:], sgu_wT_d[l], (), [sgw_fb])
                        DMA(sgb[:], sgu_bb_d[l], (), [sgb_b])
                        DMA(vg[:], sgu_vg_d[l], (), [vg_b])
                        MSET(sgw_f[64:128, :, 0:64], 0.0, [sgw_fb])
                        CP(sgw[:], sgw_f[:], [sgw_fb], [sgw_b])
                        self.psg_banks = ALLB
                        for tb in range(NB):
                            blk = slice(tb * TB, (tb + 1) * TB)
                            if tb == 0:
                                DMA(hbf_flat, h_scr[0], [h_scr_b[0]], [hbf_b])
                            wu, wu_b = w8_r[rings["w8"] % 2]; rings["w8"] += 1
                            DMAC(wu[:], w_in_d[l, :, 1536:2048].rearrange("(kc p) n -> p kc n", p=128), (), [wu_b])
                            wv, wv_b = w8_r[rings["w8"] % 2]; rings["w8"] += 1
                            DMAC(wv[:], w_in_d[l, :, 2048:2560].rearrange("(kc p) n -> p kc n", p=128), (), [wv_b])
                            for cu in range(4):
                                ps, pb = psg()
                                for kc in range(8):
                                    MM(ps[:], wu[:, kc, cu * 128:(cu + 1) * 128], hbf[:, kc, :], kc == 0, kc == 7, [wu_b, hbf_b], [pb])
                                ACT(uT[:, cu, :], ps[:], AF.Gelu_apprx_tanh, [pb], [uT_b])
                            for tt in range(4):
                                ps, pb = psg()
                                for kc in range(8):
                                    MM(ps[:], hbf[:, kc, tt * 128:(tt + 1) * 128], wv[:, kc, :], kc == 0, kc == 7, [hbf_b, wv_b], [pb])
                                ACT(vf[:], ps[:], AF.Gelu_apprx_tanh, [pb], [vf_b])
                                fw.op(self.act, lambda: nc.scalar.activation(out=vjunk[:], in_=vf[:], func=AF.Square,
                                                                             accum_out=vss[:, 0:1]),
                                      [vf_b], [vjunk_b, vss_b])
                                ACT(vss[:, 1:2], vss[:, 0:1], AF.Ln, [vss_b, cst_b], [vss_b], bias=cst[:, 0:1], scale=1.0 / 512)
                                ACT(vss[:, 1:2], vss[:, 1:2], AF.Exp, [vss_b], [vss_b], scale=-0.5)
                                STT(vn[:], vf[:], vss[:, 1:2], vg[:], ALU.mult, ALU.mult, [vf_b, vss_b, vg_b], [vn_b])
                                for g in range(4):
                                    ps2, pb2 = psg()
                                    MM(ps2[:, 0:128], vn[:, g * 128:(g + 1) * 128], sgw[:, g, :], True, True, [vn_b, sgw_b], [pb2])
                                    TT(vt[:], ps2[:, 0:128], sgb[:, g, :], ALU.add, [pb2, sgb_b], [vt_b])
                                    TT(o_b[:, g, tt * 128:(tt + 1) * 128], vt[:], uT[:, g, tt * 128:(tt + 1) * 128], ALU.mult,
                                       [vt_b, uT_b], [o_b_b])
                            if l == 0 and tb == 0:
                                self.dump("o_b0", o_b[:], [o_b_b])
                            for dcg in range(2):
                                for n in range(3):
                                    wg, wg_b = w8_r[rings["w8"] % 2]; rings["w8"] += 1
                                    c0 = 2976 + n * 1024 + dcg * 512
                                    DMAC(wg[:], w_in_d[l, :, c0:c0 + 512].rearrange("(kc p) n -> p kc n", p=128), (), [wg_b])
                                    wb, wb_b = wb_r[rings["b"] % 2]; rings["b"] += 1
                                    DMAC(wb[:], w_br_d[l, n, :, dcg * 512:(dcg + 1) * 512].rearrange("(kc p) n -> p kc n", p=128), (), [wb_b])
                                    src, src_b = [(o_a, o_a_b), (o_b, o_b_b), (o_c, o_c_b)][n]
                                    for dci in range(4):
                                        psa, pba = psg()
                                        for kc in range(8):
                                            MM(psa[:], wg[:, kc, dci * 128:(dci + 1) * 128], hbf[:, kc, :], kc == 0, kc == 7, [wg_b, hbf_b], [pba])
                                        ACT(gsb[:], psa[:], AF.Sigmoid, [pba], [gsb_b])
                                        psb, pbb = psg()
                                        for kc in range(4):
                                            rhs = src[:, kc, :] if n == 1 else src[:, kc, blk]
                                            MM(psb[:], wb[:, kc, dci * 128:(dci + 1) * 128], rhs, kc == 0, kc == 3, [wb_b, src_b], [pbb])
                                        if n == 0:
                                            TT(macc[:, dci, :], psb[:], gsb[:], ALU.mult, [pbb, gsb_b], [macc_b])
                                        else:
                                            TT(mt[:], psb[:], gsb[:], ALU.mult, [pbb, gsb_b], [mt_b])
                                            if n == 1:
                                                TT(macc[:, dci, :], macc[:, dci, :], mt[:], ALU.add, [macc_b, mt_b], [macc_b])
                                            else:
                                                TT(merged[:, dcg * 4 + dci, :], macc[:, dci, :], mt[:], ALU.add, [macc_b, mt_b], [merged_b])
                            if l == 0 and tb == 0:
                                self.dump("merged0", merged[:], [merged_b])
                            if tb + 1 < NB:
                                DMA(hbf_flat, h_scr[tb + 1], [h_scr_b[tb + 1]], [hbf_b])
                            for dcg in range(2):
                                wo, wo_b = w8_r[rings["w8"] % 2]; rings["w8"] += 1
                                DMAC(wo[:], w_out_d[l, :, dcg * 512:(dcg + 1) * 512].rearrange("(kc p) n -> p kc n", p=128), (), [wo_b])
                                for dci in range(4):
                                    dc = dcg * 4 + dci
                                    ps, pb = psg()
                                    for kc in range(8):
                                        MM(ps[:], wo[:, kc, dci * 128:(dci + 1) * 128], merged[:, kc, :], kc == 0, kc == 7, [wo_b, merged_b], [pb])
                                    STT(xT[:, dc, blk], ps[:], MOD[:, l, 16 + dc:16 + dc + 1], xT[:, dc, blk], ALU.mult, ALU.add,
                                        [pb, MOD_b, xTB[tb]], [xTB[tb]])
                        fw.barrier()
                    if l == 0:
                        self.dump("x_mid", xT[:], xTB)
                    stop_at(3)

                    mes.close()
                    with ExitStack() as ses:
                        h2, h2_b = alloc("h2", [128, 8, S], BF16, ses)
                        rs = ExitStack()
                        h2f, h2f_b = alloc("h2f", [128, 8, TB], F32, rs)
                        combT, combT_b = alloc("combT", [32, S], F32, rs)
                        w_r, w_r_b = alloc("w_r", [128, 8, 36], F32, rs)
                        DMA(w_r[:], w_r_d[l].rearrange("(kc p) n -> p kc n", p=128), (), [w_r_b])
                        NT = 16
                        lgA, lg_b = alloc("lgA", [128, NT, 36], F32, rs)
                        ohg, ohg_b = alloc("ohg", [128, NT, 4], F32, rs)
                        ex4, ex4_b = alloc("ex4", [128, NT, 4], F32, rs)
                        v1, v1_b = alloc("v1", [128, 8, NT], F32, rs)
                        el3, el3_b = alloc("el3", [128, NT, 32], F32, rs)
                        els, els_b = alloc("els", [128, NT, 8], F32, rs)
                        els2, els2_b = alloc("els2", [128, NT, 8], F32, rs)
                        oh1, oh1_b = alloc("oh1", [128, NT, 8], F32, rs)
                        oh2, oh2_b = alloc("oh2", [128, NT, 8], F32, rs)
                        comb, comb_b = alloc("comb", [128, NT, 32], F32, rs)
                        h2B = [Buf(f"h2_{b}") for b in range(NB)]
                        self.psg_banks = ALLB
                        for tb in range(NB):
                            blk = slice(tb * TB, (tb + 1) * TB)
                            make_h(tb, A2, B2, h2[:, :, blk], h2B[tb], ses, l, hf=h2f, hf_b=h2f_b)
                            for tt in range(4):
                                ps, pb = psg()
                                for kc in range(8):
                                    MM(ps[:, 0:36], h2f[:, kc, tt * 128:(tt + 1) * 128], w_r[:, kc, :], kc == 0, kc == 7, [h2f_b, w_r_b], [pb])
                                TT(lgA[:, tb * 4 + tt, :], ps[:, 0:36], b_r[:, l, :], ALU.add, [pb, b_r_b], [lg_b])
                        def red(out, in_, op, rd, wr):
                            fw.op(self.dve, lambda: nc.vector.tensor_reduce(out=out, in_=in_, axis=AX.X, op=op), rd, wr)
                        gl = lgA[:, :, 0:4]
                        el4 = lgA[:, :, 4:36].rearrange("p t (g e) -> p t g e", g=4)
                        gmax, gsum, g_w, m1, m2, e2, w1, w2 = [v1[:, i, :] for i in range(8)]
                        bc4 = lambda a: a.unsqueeze(2).broadcast_to([128, NT, 4])
                        bc8 = lambda a: a.unsqueeze(2).broadcast_to([128, NT, 8])
                        red(gmax, gl, ALU.max, [lg_b], [v1_b])
                        TT(ohg[:], gl, bc4(gmax), ALU.is_equal, [lg_b, v1_b], [ohg_b])
                        TT(ex4[:], gl, bc4(gmax), ALU.subtract, [lg_b, v1_b], [ex4_b])
                        ACT(ex4[:], ex4[:], AF.Exp, [ex4_b], [ex4_b])
                        red(gsum, ex4[:], ALU.add, [ex4_b], [v1_b])
                        RECIP(g_w, gsum, [v1_b], [v1_b])
                        TT(el3[:].rearrange("p t (g e) -> p t g e", g=4), el4,
                           ohg[:].unsqueeze(3).broadcast_to([128, NT, 4, 8]), ALU.mult, [lg_b, ohg_b], [el3_b])
                        red(els[:], el3[:].rearrange("p t (g e) -> p t e g", g=4), ALU.add, [el3_b], [els_b])
                        red(m1, els[:], ALU.max, [els_b], [v1_b])
                        TT(oh1[:], els[:], bc8(m1), ALU.is_equal, [els_b, v1_b], [oh1_b])
                        STT(els2[:], oh1[:], -1.0e30, els[:], ALU.mult, ALU.add, [oh1_b, els_b], [els2_b])
                        red(m2, els2[:], ALU.max, [els2_b], [v1_b])
                        TT(oh2[:], els2[:], bc8(m2), ALU.is_equal, [els2_b, v1_b], [oh2_b])
                        TT(e2, m2, m1, ALU.subtract, [v1_b], [v1_b])
                        ACT(e2, e2, AF.Exp, [v1_b], [v1_b])
                        TS(w1, e2, 1.0, None, ALU.add, None, [v1_b], [v1_b])
                        RECIP(w1, w1, [v1_b], [v1_b])
                        TT(w2, e2, w1, ALU.mult, [v1_b], [v1_b])
                        TT(w1, w1, g_w, ALU.mult, [v1_b], [v1_b])
                        TT(w2, w2, g_w, ALU.mult, [v1_b], [v1_b])
                        TT(oh1[:], oh1[:], bc8(w1), ALU.mult, [oh1_b, v1_b], [oh1_b])
                        TT(oh2[:], oh2[:], bc8(w2), ALU.mult, [oh2_b, v1_b], [oh2_b])
                        TT(oh1[:], oh1[:], oh2[:], ALU.add, [oh1_b, oh2_b], [oh1_b])
                        TT(comb[:].rearrange("p t (g e) -> p t g e", g=4), ohg[:].unsqueeze(3).broadcast_to([128, NT, 4, 8]),
                           oh1[:].unsqueeze(2).broadcast_to([128, NT, 4, 8]), ALU.mult, [ohg_b, oh1_b], [comb_b])
                        for tb in range(NB):
                            ps2, pb2 = psg()
                            for tt in range(4):
                                fw.op(self.pe, lambda tt=tt: nc.tensor.transpose(ps2[0:32, tt * 128:(tt + 1) * 128], comb[:, tb * 4 + tt, :], ident[:]),
                                      [comb_b, ident_b], [pb2], inc=(tt == 3))
                            CP(combT[:, tb * TB:(tb + 1) * TB], ps2[0:32, :], [pb2], [combT_b])
                        self.dump("combT", combT[:], [combT_b])
                        if l == 0:
                            self.dump("h2", h2[:], h2B)
                        DMA(comb_scr, combT[:], [combT_b], [comb_scr_b])
                        fw.barrier()
                        rs.close()
                        stop_at(4)
                        self.psg_banks = [4, 5, 6, 7]
                        EE = 2
                        wgu_r = [alloc(f"wgu{i}", [128, 2, 8, 256], BF16, ses) for i in range(3)]
                        wd_r = [alloc(f"wd{i}", [128, 2, D], BF16, ses) for i in range(4)]
                        actT, actT_b0 = alloc("actT", [128, EE, 2, S], BF16, ses)
                        actB = [[Buf(f"act{e}_{b}") for b in range(NB)] for e in range(EE)]
                        sil_r = [alloc(f"sil{i}", [128, TB], BF16, ses) for i in range(2)]
                        ut_r = [alloc(f"ut{i}", [128, TB], BF16, ses) for i in range(2)]
                        cb_r = [alloc(f"cb{i}", [128, TB], F32, ses) for i in range(3)]
                        cnt = {"gu": 0, "d": 0, "s": 0, "cb": 0, "ps": 0}
                        for e0 in range(0, 32, EE):
                            wds = []
                            for ei in range(EE):
                                e = e0 + ei
                                wgu, wgu_b = wgu_r[cnt["gu"] % 3]; cnt["gu"] += 1
                                DMAC(wgu[:, 0, :, :], weg_d[l, e].rearrange("(kc p) n -> p kc n", p=128), (), [wgu_b])
                                DMAC(wgu[:, 1, :, :], weu_d[l, e].rearrange("(kc p) n -> p kc n", p=128), (), [wgu_b])
                                wd, wd_b = wd_r[cnt["d"] % 4]; cnt["d"] += 1
                                DMAC(wd[:], wed_d[l, e].rearrange("(fc p) n -> p fc n", p=128), (), [wd_b])
                                wds.append((wd, wd_b))
                                for tb in range(NB):
                                    blk = slice(tb * TB, (tb + 1) * TB)
                                    cb, cb_b = cb_r[cnt["cb"] % 3]; cnt["cb"] += 1
                                    DMA(cb[:], comb_scr[e:e + 1, blk].partition_broadcast(128), [comb_scr_b], [cb_b])
                                    for fc in range(2):
                                        k = cnt["ps"]; cnt["ps"] += 1
                                        aps, apb = PS[k % 2], PB[k % 2]
                                        ups, upb = PS[2 + k % 2], PB[2 + k % 2]
                                        for kc in range(8):
                                            MM(aps[:], wgu[:, 0, kc, fc * 128:(fc + 1) * 128], h2[:, kc, blk], kc == 0, kc == 7, [wgu_b, h2B[tb]], [apb])
                                        for kc in range(8):
                                            MM(ups[:], wgu[:, 1, kc, fc * 128:(fc + 1) * 128], h2[:, kc, blk], kc == 0, kc == 7, [wgu_b, h2B[tb]], [upb])
                                        sil, sil_b = sil_r[k % 2]
                                        ut, ut_b = ut_r[k % 2]
                                        ACT(sil[:], aps[:], AF.Silu, [apb], [sil_b])
                                        TT(ut[:], ups[:], cb[:], ALU.mult, [upb, cb_b], [ut_b])
                                        TT(actT[:, ei, fc, blk], sil[:], ut[:], ALU.mult, [sil_b, ut_b], [actB[ei][tb]], eng=self.pool)
                            for tb in range(NB):
                                blk = slice(tb * TB, (tb + 1) * TB)
                                for dc in range(8):
                                    ps, pb = psg()
                                    n_mm = EE * 2
                                    i_mm = 0
                                    for ei in range(EE):
                                        wd, wd_b = wds[ei]
                                        for fc in range(2):
                                            MM(ps[:], wd[:, fc, dc * 128:(dc + 1) * 128], actT[:, ei, fc, blk], i_mm == 0, i_mm == n_mm - 1,
                                               [wd_b, actB[ei][tb]], [pb])
                                            i_mm += 1
                                    STT(xT[:, dc, blk], ps[:], MOD[:, l, 40 + dc:40 + dc + 1], xT[:, dc, blk], ALU.mult, ALU.add,
                                        [pb, MOD_b, xTB[tb]], [xTB[tb]])
                        fw.barrier()

            emit_output()
            self.ninstr = fw.ninstr
        return nc


def _t5_bucket(rel):
    nb = 16
    max_exact = 8
    n = np.abs(rel)
    large = max_exact + (np.log(np.maximum(n, 1).astype(np.float32) / max_exact)
                         / math.log(128 / max_exact) * (nb - max_exact)).astype(np.int32)
    large = np.minimum(large, nb - 1)
    return np.where(rel > 0, nb, 0) + np.where(n < max_exact, n, large)


def _bucket_table():
    rel = np.arange(-300, 300)
    nb = 16
    max_exact = 8
    n = np.abs(rel)
    ratio = (np.maximum(n, 1).astype(np.float32) / np.float32(max_exact)).astype(np.float32)
    large = max_exact + (np.log(ratio).astype(np.float32) / np.float32(math.log(128 / max_exact))
                         * np.float32(nb - max_exact)).astype(np.int32)
    large = np.minimum(large, nb - 1)
    return np.where(rel > 0, nb, 0) + np.where(n < max_exact, n, large)


def prep_shared(inp, depth):
    f = np.float32
    g = {}
    g["w_ada"] = np.ascontiguousarray(inp["w_ada"], f)
    g["b_adaT"] = np.ascontiguousarray(inp["b_ada"].reshape(DEPTH, 48, 128).transpose(2, 0, 1), f)
    g["norm_gT"] = np.ascontiguousarray(inp["norm_g"].reshape(DEPTH, 2, 8, 128).transpose(3, 0, 1, 2), f)
    g["w_in"] = np.ascontiguousarray(inp["w_in"], f)
    qk = inp["diff_qk_g"]
    g["qkgA"] = np.ascontiguousarray(np.tile(qk.transpose(2, 0, 1), (2, 1, 1)), f)
    g["dlam"] = np.ascontiguousarray(np.broadcast_to(inp["diff_lambda"].reshape(1, DEPTH, 256), (128, DEPTH, 256)), f)
    g["goutA"] = np.ascontiguousarray(inp["diff_out_g"].T, f)
    bt = _bucket_table()
    ki = np.arange(128)[:, None]
    qi = np.arange(128)[None, :]
    tab = inp["rel_bias"]
    tiles = np.zeros((128, 8, 128), f)
    for di, delta in enumerate((0, -1)):
        rel = ki + 128 * delta - qi
        bidx = bt[rel + 300]
        for h in range(4):
            t = tab[bidx, h]
            if delta == 0:
                allowed = (ki // 64) <= (qi // 64)
                t = np.where(allowed, t, f(-30000.0))
            tiles[:, di * 4 + h, :] = t
    g["biasA"] = tiles
    g["cfar"] = np.ascontiguousarray(np.broadcast_to(tab[15][None, :], (128, 4)), f)
    g["sgu_vg"] = np.ascontiguousarray(np.broadcast_to(inp["sgu_v_g"][:, None, :], (DEPTH, 128, 512)), f)
    g["sgu_wT"] = np.ascontiguousarray(inp["sgu_w"].transpose(0, 3, 1, 2), f)
    g["sgu_bb"] = np.ascontiguousarray(np.broadcast_to(inp["sgu_b"][:, None, :, :], (DEPTH, 128, 4, 128)), f)
    lg = inp["mla_lat_g"]
    g["latg"] = np.ascontiguousarray(lg.reshape(DEPTH, 3, 128).transpose(2, 0, 1), f)
    wuq = inp["mla_w_uq"].reshape(DEPTH, 256, 8, 96)
    g["w_uq"] = np.ascontiguousarray(np.concatenate([wuq[..., :64].reshape(DEPTH, 256, 512),
                                                     wuq[..., 64:].reshape(DEPTH, 256, 256)], axis=-1), f)
    wukv = inp["mla_w_ukv"].reshape(DEPTH, 128, 8, 128)
    g["w_ukv"] = np.ascontiguousarray(np.concatenate([wukv[..., :64].reshape(DEPTH, 128, 512),
                                                      wukv[..., 64:].reshape(DEPTH, 128, 512)], axis=-1), f)
    qkc = inp["mla_qk_g"]
    g["qkgCn"] = np.ascontiguousarray(np.tile(qkc[:, :, :64].transpose(2, 0, 1), (2, 1, 1)), f)
    g["qkgCr"] = np.ascontiguousarray(qkc[:, :, 64:].transpose(2, 0, 1), f)
    g["w_branch"] = np.ascontiguousarray(inp["w_branch"], f)
    g["w_out"] = np.ascontiguousarray(inp["w_out"], f)
    g["w_r"] = np.ascontiguousarray(np.concatenate([inp["router_g_w"], inp["router_e_w"]], axis=-1), f)
    br = np.concatenate([inp["router_g_b"], inp["router_e_b"]], axis=-1)
    g["b_r"] = np.ascontiguousarray(np.broadcast_to(br[None], (128, DEPTH, 36)), f)
    g["w_e_gate"] = np.ascontiguousarray(inp["w_e_gate"].reshape(DEPTH, 32, D, 256), f)
    g["w_e_up"] = np.ascontiguousarray(inp["w_e_up"].reshape(DEPTH, 32, D, 256), f)
    g["w_e_down"] = np.ascontiguousarray(inp["w_e_down"].reshape(DEPTH, 32, 256, D), f)
    inv_freq = (10000.0 ** (-np.arange(0, 32, 2, dtype=np.float32) / np.float32(32))).astype(f)
    g["invf"] = np.ascontiguousarray(np.concatenate([inv_freq, inv_freq])[:, None], f)
    R = np.zeros((32, 32), f)
    for i in range(16):
        R[i + 16, i] = -1.0
        R[i, i + 16] = 1.0
    g["R32T"] = R
    g["ident"] = np.eye(128, dtype=f)
    return g


def prep_core(inp, b):
    f = np.float32
    m = {}
    m["xT"] = np.ascontiguousarray(np.asarray(inp["x"][b], f).T)
    m["cT"] = np.ascontiguousarray(np.asarray(inp["c"][b], f).reshape(8, 128).T)
    m["pos32"] = np.ascontiguousarray(np.broadcast_to(np.asarray(inp["positions"][b], np.int32)[None, :], (32, S)))
    return m


_CACHE = {}


def kernel(**inputs):
    inp = {k: np.asarray(v) for k, v in inputs.items()}
    depth = int(os.environ.get("K_DEPTH", DEPTH))
    if depth not in _CACHE:
        kb = K(depth)
        _CACHE[depth] = kb.build()
    nc = _CACHE[depth]
    shared = prep_shared(inp, depth)
    in_maps = []
    for b in range(8):
        m = dict(shared)
        m.update(prep_core(inp, b))
        in_maps.append(m)
    res = run_bass_kernel_spmd(nc, in_maps, core_ids=list(range(8)))
    out = np.stack([np.asarray(r["outT"], np.float32).T for r in res.results], axis=0)
    return np.ascontiguousarray(out)
```

```python
import math
import os
from contextlib import ExitStack

import numpy as np
import concourse.bass as bass
import concourse.mybir as mybir
from concourse.bass_utils import run_bass_kernel_spmd

F32 = mybir.dt.float32
BF16 = mybir.dt.bfloat16
I32 = mybir.dt.int32
AF = mybir.ActivationFunctionType
ALU = mybir.AluOpType
AX = mybir.AxisListType

DEPTH = 4
S = 2048
D = 1024
NB = 4
TB = 512
EPS = 1e-6
IN_W = 6048
LAMBDA_INIT = [0.8 - 0.6 * math.exp(-0.3 * l) for l in range(DEPTH)]


class Eng:
    def __init__(self, name, handle, sem, step, issuer=None):
        self.name = name
        self.h = handle
        self.sem = sem
        self.step = step
        self.count = 0
        self.issuer = issuer or self
        self.waited = {}


class Buf:
    __slots__ = ("name", "w", "r", "excl")

    def __init__(self, name="", excl=False):
        self.name = name
        self.w = None
        self.r = []
        self.excl = excl


class FW:
    def __init__(self, nc, es):
        self.nc = nc
        self.es = es
        self.engs = {}
        self.ninstr = 0
        self.disabled = False

    def add_engine(self, name, handle, step=1, issuer=None):
        sem = self.es.enter_context(self.nc.semaphore("s_" + name))
        e = Eng(name, handle, sem, step, issuer)
        self.engs[name] = e
        return e

    def _wait(self, eng, dep):
        e2, cnt = dep
        iss = eng.issuer
        if iss.waited.get(e2.name, 0) >= cnt:
            return
        iss.h.wait_ge(e2.sem, cnt * e2.step)
        iss.waited[e2.name] = cnt
        self.ninstr += 1

    def op(self, eng, fn, reads=(), writes=(), inc=True):
        if self.disabled:
            return None
        iss = eng.issuer
        for b in reads:
            if b.excl:
                for r in b.r:
                    if r[0].issuer is not iss:
                        self._wait(eng, r)
            if b.w is not None:
                if b.w[0] is iss and iss.name == "pe":
                    continue
                self._wait(eng, b.w)
        for b in writes:
            if b.w is not None and (b.w[0].issuer is not iss or b.w[0].step == 16):
                self._wait(eng, b.w)
            for r in b.r:
                if r[0] is iss and iss.step == 1 and eng.step == 1:
                    continue
                self._wait(eng, r)
        ins = fn()
        self.ninstr += 1
        if inc:
            eng.count += 1
            ins.then_inc(eng.sem, eng.step)
            tag = (eng, eng.count)
        else:
            tag = (eng, eng.count + 1)
        for b in reads:
            b.r.append(tag)
            if len(b.r) > 48:
                best = {}
                for (e, c) in b.r:
                    if e.name not in best or best[e.name][1] < c:
                        best[e.name] = (e, c)
                b.r = list(best.values())
        for b in writes:
            b.w = tag
            b.r = []
        return ins

    def barrier(self):
        if self.disabled:
            return
        issuers = {}
        for e in self.engs.values():
            issuers[e.issuer.name] = e.issuer
        for iss in issuers.values():
            for e2 in self.engs.values():
                if e2 is iss or e2.count == 0:
                    continue
                if iss.waited.get(e2.name, 0) >= e2.count:
                    continue
                iss.h.wait_ge(e2.sem, e2.count * e2.step)
                iss.waited[e2.name] = e2.count
                self.ninstr += 1


class Ring:
    def __init__(self, K, name, shape, dt, n, psum=False):
        self.items = []
        for i in range(n):
            self.items.append(K.alloc(f"{name}{i}", shape, dt))
        self.i = 0

    def next(self):
        it = self.items[self.i % len(self.items)]
        self.i += 1
        return it


class StopBuild(Exception):
    pass


class K:
    def __init__(self, depth, dbg=None):
        self.stop = int(os.environ.get("K_STOP", 99))
        self.depth = depth
        self.dbg = dbg or {}
        self.nc = bass.Bass("TRN2", target_bir_lowering=False)
        self.dram = {}
        self.dbg_out = {}

    def din(self, name, shape, dt=F32):
        self.dram[name] = self.nc.dram_tensor(name, list(shape), dt, kind="ExternalInput").ap()
        return self.dram[name]

    def alloc(self, name, shape, dt, es=None):
        es = es or self.es
        self.uid = getattr(self, "uid", 0) + 1
        t = es.enter_context(self.nc.sbuf_tensor(f"sb{self.uid}_{name}", list(shape), dt))
        return t, Buf(name)

    def MM(self, out, lhsT, rhs, start, stop, rd, wr, inc=None):
        nc = self.nc
        if inc is None:
            inc = stop
        return self.fw.op(self.pe, lambda: nc.tensor.matmul(out, lhsT=lhsT, rhs=rhs, start=start, stop=stop),
                          rd, wr, inc)

    def ACT(self, out, in_, func, rd, wr, bias=None, scale=None):
        nc = self.nc
        kw = {}
        if bias is not None:
            kw["bias"] = bias
        if scale is not None:
            kw["scale"] = scale
        return self.fw.op(self.act, lambda: nc.scalar.activation(out=out, in_=in_, func=func, **kw), rd, wr)

    def TT(self, out, in0, in1, op, rd, wr, eng=None):
        nc = self.nc
        eng = eng or self.dve
        return self.fw.op(eng, lambda: eng.h.tensor_tensor(out=out, in0=in0, in1=in1, op=op), rd, wr)

    def STT(self, out, in0, scalar, in1, op0, op1, rd, wr):
        nc = self.nc
        return self.fw.op(self.dve, lambda: nc.vector.scalar_tensor_tensor(out=out, in0=in0, scalar=scalar, in1=in1,
                                                                           op0=op0, op1=op1), rd, wr)

    def TS(self, out, in0, s1, s2, op0, op1, rd, wr, eng=None):
        eng = eng or self.dve
        if op1 is None:
            return self.fw.op(eng, lambda: eng.h.tensor_scalar(out=out, in0=in0, scalar1=s1, scalar2=None, op0=op0),
                              rd, wr)
        return self.fw.op(eng, lambda: eng.h.tensor_scalar(out=out, in0=in0, scalar1=s1, scalar2=s2, op0=op0, op1=op1),
                          rd, wr)

    def CP(self, out, in_, rd, wr, eng=None):
        eng = eng or self.dve
        return self.fw.op(eng, lambda: eng.h.tensor_copy(out=out, in_=in_), rd, wr)

    def RECIP(self, out, in_, rd, wr):
        nc = self.nc
        return self.fw.op(self.dve, lambda: nc.vector.reciprocal(out=out, in_=in_), rd, wr)

    def MSET(self, ap, val, wr, eng=None):
        eng = eng or self.dve
        return self.fw.op(eng, lambda: eng.h.memset(ap, val), (), wr)

    def _dstream(self, wr, issuer, handle, pref):
        key = pref + (wr[0].name if len(wr) else "_out")
        if key not in self.dstreams:
            self.dstreams[key] = self.fw.add_engine(key, handle, step=16, issuer=issuer)
        return self.dstreams[key]

    def DMA(self, out, in_, rd, wr):
        nc = self.nc
        st = self._dstream(wr, self.sp, nc.sync, "dq_")
        return self.fw.op(st, lambda: nc.sync.dma_start(out=out, in_=in_), rd, wr)

    def DMAC(self, out, in_, rd, wr):
        nc = self.nc
        st = self._dstream(wr, self.pool, nc.gpsimd, "dg_")
        return self.fw.op(st, lambda: nc.gpsimd.dma_start(out=out, in_=in_), rd, wr)

    def rstd(self, out, ss_ps, inv_n, rd, wr):
        self.ACT(out, ss_ps, AF.Ln, rd, wr, bias=self.cst[:ss_ps.shape[0], 0:1], scale=inv_n)
        self.ACT(out, out, AF.Exp, wr, wr, scale=-0.5)

    def dump(self, name, ap, rd):
        if name not in self.dbg:
            return
        shape = list(ap.shape)
        o = self.nc.dram_tensor("dbg_" + name, shape, ap.dtype, kind="ExternalOutput").ap()
        self.dbg_out[name] = o
        self.DMA(o, ap, rd, ())

    def build(self):
        nc = self.nc
        din = self.din
        xT_d = din("xT", [D, S])
        outT_d = nc.dram_tensor("outT", [D, S], F32, kind="ExternalOutput").ap()
        cT_d = din("cT", [128, 8])
        pos_d = din("pos32", [32, S], I32)
        invf_d = din("invf", [32, 1])
        w_ada_d = din("w_ada", [DEPTH, D, 6 * D])
        b_ada_d = din("b_adaT", [128, DEPTH, 48])
        normg_d = din("norm_gT", [128, DEPTH, 2, 8])
        w_in_d = din("w_in", [DEPTH, D, IN_W])
        qkgA_d = din("qkgA", [128, DEPTH, 2])
        dlam_d = din("dlam", [128, DEPTH, 256])
        goutA_d = din("goutA", [128, DEPTH])
        biasA_d = din("biasA", [128, 8, 128])
        cfar_d = din("cfar", [128, 4])
        sgu_vg_d = din("sgu_vg", [DEPTH, 128, 512])
        sgu_wT_d = din("sgu_wT", [DEPTH, 128, 4, 128])
        sgu_bb_d = din("sgu_bb", [DEPTH, 128, 4, 128])
        latg_d = din("latg", [128, DEPTH, 3])
        w_uq_d = din("w_uq", [DEPTH, 256, 768])
        w_ukv_d = din("w_ukv", [DEPTH, 128, 1024])
        qkgCn_d = din("qkgCn", [128, DEPTH, 2])
        qkgCr_d = din("qkgCr", [32, DEPTH, 2])
        w_br_d = din("w_branch", [DEPTH, 3, 512, D])
        w_out_d = din("w_out", [DEPTH, D, D])
        w_r_d = din("w_r", [DEPTH, D, 36])
        b_r_d = din("b_r", [128, DEPTH, 36])
        weg_d = din("w_e_gate", [DEPTH, 32, D, 256])
        weu_d = din("w_e_up", [DEPTH, 32, D, 256])
        wed_d = din("w_e_down", [DEPTH, 32, 256, D])
        R32_d = din("R32T", [32, 32])
        ident_d = din("ident", [128, 128])

        with ExitStack() as es:
            self.es = es
            fw = self.fw = FW(nc, es)
            self.pe = fw.add_engine("pe", nc.tensor)
            self.act = fw.add_engine("act", nc.scalar)
            self.dve = fw.add_engine("dve", nc.vector)
            self.pool = fw.add_engine("pool", nc.gpsimd)
            self.sp = fw.add_engine("sp", nc.sync)
            self.dstreams = {}
            MM, ACT, TT, STT, TS, CP, RECIP, MSET, DMA, DMAC = (self.MM, self.ACT, self.TT, self.STT, self.TS,
                                                               self.CP, self.RECIP, self.MSET, self.DMA, self.DMAC)
            alloc = self.alloc

            PS = []
            PB = []
            for i in range(8):
                PS.append(es.enter_context(nc.psum_tensor(f"ps{i}", [128, 512], F32)))
                PB.append(Buf(f"ps{i}", excl=True))
            self.psg_i = 0
            self.psg_banks = [6, 7]
            ALLB = list(range(8))

            def psg():
                i = self.psg_banks[self.psg_i % len(self.psg_banks)]
                self.psg_i += 1
                return PS[i], PB[i]

            xT, xTb = alloc("xT", [128, 8, S], F32)
            xTB = [Buf(f"xT{b}") for b in range(NB)]
            ones_bf, ones_b = alloc("ones_bf", [128, 128], BF16)
            bones, bones_b = alloc("bones64", [128, 128], BF16)
            cst, cst_b = alloc("cst", [128, 4], F32)
            self.cst = cst
            ident, ident_b = alloc("ident", [128, 128], F32)
            R32, R32_b = alloc("R32", [32, 32], BF16)
            sinT, sin_b = alloc("sinT", [32, S], BF16)
            cosT, cos_b = alloc("cosT", [32, S], BF16)
            comb_scr = nc.dram_tensor("comb_scr", [32, S], F32, kind="Internal").ap()
            comb_scr_b = Buf("comb_scr")
            hbf, hbf_b = alloc("hbf", [128, 8, TB], BF16)
            h_scr = nc.dram_tensor("h_scr", [NB, 128, 8 * TB], BF16, kind="Internal").ap()
            h_scr_b = [Buf(f"h_scr{b}") for b in range(NB)]
            hbf_flat = hbf[:].rearrange("p c t -> p (c t)")
            self.h_r = alloc("h_r", [128, TB], F32)
            ht = alloc("h_t", [128, 2, TB], F32)
            self.h_t = (ht[0], [Buf("ht0"), Buf("ht1")])
            self.n_sq = Ring(self, "n_sq", [128, TB], BF16, 2)
            self.n_r = Ring(self, "n_r", [128, TB], F32, 4)
            Pr = Ring(self, "Pt", [128, TB], BF16, 4)
            MOD, MOD_b = alloc("MOD", [128, DEPTH, 48], F32)
            A1, A1_b = alloc("A1", [128, DEPTH, 8], F32)
            A2, A2_b = alloc("A2", [128, DEPTH, 8], F32)
            neglam, neglam_b = alloc("neglam", [128, DEPTH], F32)
            gout, gout_b = alloc("gout", [128, DEPTH], F32)
            qkgA, qkgA_b = alloc("qkgA", [128, DEPTH, 2], F32)
            latg, latg_b = alloc("latg", [128, DEPTH, 3], F32)
            qkgCn, qkgCn_b = alloc("qkgCn", [128, DEPTH, 2], F32)
            qkgCr, qkgCr_b = alloc("qkgCr", [32, DEPTH, 2], F32)
            Eb, Eb_b = alloc("Eb", [128, 8, 128], F32)
            cfar, cfar_b = alloc("cfar", [128, 4], F32)
            b_r, b_r_b = alloc("b_r", [128, DEPTH, 36], F32)

            MSET(ones_bf[:], 1.0, [ones_b])
            MSET(bones[:], 0.0, [bones_b])
            MSET(bones[0:64, 0:64], 1.0, [bones_b])
            MSET(bones[64:128, 64:128], 1.0, [bones_b])
            MSET(cst[:, 0:1], EPS, [cst_b])
            MSET(cst[:, 1:2], 0.0, [cst_b])
            DMA(ident[:], ident_d, (), [ident_b])
            DMAC(R32[:], R32_d, (), [R32_b])
            DMA(qkgA[:], qkgA_d, (), [qkgA_b])
            DMA(latg[:], latg_d, (), [latg_b])
            DMA(qkgCn[:], qkgCn_d, (), [qkgCn_b])
            DMA(qkgCr[:], qkgCr_d, (), [qkgCr_b])
            DMA(cfar[:], cfar_d, (), [cfar_b])
            DMA(b_r[:], b_r_d, (), [b_r_b])
            for c in range(8):
                DMA(xT[:, c, :], xT_d[c * 128:(c + 1) * 128, :], (), xTB)

            with ExitStack() as pes:
                pos_i, pos_ib = alloc("pos_i", [32, S], I32, pes)
                ang, ang_b = alloc("ang", [32, S], F32, pes)
                t1, t1_b = alloc("rt1", [32, S], F32, pes)
                t2, t2_b = alloc("rt2", [32, S], F32, pes)
                ki, ki_b = alloc("rki", [32, S], I32, pes)
                invf, invf_b = alloc("invf", [32, 1], F32, pes)
                DMA(pos_i[:], pos_d, (), [pos_ib])
                DMA(invf[:], invf_d, (), [invf_b])
                CP(ang[:], pos_i[:], [pos_ib], [ang_b])
                TS(ang[:], ang[:], invf[:, 0:1], None, ALU.mult, None, [ang_b, invf_b], [ang_b])
                TS(t1[:], ang[:], float(1.0 / (2 * np.pi)), None, ALU.mult, None, [ang_b], [t1_b])
                CP(ki[:], t1[:], [t1_b], [ki_b])
                CP(t1[:], ki[:], [ki_b], [t1_b])
                STT(t2[:], t1[:], -6.28125, ang[:], ALU.mult, ALU.add, [t1_b, ang_b], [t2_b])
                STT(t2[:], t1[:], -0.0019353071795864769, t2[:], ALU.mult, ALU.add, [t1_b, t2_b], [t2_b])
                TS(t1[:], t2[:], float(np.pi), -float(2 * np.pi), ALU.is_gt, ALU.mult, [t2_b], [t1_b])
                TT(t2[:], t2[:], t1[:], ALU.add, [t2_b, t1_b], [t2_b])
                TS(t1[:], t2[:], -float(np.pi), float(2 * np.pi), ALU.is_lt, ALU.mult, [t2_b], [t1_b])
                TT(t2[:], t2[:], t1[:], ALU.add, [t2_b, t1_b], [t2_b])
                ACT(sinT[:], t2[:], AF.Sin, [t2_b], [sin_b])
                TS(t2[:], t2[:], float(np.pi / 2), None, ALU.add, None, [t2_b], [t2_b])
                TS(t1[:], t2[:], float(np.pi), -float(2 * np.pi), ALU.is_gt, ALU.mult, [t2_b], [t1_b])
                TT(t2[:], t2[:], t1[:], ALU.add, [t2_b, t1_b], [t2_b])
                ACT(cosT[:], t2[:], AF.Sin, [t2_b], [cos_b])

                biasA, biasA_b = alloc("biasA", [128, 8, 128], F32, pes)
                ncf, ncf_b = alloc("ncf", [128, 4], F32, pes)
                DMA(biasA[:], biasA_d, (), [biasA_b])
                TS(ncf[:], cfar[:], -1.0, None, ALU.mult, None, [cfar_b], [ncf_b])
                for bi in range(8):
                    ACT(Eb[:, bi, :], biasA[:, bi, :], AF.Exp, [biasA_b, ncf_b], [Eb_b], bias=ncf[:, bi % 4:bi % 4 + 1], scale=1.0)
                c_sb, c_b = alloc("c_sb", [128, 8], F32, pes)
                c_act, cact_b = alloc("c_act", [128, 8], BF16, pes)
                b_ada, bada_b = alloc("b_ada", [128, DEPTH, 48], F32, pes)
                normg, normg_b = alloc("normg", [128, DEPTH, 2, 8], F32, pes)
                dlam, dlam_b = alloc("dlam", [128, DEPTH, 256], F32, pes)
                goutA, goutA_b = alloc("goutA", [128, DEPTH], F32, pes)
                DMA(c_sb[:], cT_d, (), [c_b])
                DMA(b_ada[:], b_ada_d, (), [bada_b])
                DMA(normg[:], normg_d, (), [normg_b])
                DMA(dlam[:], dlam_d, (), [dlam_b])
                DMA(goutA[:], goutA_d, (), [goutA_b])
                ACT(c_act[:], c_sb[:], AF.Silu, [c_b], [cact_b])
                wa_ring = Ring(self, "wada", [128, 8, 1024], BF16, 0)
                wa_ring.items = [alloc(f"wada{i}", [128, 8, 1024], BF16, pes) for i in range(2)]
                for l in range(self.depth):
                    ps, pb = PS[l % 2], PB[l % 2]
                    for piece in range(6):
                        wa, wab = wa_ring.next()
                        src = w_ada_d[l, :, piece * 1024:(piece + 1) * 1024].rearrange("(kc p) n -> p kc n", p=128)
                        DMAC(wa[:], src, (), [wab])
                        for jj in range(8):
                            j = piece * 8 + jj
                            for kc in range(8):
                                MM(ps[:, j:j + 1], wa[:, kc, jj * 128:(jj + 1) * 128], c_act[:, kc:kc + 1],
                                   kc == 0, kc == 7, [wab, cact_b], [pb], inc=(kc == 7 and jj == 7))
                    TT(MOD[:, l, :], ps[:, 0:48], b_ada[:, l, :], ALU.add, [pb, bada_b], [MOD_b])
                for l in range(self.depth):
                    STT(A1[:, l, :], MOD[:, l, 8:16], 1.0, normg[:, l, 0, :], ALU.add, ALU.mult, [MOD_b, normg_b], [A1_b])
                    STT(A2[:, l, :], MOD[:, l, 32:40], 1.0, normg[:, l, 1, :], ALU.add, ALU.mult, [MOD_b, normg_b], [A2_b])
                lt, lt_b = alloc("lam_t", [128, DEPTH, 2, 64], F32, pes)
                ls, ls_b = alloc("lam_s", [128, DEPTH, 2], F32, pes)
                dl4 = dlam[:].rearrange("p l (a d) -> p l a d", a=4)
                TT(lt[:, :, 0, :], dl4[:, :, 0, :], dl4[:, :, 1, :], ALU.mult, [dlam_b], [lt_b])
                TT(lt[:, :, 1, :], dl4[:, :, 2, :], dl4[:, :, 3, :], ALU.mult, [dlam_b], [lt_b])
                fw.op(self.dve, lambda: nc.vector.tensor_reduce(out=ls[:], in_=lt[:], axis=AX.X, op=ALU.add), [lt_b], [ls_b])
                ACT(ls[:], ls[:], AF.Exp, [ls_b], [ls_b])
                for l in range(self.depth):
                    STT(neglam[:, l:l + 1], ls[:, l, 0:1], -1.0, ls[:, l, 1:2], ALU.mult, ALU.add, [ls_b], [neglam_b])
                    TS(neglam[:, l:l + 1], neglam[:, l:l + 1], -LAMBDA_INIT[l], None, ALU.add, None, [neglam_b], [neglam_b])
                    TS(gout[:, l:l + 1], goutA[:, l:l + 1], 1.0 - LAMBDA_INIT[l], None, ALU.mult, None, [goutA_b], [gout_b])
                self.dump("MOD", MOD[:], [MOD_b])
                self.dump("sinT", sinT[:], [sin_b])
                self.dump("cosT", cosT[:], [cos_b])
                self.dump("neglam", neglam[:], [neglam_b])
                fw.barrier()

            def make_h(tb, A, B_ap_fn, hbf, hbf_b, tmp_es, l, hf=None, hf_b=None):
                blk = slice(tb * TB, (tb + 1) * TB)
                r_sb, r_b = self.h_r
                t_sb, t_b = self.h_t
                ps, pb = psg()
                for c in range(8):
                    sq, sq_b = self.n_sq.next()
                    ACT(sq[:], xT[:, c, blk], AF.Square, [xTB[tb]], [sq_b])
                    MM(ps[:], ones_bf[:], sq[:], c == 0, c == 7, [ones_b, sq_b], [pb], inc=True)
                self.rstd(r_sb[:], ps[:], 1.0 / D, [pb, cst_b], [r_b])
                for c in range(8):
                    STT(t_sb[:, c % 2, :], xT[:, c, blk], A[:, l, c:c + 1], r_sb[:], ALU.mult, ALU.mult,
                        [xTB[tb], r_b], [t_b[c % 2]])
                    if hf is not None:
                        ACT(hf[:, c, :], t_sb[:, c % 2, :], AF.Identity, [t_b[c % 2], MOD_b], [hf_b], bias=B_ap_fn(c))
                        CP(hbf[:, c, :], hf[:, c, :], [hf_b], [hbf_b], eng=self.pool)
                    else:
                        ACT(hbf[:, c, :], t_sb[:, c % 2, :], AF.Identity, [t_b[c % 2], MOD_b], [hbf_b], bias=B_ap_fn(c))

            def group_norm_fm(src_ps, src_pb, npart, ones_l, inv_n, g_ap, out_ap, out_bufs, g_bufs):
                sq, sq_b = self.n_sq.next()
                ACT(sq[:npart, :], src_ps, AF.Square, [src_pb], [sq_b])
                ps, pb = psg()
                MM(ps[:npart, :], ones_l, sq[:npart, :], True, True, [ones_b, bones_b, sq_b], [pb])
                r, r_b = self.n_r.next()
                self.rstd(r[:npart, :], ps[:npart, :], inv_n, [pb, cst_b], [r_b])
                STT(out_ap, src_ps, g_ap, r[:npart, :], ALU.mult, ALU.mult, [src_pb, r_b] + g_bufs, out_bufs)

            def rope(x_bf, x_b, blk, out_ap, out_bufs):
                ps, pb = psg()
                MM(ps[0:32, :], R32[:], x_bf, True, True, [R32_b, x_b], [pb])
                ta, ta_b = self.n_r.next()
                tb_, tb_b = self.n_r.next()
                TT(ta[0:32, :], x_bf, cosT[:, blk], ALU.mult, [x_b, cos_b], [ta_b])
                TT(tb_[0:32, :], ps[0:32, :], sinT[:, blk], ALU.mult, [pb, sin_b], [tb_b])
                TT(out_ap, ta[0:32, :], tb_[0:32, :], ALU.add, [ta_b, tb_b], out_bufs)

            class Lane:
                pass
            lanes = []
            for k in range(2):
                ln = Lane()
                ln.sq = self.n_sq.items[k]
                ln.nr = [self.n_r.items[2 * k], self.n_r.items[2 * k + 1]]
                ln.banks = [4 * k, 4 * k + 1, 4 * k + 2, 4 * k + 3]
                ln.bi = 0
                lanes.append(ln)

            def lane_ps(ln):
                b = ln.banks[ln.bi % 4]
                ln.bi += 1
                return PS[b], PB[b]

            def g_group_norm(ln, src_ps, src_pb, npart, ones_l, inv_n, g_ap, out_ap, out_bufs, g_bufs):
                sq, sq_b = ln.sq
                ACT(sq[:npart, :], src_ps, AF.Square, [src_pb], [sq_b])
                yield
                ps, pb = lane_ps(ln)
                MM(ps[:npart, :], ones_l, sq[:npart, :], True, True, [ones_b, bones_b, sq_b], [pb])
                yield
                r, r_b = ln.nr[0]
                ACT(r[:npart, :], ps[:npart, :], AF.Ln, [pb, cst_b], [r_b], bias=cst[:npart, 0:1], scale=inv_n)
                yield
                ACT(r[:npart, :], r[:npart, :], AF.Exp, [r_b], [r_b], scale=-0.5)
                yield
                STT(out_ap, src_ps, g_ap, r[:npart, :], ALU.mult, ALU.mult, [src_pb, r_b] + g_bufs, out_bufs)
                yield

            def g_rope(ln, x_bf, x_b, blk, out_ap, out_bufs):
                ps, pb = lane_ps(ln)
                MM(ps[0:32, :], R32[:], x_bf, True, True, [R32_b, x_b], [pb])
                yield
                ta, ta_b = ln.nr[0]
                tb_, tb_b = ln.nr[1]
                TT(ta[0:32, :], x_bf, cosT[:, blk], ALU.mult, [x_b, cos_b], [ta_b])
                yield
                TT(tb_[0:32, :], ps[0:32, :], sinT[:, blk], ALU.mult, [pb, sin_b], [tb_b])
                yield
                TT(out_ap, ta[0:32, :], tb_[0:32, :], ALU.add, [ta_b, tb_b], out_bufs)
                yield

            def run_chains(chain_fns):
                todo = list(chain_fns)
                active = [None, None]
                while todo or any(a is not None for a in active):
                    for k in range(2):
                        if active[k] is None and todo:
                            active[k] = todo.pop(0)(lanes[k])
                        if active[k] is not None:
                            try:
                                next(active[k])
                            except StopIteration:
                                active[k] = None

            def emit_output():
                for c in range(8):
                    DMA(outT_d[c * 128:(c + 1) * 128, :], xT[:, c, :], xTB, ())
                fw.barrier()

            self.sub = float(os.environ.get("K_SUB", 99))

            def sstop(n):
                if self.sub <= n and not fw.disabled:
                    fw.barrier()
                    emit_output()
                    fw.disabled = True

            def stop_at(n):
                if self.stop <= n and not fw.disabled:
                    fw.barrier()
                    emit_output()
                    fw.disabled = True

            if True:
              stop_at(0)
              for l in range(self.depth):
                with ExitStack() as les:
                  with ExitStack() as mes:
                    o_c, o_c_b = alloc("o_c", [128, 4, S], BF16, mes)
                    B1 = lambda c: MOD[:, l, c:c + 1]
                    B2 = lambda c: MOD[:, l, 24 + c:24 + c + 1]

                    with ExitStack() as ses:
                        w_mla, w_mla_b = alloc("w_mla", [128, 8, 416], BF16, ses)
                        w_uq, w_uq_b = alloc("w_uq", [128, 2, 768], BF16, ses)
                        w_ukv, w_ukv_b = alloc("w_ukv", [128, 1024], BF16, ses)
                        Kn, Kn_b = alloc("Kn", [128, 4, S], BF16, ses)
                        Vc, Vc_b = alloc("Vc", [128, 16, 512], BF16, ses)
                        Kr, Kr_b = alloc("Kr", [32, S], BF16, ses)
                        cqn, cqn_b = alloc("cqn", [128, 2, TB], BF16, ses)
                        cqf, cqf_b = alloc("cqf", [128, 2, TB], F32, ses)
                        cqs, cqs_b = alloc("cqs", [128, 2, TB], BF16, ses)
                        ckvn, ckvn_b = alloc("ckvn", [128, TB], BF16, ses)
                        qn, qn_b = alloc("qn", [128, 4, TB], BF16, ses)
                        qr, qr_b = alloc("qr", [32, 8, TB], BF16, ses)
                        xr_r = [alloc(f"xr{i}", [32, TB], BF16, ses) for i in range(2)]
                        rc, rc_b = alloc("rc", [128, TB], F32, ses)
                        acc_r = [alloc(f"accC{i}", [128, TB], F32, ses) for i in range(2)]
                        accb_r = [alloc(f"accCb{i}", [128, TB], BF16, ses) for i in range(2)]
                        DMAC(w_mla[:], w_in_d[l, :, 2560:2976].rearrange("(kc p) n -> p kc n", p=128), (), [w_mla_b])
                        DMAC(w_uq[:], w_uq_d[l].rearrange("(kc p) n -> p kc n", p=128), (), [w_uq_b])
                        DMAC(w_ukv[:], w_ukv_d[l], (), [w_ukv_b])
                        sc_c = float(96 ** -0.5)
                        for tb in range(NB):
                            blk = slice(tb * TB, (tb + 1) * TB)
                            self.psg_banks = ALLB
                            make_h(tb, A1, B1, hbf, hbf_b, ses, l)
                            DMA(h_scr[tb], hbf_flat, [hbf_b], [h_scr_b[tb]])
                            if l == 0 and tb == 0:
                                self.dump("h0", hbf[:], [hbf_b])
                            sstop(1)
                            for j in range(2):
                                ps, pb = psg()
                                for kc in range(8):
                                    MM(ps[:], w_mla[:, kc, j * 128:(j + 1) * 128], hbf[:, kc, :], kc == 0, kc == 7,
                                       [w_mla_b, hbf_b], [pb])
                                kvar = int(os.environ.get("K_VAR", 0))
                                if kvar in (0, 2):
                                    ACT(cqs[:, j, :], ps[:], AF.Square, [pb], [cqs_b])
                                if kvar in (0, 3):
                                    CP(cqf[:, j, :], ps[:], [pb], [cqf_b])
                            sstop(1.2)
                            ps, pb = psg()
                            for j in range(2):
                                MM(ps[:], ones_bf[:], cqs[:, j, :], j == 0, j == 1, [ones_b, cqs_b], [pb])
                            r, r_b = self.n_r.next()
                            self.rstd(r[:], ps[:], 1.0 / 256, [pb, cst_b], [r_b])
                            for j in range(2):
                                STT(cqn[:, j, :], cqf[:, j, :], latg[:, l, j:j + 1], r[:], ALU.mult, ALU.mult,
                                    [cqf_b, r_b, latg_b], [cqn_b])
                            sstop(1.5)

                            def ch_ckv(ln):
                                ps, pb = lane_ps(ln)
                                for kc in range(8):
                                    MM(ps[:], w_mla[:, kc, 256:384], hbf[:, kc, :], kc == 0, kc == 7, [w_mla_b, hbf_b], [pb])
                                yield
                                yield from g_group_norm(ln, ps[:], pb, 128, ones_bf[:], 1.0 / 128, latg[:, l, 2:3], ckvn[:], [ckvn_b], [latg_b])

                            def ch_kr(ln):
                                ps, pb = lane_ps(ln)
                                for kc in range(8):
                                    MM(ps[0:32, :], w_mla[:, kc, 384:416], hbf[:, kc, :], kc == 0, kc == 7, [w_mla_b, hbf_b], [pb])
                                yield
                                x, x_b = xr_r[lanes.index(ln)]
                                yield from g_group_norm(ln, ps[0:32, :], pb, 32, ones_bf[0:32, 0:32], 1.0 / 32, qkgCr[:, l, 1:2], x[:], [x_b], [qkgCr_b])
                                yield from g_rope(ln, x[:], x_b, blk, Kr[:, blk], [Kr_b])

                            def ch_qnope(j):
                                def f(ln):
                                    ps, pb = lane_ps(ln)
                                    for kc in range(2):
                                        MM(ps[:], w_uq[:, kc, j * 128:(j + 1) * 128], cqn[:, kc, :], kc == 0, kc == 1, [w_uq_b, cqn_b], [pb])
                                    yield
                                    yield from g_group_norm(ln, ps[:], pb, 128, bones[:], 1.0 / 64, qkgCn[:, l, 0:1], qn[:, j, :], [qn_b], [qkgCn_b])
                                return f

                            def ch_qrope(h):
                                def f(ln):
                                    ps, pb = lane_ps(ln)
                                    for kc in range(2):
                                        MM(ps[0:32, :], w_uq[:, kc, 512 + h * 32:512 + (h + 1) * 32], cqn[:, kc, :], kc == 0, kc == 1, [w_uq_b, cqn_b], [pb])
                                    yield
                                    x, x_b = xr_r[lanes.index(ln)]
                                    yield from g_group_norm(ln, ps[0:32, :], pb, 32, ones_bf[0:32, 0:32], 1.0 / 32, qkgCr[:, l, 0:1], x[:], [x_b], [qkgCr_b])
                                    yield from g_rope(ln, x[:], x_b, blk, qr[:, h, :], [qr_b])
                                return f

                            def ch_knope(j):
                                def f(ln):
                                    ps, pb = lane_ps(ln)
                                    MM(ps[:], w_ukv[:, j * 128:(j + 1) * 128], ckvn[:], True, True, [w_ukv_b, ckvn_b], [pb])
                                    yield
                                    yield from g_group_norm(ln, ps[:], pb, 128, bones[:], 1.0 / 64, qkgCn[:, l, 1:2], Kn[:, j, blk], [Kn_b], [qkgCn_b])
                                return f

                            def ch_v(tt):
                                def f(ln):
                                    ps, pb = lane_ps(ln)
                                    MM(ps[:], ckvn[:, tt * 128:(tt + 1) * 128], w_ukv[:, 512:1024], True, True, [ckvn_b, w_ukv_b], [pb])
                                    yield
                                    CP(Vc[:, tb * 4 + tt, :], ps[:], [pb], [Vc_b])
                                    yield
                                return f

                            run_chains([ch_ckv, ch_kr])
                            sstop(3)
                            run_chains([ch_qnope(j) for j in range(4)] + [ch_qrope(h) for h in range(8)]
                                       + [ch_knope(j) for j in range(4)] + [ch_v(tt) for tt in range(4)])
                            if l == 0 and tb == 0:
                                self.dump("qn0", qn[:], [qn_b])
                                self.dump("qr0", qr[:], [qr_b])
                                self.dump("Kr0", Kr[:, 0:TB], [Kr_b])
                                self.dump("Kn0", Kn[:, :, 0:TB], [Kn_b])
                                self.dump("Vc0", Vc[:, 0:4, :], [Vc_b])
                            sstop(4)
                            self.psg_banks = [6, 7]
                            nkt = 4 * (tb + 1)
                            steps = [(j, hh, kt) for j in range(4) for hh in range(2) for kt in range(nkt)]
                            sring = [0]

                            def emit_S(st, i):
                                j, hh, kt = st
                                h = 2 * j + hh
                                rows = slice(hh * 64, (hh + 1) * 64)
                                j0 = max(0, kt - 4 * tb)
                                cols = slice(j0 * 128, TB)
                                Sps, Spb = PS[i % 2], PB[i % 2]
                                MM(Sps[:, cols], Kn[rows, j, kt * 128:(kt + 1) * 128], qn[rows, j, cols], True, False,
                                   [Kn_b, qn_b], [Spb], inc=False)
                                MM(Sps[:, cols], Kr[:, kt * 128:(kt + 1) * 128], qr[:, h, cols], False, True,
                                   [Kr_b, qr_b], [Spb])

                            def emit_rest(st, i):
                                j, hh, kt = st
                                h = 2 * j + hh
                                rows = slice(hh * 64, (hh + 1) * 64)
                                j0 = max(0, kt - 4 * tb)
                                cols = slice(j0 * 128, TB)
                                Sps, Spb = PS[i % 2], PB[i % 2]
                                Ops, Opb = PS[2 + j % 2], PB[2 + j % 2]
                                Sms, Smb = PS[4 + j % 2], PB[4 + j % 2]
                                P, P_b = Pr.next()
                                ACT(P[:, cols], Sps[:, cols], AF.Exp, [Spb], [P_b], scale=sc_c)
                                if kt >= 4 * tb:
                                    MSET(P[64:128, j0 * 128:j0 * 128 + 64], 0.0, [P_b])
                                first = (kt == 0)
                                last = (kt == nkt - 1)
                                MM(Ops[rows, cols], Vc[:, kt, h * 64:(h + 1) * 64], P[:, cols], first, last, [Vc_b, P_b], [Opb],
                                   inc=True)
                                acc, acc_b = acc_r[h % 2]
                                if first:
                                    CP(acc[:, cols], P[:, cols], [P_b], [acc_b])
                                else:
                                    TT(acc[:, cols], acc[:, cols], P[:, cols], ALU.add, [acc_b, P_b], [acc_b])
                                if last:
                                    def fin(acc=acc, acc_b=acc_b, h=h, hh=hh, j=j, rows=rows, Ops=Ops, Opb=Opb, Sms=Sms, Smb=Smb):
                                        accb, accb_b = accb_r[h % 2]
                                        CP(accb[:], acc[:], [acc_b], [accb_b])
                                        MM(Sms[rows, :], ones_bf[:, 0:64], accb[:], True, True, [ones_b, accb_b], [Smb], inc=True)
                                        if hh == 1:
                                            ACT(rc[:], Sms[:], AF.Ln, [Smb], [rc_b])
                                            ACT(rc[:], rc[:], AF.Exp, [rc_b], [rc_b], scale=-1.0)
                                            TT(o_c[:, j, blk], Ops[:], rc[:], ALU.mult, [Opb, rc_b], [o_c_b])
                                    pending.append((i + 2, fin))

                            pending = []
                            emit_S(steps[0], 0)
                            for i, st in enumerate(steps):
                                if i + 1 < len(steps):
                                    emit_S(steps[i + 1], i + 1)
                                emit_rest(st, i)
                                while pending and pending[0][0] <= i:
                                    pending.pop(0)[1]()
                            while pending:
                                pending.pop(0)[1]()
                        self.dump("o_c", o_c[:], [o_c_b])
                        fw.barrier()
                    stop_at(1)

                    o_a, o_a_b = alloc("o_a", [128, 4, S], BF16, mes)
                    with ExitStack() as ses:
                        wa_r = [alloc(f"w_a{i}", [128, 8, 512], BF16, ses) for i in range(2)]
                        wa_i = [0]
                        Ka, Ka_b = alloc("Ka", [128, 4, S], BF16, ses)
                        Va, Va_b = alloc("Va", [128, 16, 512], BF16, ses)
                        Qa, Qa_b = alloc("Qa", [128, 4, TB], BF16, ses)
                        rc, rc_b = alloc("rca", [128, TB], F32, ses)
                        accA_r = [alloc(f"accA{i}", [128, TB], F32, ses) for i in range(2)]
                        accAb, accAb_b = alloc("accAb", [128, TB], BF16, ses)
                        o0, o0_b = alloc("o0", [128, TB], F32, ses)
                        o1, o1_b = alloc("o1", [128, TB], F32, ses)
                        dd, dd_b = alloc("dd", [128, TB], F32, ses)
                        sc_a = 0.125
                        for tb in range(NB):
                            blk = slice(tb * TB, (tb + 1) * TB)
                            self.psg_banks = ALLB
                            if tb == 0:
                                DMA(hbf_flat, h_scr[0], [h_scr_b[0]], [hbf_b])
                            wqkv = []
                            for q in range(3):
                                if q == 0 and tb > 0:
                                    w_a, w_a_b = qpre
                                else:
                                    w_a, w_a_b = wa_r[wa_i[0] % 2]; wa_i[0] += 1
                                    DMAC(w_a[:], w_in_d[l, :, q * 512:(q + 1) * 512].rearrange("(kc p) n -> p kc n", p=128), (), [w_a_b])
                                if q <= 1:
                                    def ch_qk(hd, q=q, w_a=w_a, w_a_b=w_a_b):
                                        def f(ln):
                                            ps, pb = lane_ps(ln)
                                            for kc in range(8):
                                                MM(ps[:], w_a[:, kc, hd * 128:(hd + 1) * 128], hbf[:, kc, :], kc == 0, kc == 7, [w_a_b, hbf_b], [pb])
                                            yield
                                            if q == 0:
                                                yield from g_group_norm(ln, ps[:], pb, 128, bones[:], 1.0 / 64, qkgA[:, l, 0:1], Qa[:, hd, :], [Qa_b], [qkgA_b])
                                            else:
                                                yield from g_group_norm(ln, ps[:], pb, 128, bones[:], 1.0 / 64, qkgA[:, l, 1:2], Ka[:, hd, blk], [Ka_b], [qkgA_b])
                                        return f
                                    run_chains([ch_qk(hd) for hd in range(4)])
                                else:
                                    for tt in range(4):
                                        ps, pb = psg()
                                        for kc in range(8):
                                            MM(ps[:], hbf[:, kc, tt * 128:(tt + 1) * 128], w_a[:, kc, :], kc == 0, kc == 7, [hbf_b, w_a_b], [pb])
                                        CP(Va[:, tb * 4 + tt, :], ps[:], [pb], [Va_b])
                            if l == 0 and tb == 0:
                                self.dump("Qa0", Qa[:], [Qa_b])
                                self.dump("Va0", Va[:, 0:4, :], [Va_b])
                            if tb + 1 < NB:
                                DMA(hbf_flat, h_scr[tb + 1], [h_scr_b[tb + 1]], [hbf_b])
                                qpre = wa_r[wa_i[0] % 2]; wa_i[0] += 1
                                DMAC(qpre[0][:], w_in_d[l, :, 0:512].rearrange("(kc p) n -> p kc n", p=128), (), [qpre[1]])
                            self.psg_banks = [7]
                            SB3 = [0, 1, 6]
                            nkt = 4 * (tb + 1)
                            steps = [(hd, m, kt) for hd in range(4) for m in range(2) for kt in range(nkt)]

                            def emit_S(st, i):
                                hd, m, kt = st
                                rows = slice(m * 64, (m + 1) * 64)
                                j0 = max(0, kt - 4 * tb)
                                cols = slice(j0 * 128, TB)
                                Sps, Spb = PS[SB3[i % 3]], PB[SB3[i % 3]]
                                MM(Sps[:, cols], Ka[rows, hd, kt * 128:(kt + 1) * 128], Qa[rows, hd, cols], True, True,
                                   [Ka_b, Qa_b], [Spb])

                            def emit_rest(st, i):
                                hd, m, kt = st
                                sidx = hd * 2 + m
                                j0 = max(0, kt - 4 * tb)
                                cols = slice(j0 * 128, TB)
                                Sps, Spb = PS[SB3[i % 3]], PB[SB3[i % 3]]
                                Ops, Opb = PS[2 + sidx % 2], PB[2 + sidx % 2]
                                Sms, Smb = PS[4 + sidx % 2], PB[4 + sidx % 2]
                                P, P_b = Pr.next()
                                jq_far0 = max(j0, kt - 4 * tb + 2)
                                ACT(P[:, cols], Sps[:, cols], AF.Exp, [Spb, cfar_b], [P_b], bias=cfar[:, hd:hd + 1], scale=sc_a)
                                for jq in range(j0, min(4, jq_far0)):
                                    delta = kt - (4 * tb + jq)
                                    bi = (0 if delta == 0 else 1) * 4 + hd
                                    qc = slice(jq * 128, (jq + 1) * 128)
                                    TT(P[:, qc], P[:, qc], Eb[:, bi, :], ALU.mult, [P_b, Eb_b], [P_b], eng=self.pool)
                                first = (kt == 0)
                                last = (kt == nkt - 1)
                                MM(Ops[:, cols], Va[:, kt, hd * 128:(hd + 1) * 128], P[:, cols], first, last, [Va_b, P_b], [Opb],
                                   inc=True)
                                acc, acc_b = accA_r[sidx % 2]
                                if first:
                                    CP(acc[:, cols], P[:, cols], [P_b], [acc_b])
                                else:
                                    TT(acc[:, cols], acc[:, cols], P[:, cols], ALU.add, [acc_b, P_b], [acc_b])
                                if last:
                                    def fin(acc=acc, acc_b=acc_b, hd=hd, m=m, Ops=Ops, Opb=Opb, Sms=Sms, Smb=Smb):
                                        CP(accAb[:], acc[:], [acc_b], [accAb_b])
                                        MM(Sms[:], ones_bf[:], accAb[:], True, True, [ones_b, accAb_b], [Smb], inc=True)
                                        ACT(rc[:], Sms[:], AF.Ln, [Smb], [rc_b])
                                        ACT(rc[:], rc[:], AF.Exp, [rc_b], [rc_b], scale=-1.0)
                                        if m == 0:
                                            TT(o0[:], Ops[:], rc[:], ALU.mult, [Opb, rc_b], [o0_b])
                                        else:
                                            TT(o1[:], Ops[:], rc[:], ALU.mult, [Opb, rc_b], [o1_b])
                                            STT(dd[:], o1[:], neglam[:, l:l + 1], o0[:], ALU.mult, ALU.add, [o1_b, o0_b, neglam_b], [dd_b])
                                            sq, sq_b = self.n_sq.next()
                                            ACT(sq[:], dd[:], AF.Square, [dd_b], [sq_b])
                                            ps, pb = psg()
                                            MM(ps[:], ones_bf[:], sq[:], True, True, [ones_b, sq_b], [pb])
                                            r, r_b = self.n_r.next()
                                            self.rstd(r[:], ps[:], 1.0 / 128, [pb, cst_b], [r_b])
                                            STT(o_a[:, hd, blk], dd[:], gout[:, l:l + 1], r[:], ALU.mult, ALU.mult,
                                                [dd_b, r_b, gout_b], [o_a_b])
                                    pending.append((i + 2, fin))

                            pending = []
                            emit_S(steps[0], 0)
                            emit_S(steps[1], 1)
                            for i, st in enumerate(steps):
                                if i + 2 < len(steps):
                                    emit_S(steps[i + 2], i + 2)
                                emit_rest(st, i)
                                while pending and pending[0][0] <= i:
                                    pending.pop(0)[1]()
                            while pending:
                                pending.pop(0)[1]()
                        self.dump("o_a", o_a[:], [o_a_b])
                        fw.barrier()
                    stop_at(2)

                    with ExitStack() as ses:
                        w8_r = [alloc(f"w8_{i}", [128, 8, 512], BF16, ses) for i in range(2)]
                        wb_r = [alloc(f"wb{i}", [128, 4, 512], BF16, ses) for i in range(2)]
                        rings = {"w8": 0, "b": 0}
                        sgw_f, sgw_fb = alloc("sgw_f", [128, 4, 128], F32, ses)
                        sgw, sgw_b = alloc("sgw", [128, 4, 128], BF16, ses)
                        sgb, sgb_b = alloc("sgb", [128, 4, 128], F32, ses)
                        vg, vg_b = alloc("vg", [128, 512], F32, ses)
                        uT, uT_b = alloc("uT", [128, 4, TB], BF16, ses)
                        o_b, o_b_b = alloc("o_b", [128, 4, TB], BF16, ses)
                        vf, vf_b = alloc("vf", [128, 512], F32, ses)
                        vjunk, vjunk_b = alloc("vjunk", [128, 512], BF16, ses)
                        vss, vss_b = alloc("vss", [128, 2], F32, ses)
                        vn, vn_b = alloc("vn", [128, 512], BF16, ses)
                        vt, vt_b = alloc("vt", [128, 128], F32, ses)
                        gsb, gsb_b = alloc("gsb", [128, TB], F32, ses)
                        mt, mt_b = alloc("mt", [128, TB], F32, ses)
                        macc, macc_b = alloc("macc", [128, 4, TB], F32, ses)
                        merged, merged_b = alloc("merged", [128, 8, TB], BF16, ses)
                        DMA(sgw_f[:], sgu_wT_d[l], (), [sgw_fb])
                        DMA(sgb[:], sgu_bb_d[l], (), [sgb_b])
                        DMA(vg[:], sgu_vg_d[l], (), [vg_b])
                        MSET(sgw_f[64:128, :, 0:64], 0.0, [sgw_fb])
                        CP(sgw[:], sgw_f[:], [sgw_fb], [sgw_b])
                        self.psg_banks = ALLB
                        for tb in range(NB):
                            blk = slice(tb * TB, (tb + 1) * TB)
                            if tb == 0:
                                DMA(hbf_flat, h_scr[0], [h_scr_b[0]], [hbf_b])
                            wu, wu_b = w8_r[rings["w8"] % 2]; rings["w8"] += 1
                            DMAC(wu[:], w_in_d[l, :, 1536:2048].rearrange("(kc p) n -> p kc n", p=128), (), [wu_b])
                            wv, wv_b = w8_r[rings["w8"] % 2]; rings["w8"] += 1
                            DMAC(wv[:], w_in_d[l, :, 2048:2560].rearrange("(kc p) n -> p kc n", p=128), (), [wv_b])
                            for cu in range(4):
                                ps, pb = psg()
                                for kc in range(8):
                                    MM(ps[:], wu[:, kc, cu * 128:(cu + 1) * 128], hbf[:, kc, :], kc == 0, kc == 7, [wu_b, hbf_b], [pb])
                                ACT(uT[:, cu, :], ps[:], AF.Gelu_apprx_tanh, [pb], [uT_b])
                            for tt in range(4):
                                ps, pb = psg()
                                for kc in range(8):
                                    MM(ps[:], hbf[:, kc, tt * 128:(tt + 1) * 128], wv[:, kc, :], kc == 0, kc == 7, [hbf_b, wv_b], [pb])
                                ACT(vf[:], ps[:], AF.Gelu_apprx_tanh, [pb], [vf_b])
                                fw.op(self.act, lambda: nc.scalar.activation(out=vjunk[:], in_=vf[:], func=AF.Square,
                                                                             accum_out=vss[:, 0:1]),
                                      [vf_b], [vjunk_b, vss_b])
                                ACT(vss[:, 1:2], vss[:, 0:1], AF.Ln, [vss_b, cst_b], [vss_b], bias=cst[:, 0:1], scale=1.0 / 512)
                                ACT(vss[:, 1:2], vss[:, 1:2], AF.Exp, [vss_b], [vss_b], scale=-0.5)
                                STT(vn[:], vf[:], vss[:, 1:2], vg[:], ALU.mult, ALU.mult, [vf_b, vss_b, vg_b], [vn_b])
                                for g in range(4):
                                    ps2, pb2 = psg()
                                    MM(ps2[:, 0:128], vn[:, g * 128:(g + 1) * 128], sgw[:, g, :], True, True, [vn_b, sgw_b], [pb2])
                                    TT(vt[:], ps2[:, 0:128], sgb[:, g, :], ALU.add, [pb2, sgb_b], [vt_b])
                                    TT(o_b[:, g, tt * 128:(tt + 1) * 128], vt[:], uT[:, g, tt * 128:(tt + 1) * 128], ALU.mult,
                                       [vt_b, uT_b], [o_b_b])
                            if l == 0 and tb == 0:
                                self.dump("o_b0", o_b[:], [o_b_b])
                            for dcg in range(2):
                                for n in range(3):
                                    wg, wg_b = w8_r[rings["w8"] % 2]; rings["w8"] += 1
                                    c0 = 2976 + n * 1024 + dcg * 512
                                    DMAC(wg[:], w_in_d[l, :, c0:c0 + 512].rearrange("(kc p) n -> p kc n", p=128), (), [wg_b])
                                    wb, wb_b = wb_r[rings["b"] % 2]; rings["b"] += 1
                                    DMAC(wb[:], w_br_d[l, n, :, dcg * 512:(dcg + 1) * 512].rearrange("(kc p) n -> p kc n", p=128), (), [wb_b])
                                    src, src_b = [(o_a, o_a_b), (o_b, o_b_b), (o_c, o_c_b)][n]
                                    for dci in range(4):
                                        psa, pba = psg()
                                        for kc in range(8):
                                            MM(psa[:], wg[:, kc, dci * 128:(dci + 1) * 128], hbf[:, kc, :], kc == 0, kc == 7, [wg_b, hbf_b], [pba])
                                        ACT(gsb[:], psa[:], AF.Sigmoid, [pba], [gsb_b])
                                        psb, pbb = psg()
                                        for kc in range(4):
                                            rhs = src[:, kc, :] if n == 1 else src[:, kc, blk]
                                            MM(psb[:], wb[:, kc, dci * 128:(dci + 1) * 128], rhs, kc == 0, kc == 3, [wb_b, src_b], [pbb])
                                        if n == 0:
                                            TT(macc[:, dci, :], psb[:], gsb[:], ALU.mult, [pbb, gsb_b], [macc_b])
                                        else:
                                            TT(mt[:], psb[:], gsb[:], ALU.mult, [pbb, gsb_b], [mt_b])
                                            if n == 1:
                                                TT(macc[:, dci, :], macc[:, dci, :], mt[:], ALU.add, [macc_b, mt_b], [macc_b])
                                            else:
                                                TT(merged[:, dcg * 4 + dci, :], macc[:, dci, :], mt[:], ALU.add, [macc_b, mt_b], [merged_b])
                            if l == 0 and tb == 0:
                                self.dump("merged0", merged[:], [merged_b])
                            if tb + 1 < NB:
                                DMA(hbf_flat, h_scr[tb + 1], [h_scr_b[tb + 1]], [hbf_b])
                            for dcg in range(2):
                                wo, wo_b = w8_r[rings["w8"] % 2]; rings["w8"] += 1
                                DMAC(wo[:], w_out_d[l, :, dcg * 512:(dcg + 1) * 512].rearrange("(kc p) n -> p kc n", p=128), (), [wo_b])
                                for dci in range(4):
                                    dc = dcg * 4 + dci
                                    ps, pb = psg()
                                    for kc in range(8):
                                        MM(ps[:], wo[:, kc, dci * 128:(dci + 1) * 128], merged[:, kc, :], kc == 0, kc == 7, [wo_b, merged_b], [pb])
                                    STT(xT[:, dc, blk], ps[:], MOD[:, l, 16 + dc:16 + dc + 1], xT[:, dc, blk], ALU.mult, ALU.add,
                                        [pb, MOD_b, xTB[tb]], [xTB[tb]])
                        fw.barrier()
                    if l == 0:
                        self.dump("x_mid", xT[:], xTB)
                    stop_at(3)

                    mes.close()
                    with ExitStack() as ses:
                        h2, h2_b = alloc("h2", [128, 8, S], BF16, ses)
                        rs = ExitStack()
                        h2f, h2f_b = alloc("h2f", [128, 8, TB], F32, rs)
                        combT, combT_b = alloc("combT", [32, S], F32, rs)
                        w_r, w_r_b = alloc("w_r", [128, 8, 36], F32, rs)
                        DMA(w_r[:], w_r_d[l].rearrange("(kc p) n -> p kc n", p=128), (), [w_r_b])
                        NT = 16
                        lgA, lg_b = alloc("lgA", [128, NT, 36], F32, rs)
                        ohg, ohg_b = alloc("ohg", [128, NT, 4], F32, rs)
                        ex4, ex4_b = alloc("ex4", [128, NT, 4], F32, rs)
                        v1, v1_b = alloc("v1", [128, 8, NT], F32, rs)
                        el3, el3_b = alloc("el3", [128, NT, 32], F32, rs)
                        els, els_b = alloc("els", [128, NT, 8], F32, rs)
                        els2, els2_b = alloc("els2", [128, NT, 8], F32, rs)
                        oh1, oh1_b = alloc("oh1", [128, NT, 8], F32, rs)
                        oh2, oh2_b = alloc("oh2", [128, NT, 8], F32, rs)
                        comb, comb_b = alloc("comb", [128, NT, 32], F32, rs)
                        h2B = [Buf(f"h2_{b}") for b in range(NB)]
                        self.psg_banks = ALLB
                        for tb in range(NB):
                            blk = slice(tb * TB, (tb + 1) * TB)
                            make_h(tb, A2, B2, h2[:, :, blk], h2B[tb], ses, l, hf=h2f, hf_b=h2f_b)
                            for tt in range(4):
                                ps, pb = psg()
                                for kc in range(8):
                                    MM(ps[:, 0:36], h2f[:, kc, tt * 128:(tt + 1) * 128], w_r[:, kc, :], kc == 0, kc == 7, [h2f_b, w_r_b], [pb])
                                TT(lgA[:, tb * 4 + tt, :], ps[:, 0:36], b_r[:, l, :], ALU.add, [pb, b_r_b], [lg_b])
                        def red(out, in_, op, rd, wr):
                            fw.op(self.dve, lambda: nc.vector.tensor_reduce(out=out, in_=in_, axis=AX.X, op=op), rd, wr)
                        gl = lgA[:, :, 0:4]
                        el4 = lgA[:, :, 4:36].rearrange("p t (g e) -> p t g e", g=4)
                        gmax, gsum, g_w, m1, m2, e2, w1, w2 = [v1[:, i, :] for i in range(8)]
                        bc4 = lambda a: a.unsqueeze(2).broadcast_to([128, NT, 4])
                        bc8 = lambda a: a.unsqueeze(2).broadcast_to([128, NT, 8])
                        red(gmax, gl, ALU.max, [lg_b], [v1_b])
                        TT(ohg[:], gl, bc4(gmax), ALU.is_equal, [lg_b, v1_b], [ohg_b])
                        TT(ex4[:], gl, bc4(gmax), ALU.subtract, [lg_b, v1_b], [ex4_b])
                        ACT(ex4[:], ex4[:], AF.Exp, [ex4_b], [ex4_b])
                        red(gsum, ex4[:], ALU.add, [ex4_b], [v1_b])
                        RECIP(g_w, gsum, [v1_b], [v1_b])
                        TT(el3[:].rearrange("p t (g e) -> p t g e", g=4), el4,
                           ohg[:].unsqueeze(3).broadcast_to([128, NT, 4, 8]), ALU.mult, [lg_b, ohg_b], [el3_b])
                        red(els[:], el3[:].rearrange("p t (g e) -> p t e g", g=4), ALU.add, [el3_b], [els_b])
                        red(m1, els[:], ALU.max, [els_b], [v1_b])
                        TT(oh1[:], els[:], bc8(m1), ALU.is_equal, [els_b, v1_b], [oh1_b])
                        STT(els2[:], oh1[:], -1.0e30, els[:], ALU.mult, ALU.add, [oh1_b, els_b], [els2_b])
                        red(m2, els2[:], ALU.max, [els2_b], [v1_b])
                        TT(oh2[:], els2[:], bc8(m2), ALU.is_equal, [els2_b, v1_b], [oh2_b])
                        TT(e2, m2, m1, ALU.subtract, [v1_b], [v1_b])
                        ACT(e2, e2, AF.Exp, [v1_b], [v1_b])
                        TS(w1, e2, 1.0, None, ALU.add, None, [v1_b], [v1_b])
                        RECIP(w1, w1, [v1_b], [v1_b])
                        TT(w2, e2, w1, ALU.mult, [v1_b], [v1_b])
                        TT(w1, w1, g_w, ALU.mult, [v1_b], [v1_b])
                        TT(w2, w2, g_w, ALU.mult, [v1_b], [v1_b])
                        TT(oh1[:], oh1[:], bc8(w1), ALU.mult, [oh1_b, v1_b], [oh1_b])
                        TT(oh2[:], oh2[:], bc8(w2), ALU.mult, [oh2_b, v1_b], [oh2_b])
                        TT(oh1[:], oh1[:], oh2[:], ALU.add, [oh1_b, oh2_b], [oh1_b])
                        TT(comb[:].rearrange("p t (g e) -> p t g e", g=4), ohg[:].unsqueeze(3).broadcast_to([128, NT, 4, 8]),
                           oh1[:].unsqueeze(2).broadcast_to([128, NT, 4, 8]), ALU.mult, [ohg_b, oh1_b], [comb_b])
                        for tb in range(NB):
                            ps2, pb2 = psg()
                            for tt in range(4):
                                fw.op(self.pe, lambda tt=tt: nc.tensor.transpose(ps2[0:32, tt * 128:(tt + 1) * 128], comb[:, tb * 4 + tt, :], ident[:]),
                                      [comb_b, ident_b], [pb2], inc=(tt == 3))
                            CP(combT[:, tb * TB:(tb + 1) * TB], ps2[0:32, :], [pb2], [combT_b])
                        self.dump("combT", combT[:], [combT_b])
                        if l == 0:
                            self.dump("h2", h2[:], h2B)
                        DMA(comb_scr, combT[:], [combT_b], [comb_scr_b])
                        fw.barrier()
                        rs.close()
                        stop_at(4)
                        self.psg_banks = [4, 5, 6, 7]
                        EE = 2
                        wgu_r = [alloc(f"wgu{i}", [128, 2, 8, 256], BF16, ses) for i in range(3)]
                        wd_r = [alloc(f"wd{i}", [128, 2, D], BF16, ses) for i in range(4)]
                        actT, actT_b0 = alloc("actT", [128, EE, 2, S], BF16, ses)
                        actB = [[Buf(f"act{e}_{b}") for b in range(NB)] for e in range(EE)]
                        sil_r = [alloc(f"sil{i}", [128, TB], BF16, ses) for i in range(2)]
                        ut_r = [alloc(f"ut{i}", [128, TB], BF16, ses) for i in range(2)]
                        cb_r = [alloc(f"cb{i}", [128, TB], F32, ses) for i in range(3)]
                        cnt = {"gu": 0, "d": 0, "s": 0, "cb": 0, "ps": 0}
                        for e0 in range(0, 32, EE):
                            wds = []
                            for ei in range(EE):
                                e = e0 + ei
                                wgu, wgu_b = wgu_r[cnt["gu"] % 3]; cnt["gu"] += 1
                                DMAC(wgu[:, 0, :, :], weg_d[l, e].rearrange("(kc p) n -> p kc n", p=128), (), [wgu_b])
                                DMAC(wgu[:, 1, :, :], weu_d[l, e].rearrange("(kc p) n -> p kc n", p=128), (), [wgu_b])
                                wd, wd_b = wd_r[cnt["d"] % 4]; cnt["d"] += 1
                                DMAC(wd[:], wed_d[l, e].rearrange("(fc p) n -> p fc n", p=128), (), [wd_b])
                                wds.append((wd, wd_b))
                                for tb in range(NB):
                                    blk = slice(tb * TB, (tb + 1) * TB)
                                    cb, cb_b = cb_r[cnt["cb"] % 3]; cnt["cb"] += 1
                                    DMA(cb[:], comb_scr[e:e + 1, blk].partition_broadcast(128), [comb_scr_b], [cb_b])
                                    for fc in range(2):
                                        k = cnt["ps"]; cnt["ps"] += 1
                                        aps, apb = PS[k % 2], PB[k % 2]
                                        ups, upb = PS[2 + k % 2], PB[2 + k % 2]
                                        for kc in range(8):
                                            MM(aps[:], wgu[:, 0, kc, fc * 128:(fc + 1) * 128], h2[:, kc, blk], kc == 0, kc == 7, [wgu_b, h2B[tb]], [apb])
                                        for kc in range(8):
                                            MM(ups[:], wgu[:, 1, kc, fc * 128:(fc + 1) * 128], h2[:, kc, blk], kc == 0, kc == 7, [wgu_b, h2B[tb]], [upb])
                                        sil, sil_b = sil_r[k % 2]
                                        ut, ut_b = ut_r[k % 2]
                                        ACT(sil[:], aps[:], AF.Silu, [apb], [sil_b])
                                        TT(ut[:], ups[:], cb[:], ALU.mult, [upb, cb_b], [ut_b])
                                        TT(actT[:, ei, fc, blk], sil[:], ut[:], ALU.mult, [sil_b, ut_b], [actB[ei][tb]], eng=self.pool)
                            for tb in range(NB):
                                blk = slice(tb * TB, (tb + 1) * TB)
                                for dc in range(8):
                                    ps, pb = psg()
                                    n_mm = EE * 2
                                    i_mm = 0
                                    for ei in range(EE):
                                        wd, wd_b = wds[ei]
                                        for fc in range(2):
                                            MM(ps[:], wd[:, fc, dc * 128:(dc + 1) * 128], actT[:, ei, fc, blk], i_mm == 0, i_mm == n_mm - 1,
                                               [wd_b, actB[ei][tb]], [pb])
                                            i_mm += 1
                                    STT(xT[:, dc, blk], ps[:], MOD[:, l, 40 + dc:40 + dc + 1], xT[:, dc, blk], ALU.mult, ALU.add,
                                        [pb, MOD_b, xTB[tb]], [xTB[tb]])
                        fw.barrier()

            emit_output()
            self.ninstr = fw.ninstr
        return nc


def _t5_bucket(rel):
    nb = 16
    max_exact = 8
    n = np.abs(rel)
    large = max_exact + (np.log(np.maximum(n, 1).astype(np.float32) / max_exact)
                         / math.log(128 / max_exact) * (nb - max_exact)).astype(np.int32)
    large = np.minimum(large, nb - 1)
    return np.where(rel > 0, nb, 0) + np.where(n < max_exact, n, large)


def _bucket_table():
    rel = np.arange(-300, 300)
    nb = 16
    max_exact = 8
    n = np.abs(rel)
    ratio = (np.maximum(n, 1).astype(np.float32) / np.float32(max_exact)).astype(np.float32)
    large = max_exact + (np.log(ratio).astype(np.float32) / np.float32(math.log(128 / max_exact))
                         * np.float32(nb - max_exact)).astype(np.int32)
    large = np.minimum(large, nb - 1)
    return np.where(rel > 0, nb, 0) + np.where(n < max_exact, n, large)


def prep_shared(inp, depth):
    f = np.float32
    g = {}
    g["w_ada"] = np.ascontiguousarray(inp["w_ada"], f)
    g["b_adaT"] = np.ascontiguousarray(inp["b_ada"].reshape(DEPTH, 48, 128).transpose(2, 0, 1), f)
    g["norm_gT"] = np.ascontiguousarray(inp["norm_g"].reshape(DEPTH, 2, 8, 128).transpose(3, 0, 1, 2), f)
    g["w_in"] = np.ascontiguousarray(inp["w_in"], f)
    qk = inp["diff_qk_g"]
    g["qkgA"] = np.ascontiguousarray(np.tile(qk.transpose(2, 0, 1), (2, 1, 1)), f)
    g["dlam"] = np.ascontiguousarray(np.broadcast_to(inp["diff_lambda"].reshape(1, DEPTH, 256), (128, DEPTH, 256)), f)
    g["goutA"] = np.ascontiguousarray(inp["diff_out_g"].T, f)
    bt = _bucket_table()
    ki = np.arange(128)[:, None]
    qi = np.arange(128)[None, :]
    tab = inp["rel_bias"]
    tiles = np.zeros((128, 8, 128), f)
    for di, delta in enumerate((0, -1)):
        rel = ki + 128 * delta - qi
        bidx = bt[rel + 300]
        for h in range(4):
            t = tab[bidx, h]
            if delta == 0:
                allowed = (ki // 64) <= (qi // 64)
                t = np.where(allowed, t, f(-30000.0))
            tiles[:, di * 4 + h, :] = t
    g["biasA"] = tiles
    g["cfar"] = np.ascontiguousarray(np.broadcast_to(tab[15][None, :], (128, 4)), f)
    g["sgu_vg"] = np.ascontiguousarray(np.broadcast_to(inp["sgu_v_g"][:, None, :], (DEPTH, 128, 512)), f)
    g["sgu_wT"] = np.ascontiguousarray(inp["sgu_w"].transpose(0, 3, 1, 2), f)
    g["sgu_bb"] = np.ascontiguousarray(np.broadcast_to(inp["sgu_b"][:, None, :, :], (DEPTH, 128, 4, 128)), f)
    lg = inp["mla_lat_g"]
    g["latg"] = np.ascontiguousarray(lg.reshape(DEPTH, 3, 128).transpose(2, 0, 1), f)
    wuq = inp["mla_w_uq"].reshape(DEPTH, 256, 8, 96)
    g["w_uq"] = np.ascontiguousarray(np.concatenate([wuq[..., :64].reshape(DEPTH, 256, 512),
                                                     wuq[..., 64:].reshape(DEPTH, 256, 256)], axis=-1), f)
    wukv = inp["mla_w_ukv"].reshape(DEPTH, 128, 8, 128)
    g["w_ukv"] = np.ascontiguousarray(np.concatenate([wukv[..., :64].reshape(DEPTH, 128, 512),
                                                      wukv[..., 64:].reshape(DEPTH, 128, 512)], axis=-1), f)
    qkc = inp["mla_qk_g"]
    g["qkgCn"] = np.ascontiguousarray(np.tile(qkc[:, :, :64].transpose(2, 0, 1), (2, 1, 1)), f)
    g["qkgCr"] = np.ascontiguousarray(qkc[:, :, 64:].transpose(2, 0, 1), f)
    g["w_branch"] = np.ascontiguousarray(inp["w_branch"], f)
    g["w_out"] = np.ascontiguousarray(inp["w_out"], f)
    g["w_r"] = np.ascontiguousarray(np.concatenate([inp["router_g_w"], inp["router_e_w"]], axis=-1), f)
    br = np.concatenate([inp["router_g_b"], inp["router_e_b"]], axis=-1)
    g["b_r"] = np.ascontiguousarray(np.broadcast_to(br[None], (128, DEPTH, 36)), f)
    g["w_e_gate"] = np.ascontiguousarray(inp["w_e_gate"].reshape(DEPTH, 32, D, 256), f)
    g["w_e_up"] = np.ascontiguousarray(inp["w_e_up"].reshape(DEPTH, 32, D, 256), f)
    g["w_e_down"] = np.ascontiguousarray(inp["w_e_down"].reshape(DEPTH, 32, 256, D), f)
    inv_freq = (10000.0 ** (-np.arange(0, 32, 2, dtype=np.float32) / np.float32(32))).astype(f)
    g["invf"] = np.ascontiguousarray(np.concatenate([inv_freq, inv_freq])[:, None], f)
    R = np.zeros((32, 32), f)
    for i in range(16):
        R[i + 16, i] = -1.0
        R[i, i + 16] = 1.0
    g["R32T"] = R
    g["ident"] = np.eye(128, dtype=f)
    return g


def prep_core(inp, b):
    f = np.float32
    m = {}
    m["xT"] = np.ascontiguousarray(np.asarray(inp["x"][b], f).T)
    m["cT"] = np.ascontiguousarray(np.asarray(inp["c"][b], f).reshape(8, 128).T)
    m["pos32"] = np.ascontiguousarray(np.broadcast_to(np.asarray(inp["positions"][b], np.int32)[None, :], (32, S)))
    return m


_CACHE = {}


def kernel(**inputs):
    inp = {k: np.asarray(v) for k, v in inputs.items()}
    depth = int(os.environ.get("K_DEPTH", DEPTH))
    if depth not in _CACHE:
        kb = K(depth)
        _CACHE[depth] = kb.build()
    nc = _CACHE[depth]
    shared = prep_shared(inp, depth)
    in_maps = []
    for b in range(8):
        m = dict(shared)
        m.update(prep_core(inp, b))
        in_maps.append(m)
    res = run_bass_kernel_spmd(nc, in_maps, core_ids=list(range(8)))
    out = np.stack([np.asarray(r["outT"], np.float32).T for r in res.results], axis=0)
    return np.ascontiguousarray(out)
```
